# Optimizing a Trainium2 kernel written in Bass

```python
import jax, jax.numpy as jnp
from jax import lax
import numpy as np

D_MODEL = 2048
BATCH = 8
SEQ = 4096
DEPTH = 4

RET_HEADS = 4
RET_DK = 128
RET_DV = 128
RET_WIDTH = RET_HEADS * RET_DV
RET_CHUNK = 128
ROPE_BASE = 10000.0
DIL_HEADS = 6
DIL_HD = 128
DIL_WIDTH = DIL_HEADS * DIL_HD
DIL_PATTERNS = ((128, 1), (512, 4), (2048, 16))
DIL_BLOCK = 128
GLA_HEADS = 6
GLA_DK = 64
GLA_DV = 128
GLA_QK_WIDTH = GLA_HEADS * GLA_DK
GLA_V_WIDTH = GLA_HEADS * GLA_DV
GLA_GATE_RANK = 16
GLA_TAU = 16.0
GLA_CHUNK = 64
MIX_WIDTH = RET_WIDTH + DIL_WIDTH + GLA_V_WIDTH
N_GROUPS = 4
EXPERTS_PER_GROUP = 8
N_EXPERTS = N_GROUPS * EXPERTS_PER_GROUP
TOP_K = 2
D_FF_EXPERT = 512
MOE_BLOCK = 128
LN_EPS = 1e-5
ALPHA = (2 * DEPTH) ** 0.25
BETA = (8 * DEPTH) ** -0.25

SPLIT_SIZES = (RET_WIDTH, RET_WIDTH, RET_WIDTH, RET_WIDTH,
               DIL_WIDTH, DIL_WIDTH, DIL_WIDTH,
               GLA_QK_WIDTH, GLA_QK_WIDTH, GLA_V_WIDTH, GLA_V_WIDTH, GLA_GATE_RANK)
IN_WIDTH = sum(SPLIT_SIZES)
SPLIT_POINTS = tuple(int(v) for v in np.cumsum(SPLIT_SIZES)[:-1])

kernel_name = 'hymba_style_ret_dilated_gla_hmoe_deepnorm'


def layer_norm(x, g, b):
    xf = x.astype(jnp.float32)
    mu = xf.mean(-1, keepdims=True)
    var = jnp.square(xf - mu).mean(-1, keepdims=True)
    return ((xf - mu) * lax.rsqrt(var + LN_EPS)).astype(x.dtype) * g + b


def head_norm(t):
    tf = t.astype(jnp.float32)
    mu = tf.mean(-1, keepdims=True)
    var = jnp.square(tf - mu).mean(-1, keepdims=True)
    return ((tf - mu) * lax.rsqrt(var + LN_EPS)).astype(t.dtype)


def heads(t, n):
    b, s, w = t.shape
    return t.reshape(b, s, n, w // n).transpose(0, 2, 1, 3)


def merge(t):
    b, h, s, d = t.shape
    return t.transpose(0, 2, 1, 3).reshape(b, s, h * d)


def rotate(t, pos):
    half = t.shape[-1] // 2
    inv = ROPE_BASE ** (-jnp.arange(half, dtype=jnp.float32) / half)
    ang = pos.astype(jnp.float32)[:, None] * inv[None, :]
    cos, sin = jnp.cos(ang).astype(t.dtype), jnp.sin(ang).astype(t.dtype)
    t1, t2 = t[..., :half], t[..., half:]
    return jnp.concatenate([t1 * cos - t2 * sin, t1 * sin + t2 * cos], axis=-1)


def retention(q, k, v):
    B, H, S, dk = q.shape
    C = RET_CHUNK
    N = S // C
    log_g = jnp.log1p(-jnp.exp2(-5.0 - jnp.arange(H, dtype=jnp.float32)))
    idx = jnp.arange(C, dtype=jnp.float32)
    diff = idx[:, None] - idx[None, :]
    intra_decay = jnp.where(diff >= 0, jnp.exp(jnp.maximum(diff, 0.0)[None] * log_g[:, None, None]), 0.0).astype(q.dtype)
    k_decay = jnp.exp((C - 1 - idx)[None] * log_g[:, None]).astype(q.dtype)
    q_decay = jnp.exp((idx + 1)[None] * log_g[:, None]).astype(q.dtype)
    chunk_decay = jnp.exp(C * log_g).astype(q.dtype)[None, :, None, None]
    qc = q.reshape(B, H, N, C, dk)
    kc = k.reshape(B, H, N, C, dk)
    vc = v.reshape(B, H, N, C, -1)
    scores = jnp.einsum('bhncd,bhnmd->bhncm', qc, kc) * intra_decay[None, :, None]
    intra = jnp.einsum('bhncm,bhnme->bhnce', scores, vc)
    kv = jnp.einsum('bhncd,bhnce->bhnde', kc * k_decay[None, :, None, :, None], vc)

    def step(state, kv_n):
        return state * chunk_decay + kv_n, state

    _, prev = lax.scan(step, jnp.zeros_like(kv[:, :, 0]), jnp.moveaxis(kv, 2, 0))
    prev = jnp.moveaxis(prev, 0, 2)
    cross = jnp.einsum('bhncd,bhnde->bhnce', qc * q_decay[None, :, None, :, None], prev)
    return (intra + cross).reshape(B, H, S, -1)


def dilated_branch(q, k, v, window, dilation):
    B, H, S, d = q.shape
    W = window // dilation
    Q = DIL_BLOCK
    span = dilation * Q
    Sp = -(-S // span) * span
    L = Sp // dilation
    nb = L // Q

    def to_sub(t):
        t = jnp.pad(t, ((0, 0), (0, 0), (0, Sp - S), (0, 0)))
        t = t.reshape(B, H, L, dilation, d).transpose(0, 1, 3, 2, 4)
        return t.reshape(B, H, dilation, nb, Q, d)

    def with_prev(t):
        prev = jnp.pad(t, ((0, 0), (0, 0), (0, 0), (1, 0), (0, 0), (0, 0)))[:, :, :, :-1]
        return jnp.concatenate([prev, t], axis=4)

    qs = to_sub(q)
    kb = with_prev(to_sub(k))
    vb = with_prev(to_sub(v))
    scores = jnp.einsum('bhrnqd,bhrnkd->bhrnqk', qs, kb, preferred_element_type=jnp.float32) * (d ** -0.5)
    qi = jnp.arange(Q)[:, None] + Q
    kj = jnp.arange(2 * Q)[None, :]
    dist = qi - kj
    band = (dist >= 0) & (dist <= W)
    blk = jnp.arange(nb)[:, None, None]
    valid = band[None] & ((blk > 0) | (kj[None] >= Q))
    scores = jnp.where(valid, scores, jnp.finfo(jnp.float32).min)
    m = scores.max(-1)
    p = jnp.exp(scores - m[..., None])
    s = p.sum(-1)
    o = jnp.einsum('bhrnqk,bhrnkd->bhrnqd', p, vb.astype(jnp.float32)) / s[..., None]

    def from_sub(t):
        extra = t.shape[5:]
        t = t.reshape((B, H, dilation, L) + extra)
        t = jnp.moveaxis(t, 2, 3).reshape((B, H, Sp) + extra)
        return t[:, :, :S]

    return from_sub(o), from_sub(m), from_sub(s)


def dilated_attention(q, k, v):
    outs, maxes, dens = [], [], []
    for window, dilation in DIL_PATTERNS:
        o, m, s = dilated_branch(q, k, v, window, dilation)
        outs.append(o)
        maxes.append(m)
        dens.append(s)
    m_all = jnp.stack(maxes)
    w = jnp.exp(m_all - m_all.max(0, keepdims=True)) * jnp.stack(dens)
    w = w / w.sum(0, keepdims=True)
    out = jnp.einsum('pbhs,pbhsd->bhsd', w, jnp.stack(outs))
    return out.astype(q.dtype)


def gla(q, k, v, log_a):
    B, H, S, dk = q.shape
    dv = v.shape[-1]
    C = GLA_CHUNK
    N = S // C
    f32 = jnp.float32

    def chunks(t):
        return jnp.moveaxis(t.astype(f32).reshape(B, H, N, C, t.shape[-1]), 2, 0)

    causal = jnp.tril(jnp.ones((C, C), dtype=bool))[:, :, None]

    def step(state, inp):
        qc, kc, vc, ac = inp
        b = jnp.cumsum(ac, axis=2)
        dec = jnp.exp(jnp.where(causal, b[:, :, :, None, :] - b[:, :, None, :, :], -jnp.inf))
        A = jnp.einsum('bhtd,bhsd,bhtsd->bhts', qc, kc, dec)
        intra = jnp.einsum('bhts,bhse->bhte', A, vc)
        inter = jnp.einsum('bhtd,bhde->bhte', qc * jnp.exp(b), state)
        b_last = b[:, :, -1:, :]
        new_state = jnp.exp(b_last[:, :, 0, :, None]) * state + jnp.einsum('bhsd,bhse->bhde', kc * jnp.exp(b_last - b), vc)
        return new_state, intra + inter

    state0 = jnp.zeros((B, H, dk, dv), f32)
    _, out = lax.scan(step, state0, (chunks(q), chunks(k), chunks(v), chunks(log_a)))
    return jnp.moveaxis(out, 0, 2).reshape(B, H, S, dv).astype(v.dtype)


def hier_moe(x, w_rg, b_rg, w_re, b_re, w_gate, w_up, w_down):
    B, S, D = x.shape
    T = B * S
    xf = x.reshape(T, D)
    g_logits = (xf @ w_rg + b_rg).astype(jnp.float32)
    g_probs = jax.nn.softmax(g_logits, axis=-1)
    g_sel = jnp.argmax(g_logits, axis=-1)
    g_w = jnp.take_along_axis(g_probs, g_sel[:, None], axis=-1)[:, 0]
    e_logits = (xf @ w_re + b_re).astype(jnp.float32).reshape(T, N_GROUPS, EXPERTS_PER_GROUP)
    sel_logits = jnp.take_along_axis(e_logits, g_sel[:, None, None], axis=1)[:, 0]
    top_v, top_i = lax.top_k(sel_logits, TOP_K)
    gate = g_w[:, None] * jax.nn.softmax(top_v, axis=-1)
    expert_id = g_sel[:, None].astype(jnp.int32) * EXPERTS_PER_GROUP + top_i.astype(jnp.int32)

    n_assign = T * TOP_K
    e_flat = expert_id.reshape(-1)
    w_flat = gate.reshape(-1)
    tok_flat = jnp.arange(n_assign, dtype=jnp.int32) // TOP_K
    order = jnp.argsort(e_flat)
    e_sorted = e_flat[order]
    counts = jnp.bincount(e_flat, length=N_EXPERTS)
    starts = jnp.cumsum(counts) - counts
    pcounts = (counts + MOE_BLOCK - 1) // MOE_BLOCK * MOE_BLOCK
    pends = jnp.cumsum(pcounts)
    pstarts = pends - pcounts
    dest = pstarts[e_sorted] + jnp.arange(n_assign, dtype=jnp.int32) - starts[e_sorted]
    n_rows = -(-(n_assign + N_EXPERTS * (MOE_BLOCK - 1)) // MOE_BLOCK) * MOE_BLOCK
    n_blk = n_rows // MOE_BLOCK
    row_tok = jnp.full((n_rows,), T, jnp.int32).at[dest].set(tok_flat[order])
    row_w = jnp.zeros((n_rows,), x.dtype).at[dest].set(w_flat[order].astype(x.dtype))
    blk_expert = jnp.minimum(jnp.searchsorted(pends, jnp.arange(n_blk) * MOE_BLOCK, side='right'), N_EXPERTS - 1)
    x_pad = jnp.concatenate([xf, jnp.zeros((1, D), x.dtype)], axis=0)

    def expert_block(args):
        rows, wts, e = args
        xb = x_pad[rows]
        h = jax.nn.silu(xb @ w_gate[e]) * (xb @ w_up[e])
        return (h @ w_down[e]) * wts[:, None]

    yb = lax.map(expert_block, (row_tok.reshape(n_blk, MOE_BLOCK), row_w.reshape(n_blk, MOE_BLOCK), blk_expert))
    y = jnp.zeros((T + 1, D), x.dtype).at[row_tok].add(yb.reshape(n_rows, D))[:T]
    return y.reshape(B, S, D)


def setup_inputs(seed: int = 0) -> dict:
    key = jax.random.key(seed)
    ks = jax.random.split(key, 18)
    f32 = jnp.float32

    def nrm(k, shape, scale):
        return jax.random.normal(k, shape, f32) * scale

    return {
        'x': nrm(ks[0], (BATCH, SEQ, D_MODEL), 1.0),
        'w_in': nrm(ks[1], (DEPTH, D_MODEL, IN_WIDTH), D_MODEL ** -0.5),
        'w_gla_gate': nrm(ks[2], (DEPTH, GLA_GATE_RANK, GLA_QK_WIDTH), GLA_GATE_RANK ** -0.5),
        'b_gla_gate': nrm(ks[3], (DEPTH, GLA_QK_WIDTH), 0.1),
        'ret_norm_g': 1.0 + nrm(ks[4], (DEPTH, RET_WIDTH), 0.02),
        'gla_norm_g': 1.0 + nrm(ks[5], (DEPTH, GLA_V_WIDTH), 0.02),
        'w_out': nrm(ks[6], (DEPTH, MIX_WIDTH, D_MODEL), BETA * MIX_WIDTH ** -0.5),
        'ln1_g': 1.0 + nrm(ks[7], (DEPTH, D_MODEL), 0.02),
        'ln1_b': nrm(ks[8], (DEPTH, D_MODEL), 0.02),
        'w_router_group': nrm(ks[9], (DEPTH, D_MODEL, N_GROUPS), D_MODEL ** -0.5),
        'b_router_group': nrm(ks[10], (DEPTH, N_GROUPS), 0.01),
        'w_router_expert': nrm(ks[11], (DEPTH, D_MODEL, N_EXPERTS), D_MODEL ** -0.5),
        'b_router_expert': nrm(ks[12], (DEPTH, N_EXPERTS), 0.01),
        'w_expert_gate': nrm(ks[13], (DEPTH, N_EXPERTS, D_MODEL, D_FF_EXPERT), D_MODEL ** -0.5),
        'w_expert_up': nrm(ks[14], (DEPTH, N_EXPERTS, D_MODEL, D_FF_EXPERT), D_MODEL ** -0.5),
        'w_expert_down': nrm(ks[15], (DEPTH, N_EXPERTS, D_FF_EXPERT, D_MODEL), BETA * D_FF_EXPERT ** -0.5),
        'ln2_g': 1.0 + nrm(ks[16], (DEPTH, D_MODEL), 0.02),
        'ln2_b': nrm(ks[17], (DEPTH, D_MODEL), 0.02),
    }


def reference(x, w_in, w_gla_gate, b_gla_gate, ret_norm_g, gla_norm_g, w_out, ln1_g, ln1_b,
              w_router_group, b_router_group, w_router_expert, b_router_expert,
              w_expert_gate, w_expert_up, w_expert_down, ln2_g, ln2_b):
    S = x.shape[1]
    pos = jnp.arange(S)
    for l in range(DEPTH):
        proj = x @ w_in[l]
        rq, rk, rv, rg, dq, dk, dv, gq, gk, gv, gr, ga = jnp.split(proj, SPLIT_POINTS, axis=-1)
        q_r = rotate(heads(rq, RET_HEADS), pos)
        k_r = rotate(heads(rk, RET_HEADS), pos) * (RET_DK ** -0.5)
        ret = retention(q_r, k_r, heads(rv, RET_HEADS))
        ret = jax.nn.silu(rg) * (merge(head_norm(ret)) * ret_norm_g[l])
        dil = merge(dilated_attention(heads(dq, DIL_HEADS), heads(dk, DIL_HEADS), heads(dv, DIL_HEADS)))
        log_a = jax.nn.log_sigmoid((ga @ w_gla_gate[l] + b_gla_gate[l]).astype(jnp.float32)) / GLA_TAU
        g_o = gla(heads(gq, GLA_HEADS) * (GLA_DK ** -0.5), heads(gk, GLA_HEADS), heads(gv, GLA_HEADS), heads(log_a, GLA_HEADS))
        g_o = jax.nn.silu(gr) * (merge(head_norm(g_o)) * gla_norm_g[l])
        mixed = jnp.concatenate([ret, dil, g_o], axis=-1) @ w_out[l]
        x = layer_norm(ALPHA * x + mixed, ln1_g[l], ln1_b[l])
        moe = hier_moe(x, w_router_group[l], b_router_group[l], w_router_expert[l], b_router_expert[l],
                       w_expert_gate[l], w_expert_up[l], w_expert_down[l])
        x = layer_norm(ALPHA * x + moe, ln2_g[l], ln2_b[l])
    return x
```

```python
import numpy as np
from contextlib import ExitStack
import concourse.bass as bass
import concourse.mybir as mybir
from concourse.bass_utils import run_bass_kernel_spmd

F32 = mybir.dt.float32
BF16 = mybir.dt.bfloat16
I32 = mybir.dt.int32
AF = mybir.ActivationFunctionType
ALU = mybir.AluOpType
AX = mybir.AxisListType

ND = 6
SEQ = 4096
DM = 2048
NT = SEQ // 128
DEPTH = 4
INW = 6672
ALPHA = float((2 * DEPTH) ** 0.25)
EPS = 1e-5
BLK = 384
NBLK = -(-(2 * SEQ + 32 * (BLK - 1)) // BLK)
NROWS = NBLK * BLK
BIG = 30000.0


class Res:
    __slots__ = ("name", "w", "r")

    def __init__(self, name=""):
        self.name = name
        self.w = {}
        self.r = {}


class Sched:
    def __init__(self, nc, es, same_sync=True):
        self.nc = nc
        self.same_sync = same_sync
        self.e = dict(pe=nc.tensor, act=nc.scalar, dve=nc.vector, pool=nc.gpsimd, sp=nc.sync)
        self.sem = {k: es.enter_context(nc.semaphore("s_" + k)) for k in ("pe", "act", "dve", "pool")}
        self.cnt = {k: 0 for k in self.sem}
        self.pending = {k: False for k in self.sem}
        self.dsem = {q: [es.enter_context(nc.semaphore("d_%s%d" % (q, i))) for i in range(ND)]
                     for q in ("sp", "pool", "act")}
        self.dcnt = {q: [0] * ND for q in self.dsem}
        self.dnext = {q: 0 for q in self.dsem}
        self.known = {k: {} for k in self.e}
        self.n_ins = 0
        self.n_wait = 0

    def _semof(self, key):
        if key[0] == "c":
            return self.sem[key[1]], 1
        return self.dsem[key[1]][key[2]], 16

    def _wait(self, eng, key, val):
        if self.known[eng].get(key, 0) >= val:
            return
        sem, mult = self._semof(key)
        self.e[eng].wait_ge(sem, val * mult)
        self.known[eng][key] = val
        self.n_wait += 1

    def _deps(self, eng, reads, writes):
        deps = {}
        own = ("c", eng)
        for r in reads:
            for k, v in r.w.items():
                if deps.get(k, 0) < v:
                    deps[k] = v
        for w in writes:
            for d in (w.w, w.r):
                for k, v in d.items():
                    if k == own:
                        continue
                    if deps.get(k, 0) < v:
                        deps[k] = v
        for k, v in deps.items():
            if k == own and (eng == "pe" or not self.same_sync):
                continue
            self._wait(eng, k, v)

    def _mark(self, key, val, reads, writes):
        for r in reads:
            if r.r.get(key, 0) < val:
                r.r[key] = val
        for w in writes:
            w.w = {key: val}
            w.r = {}

    def op(self, eng, fn, reads=(), writes=(), signal=True):
        self._deps(eng, reads, writes)
        ins = fn()
        self.n_ins += 1
        if signal:
            self.cnt[eng] += 1
            ins.then_inc(self.sem[eng], 1)
            self.pending[eng] = False
            val = self.cnt[eng]
        else:
            self.pending[eng] = True
            val = self.cnt[eng] + 1
        self._mark(("c", eng), val, reads, writes)
        return ins

    def dma(self, q, fn, reads=(), writes=()):
        slot = self.dnext[q]
        self.dnext[q] = (slot + 1) % ND
        key = ("d", q, slot)
        if self.dcnt[q][slot] > 0:
            self._wait(q, key, self.dcnt[q][slot])
        self._deps(q, reads, writes)
        ins = fn()
        self.n_ins += 1
        self.dcnt[q][slot] += 1
        ins.then_inc(self.dsem[q][slot], 16)
        self._mark(key, self.dcnt[q][slot], reads, writes)
        return ins

    def barrier(self):
        for k in self.pending:
            assert not self.pending[k], "pending unsignaled instruction on " + k
        for eng in self.e.keys():
            for k in self.cnt:
                if self.cnt[k] > 0:
                    self._wait(eng, ("c", k), self.cnt[k])
            for q in self.dcnt:
                for i in range(ND):
                    if self.dcnt[q][i] > 0:
                        self._wait(eng, ("d", q, i), self.dcnt[q][i])


class Ring:
    def __init__(self, items):
        self.items = items
        self.i = 0

    def next(self):
        it = self.items[self.i]
        self.i = (self.i + 1) % len(self.items)
        return it


def host_consts():
    c = {}
    i = np.arange(128)
    c["ident"] = np.eye(128, dtype=np.float32)
    c["mask01"] = (i[:, None] <= i[None, :]).astype(np.float32)
    c["stri"] = (i[:, None] < i[None, :]).astype(np.float32)
    j = np.arange(256)
    band = (j[None, :] >= i[:, None]) & (j[None, :] <= i[:, None] + 128)
    c["dmask1"] = np.where(band, 0.0, -BIG).astype(np.float32)
    c["dmask0"] = np.where(band & (j[None, :] >= 128), 0.0, -BIG).astype(np.float32)
    half = 64
    inv = (np.float32(10000.0) ** (-np.arange(half, dtype=np.float32) / np.float32(half))).astype(np.float32)
    pos = np.arange(SEQ, dtype=np.float32)
    ang = (pos[:, None] * inv[None, :]).astype(np.float32)
    cos = np.cos(ang).astype(np.float32)
    sin = np.sin(ang).astype(np.float32)
    h = np.arange(4, dtype=np.float32)
    log_g = np.log1p(-np.exp2(-5.0 - h)).astype(np.float64)
    cc = (np.arange(SEQ) % 128).astype(np.float64)
    qd = np.exp((cc[:, None] + 1.0) * log_g[None, :])
    kd = np.exp(-(cc[:, None] + 1.0) * log_g[None, :]) * (128.0 ** -0.5)
    rope = np.zeros((4, SEQ, 4, 64), np.float32)
    rope[0] = cos[:, None, :] * qd[:, :, None]
    rope[1] = sin[:, None, :] * qd[:, :, None]
    rope[2] = cos[:, None, :] * kd[:, :, None]
    rope[3] = sin[:, None, :] * kd[:, :, None]
    c["rope"] = rope.reshape(4, SEQ, 256)
    c["g128"] = [float(np.exp(128.0 * lg)) for lg in log_g]
    return c


def build(n_layers=DEPTH, debug=(), stop_phase=99):
    nc = bass.Bass("TRN2", target_bir_lowering=False)
    hc = host_consts()
    g128 = hc["g128"]

    def din(name, shape, dt=F32):
        return nc.dram_tensor(name, list(shape), dt, kind="ExternalInput").ap()

    def dscr(name, shape, dt):
        return nc.dram_tensor(name, list(shape), dt, kind=("ExternalOutput" if name in debug else "Internal")).ap()

    x_in = din("x", [SEQ, DM])
    w_in = din("w_in", [DEPTH, DM, INW])
    w_gg = din("w_gla_gate", [DEPTH, 16, 384])
    b_gg = din("b_gla_gate", [DEPTH, 384])
    ret_g = din("ret_norm_g", [DEPTH, 512])
    gla_g = din("gla_norm_g", [DEPTH, 768])
    w_out = din("w_out", [DEPTH, DM, DM])
    ln1_g = din("ln1_g", [DEPTH, DM])
    ln1_b = din("ln1_b", [DEPTH, DM])
    w_rt = din("w_router", [DEPTH, DM, 36])
    b_rt = din("b_router", [DEPTH, 36])
    if stop_phase >= 7:
        w_eg = din("w_expert_gate", [DEPTH * 32 * 128 * 4, 2048])
        w_eu = din("w_expert_up", [DEPTH * 32 * 128 * 4, 2048])
        w_ed = din("w_expert_down", [DEPTH * 32 * 512, DM])
    ln2_g = din("ln2_g", [DEPTH, DM])
    ln2_b = din("ln2_b", [DEPTH, DM])
    c_ident = din("c_ident", [128, 128])
    c_mask01 = din("c_mask01", [128, 128])
    c_stri = din("c_stri", [128, 128])
    c_dmask0 = din("c_dmask0", [128, 256])
    c_dmask1 = din("c_dmask1", [128, 256])
    c_rope = din("c_rope", [4, SEQ, 256])
    y_out = nc.dram_tensor("y", [SEQ, DM], F32, kind="ExternalOutput").ap()

    P = dscr("P", [SEQ, 5120], BF16)
    PT = dscr("PT", [1536, SEQ], BF16)
    GAT = dscr("GAT", [16, SEQ], F32)
    DO = dscr("DO", [3, SEQ, 6 * 130], F32)
    MIX = dscr("MIX", [SEQ, DM], BF16)
    X1 = dscr("X1", [SEQ, DM], F32)
    X1B = dscr("X1B", [SEQ, DM], BF16)
    XG = dscr("XG", [NROWS, DM], BF16)
    YB = dscr("YB", [NROWS, DM], F32)
    XA = dscr("XA", [SEQ, DM], F32)
    XB_ = dscr("XBb", [SEQ, DM], F32)

    with ExitStack() as es0:
        S = Sched(nc, es0)
        E = S.e
        rr = {"i": 0}

        def alt(*engs):
            rr["i"] += 1
            return engs[rr["i"] % len(engs)]

        def ecopy(eng, out, in_):
            if eng == "act":
                return nc.scalar.copy(out=out, in_=in_)
            return E[eng].tensor_copy(out=out, in_=in_)

        def dma(q, out, in_, reads=(), writes=()):
            return S.dma(q, lambda: E[q].dma_start(out=out, in_=in_), reads, writes)

        uid = [0]

        def mk(es, name, shape, dt, n=1):
            items = []
            for k in range(n):
                uid[0] += 1
                t = es.enter_context(nc.sbuf_tensor("%s%d_%d" % (name, k, uid[0]), list(shape), dt))
                items.append((t, Res(name)))
            return items[0] if n == 1 else Ring(items)

        def mkp(es, name, shape, dt, n=1):
            items = []
            for k in range(n):
                uid[0] += 1
                t = es.enter_context(nc.psum_tensor("%s%d_%d" % (name, k, uid[0]), list(shape), dt))
                items.append((t, Res(name)))
            return items[0] if n == 1 else Ring(items)

        ident_f, r_identf = mk(es0, "identf", [128, 128], F32)
        ident_b, r_identb = mk(es0, "identb", [128, 128], BF16)
        mask01, r_mask01 = mk(es0, "mask01", [128, 128], F32)
        ones_b, r_onesb = mk(es0, "onesb", [128, 128], BF16)
        ones_f, r_onesf = mk(es0, "onesf", [128, 128], F32)
        dma("sp", ident_f[:], c_ident, writes=[r_identf])
        dma("sp", mask01[:], c_mask01, writes=[r_mask01])
        S.op("dve", lambda: nc.vector.tensor_copy(out=ident_b[:], in_=ident_f[:]), [r_identf], [r_identb])
        S.op("dve", lambda: nc.vector.memset(ones_b[:], 1.0), [], [r_onesb])
        S.op("dve", lambda: nc.vector.memset(ones_f[:], 1.0), [], [r_onesf])

        OH, r_OH = mk(es0, "OH", [128, NT, 2, 32], F32)
        A_b, r_Ab = mk(es0, "A_b", [128, NT, 32], BF16)
        GATE, r_GATE = mk(es0, "GATE", [128, NT, 2], F32)
        DESTI, r_DESTI = mk(es0, "DESTI", [128, NT, 2], I32)
        IDXG, r_IDXG = mk(es0, "IDXG", [128, NBLK, 4], I32)
        IDXD, r_IDXD = mk(es0, "IDXD", [128, NBLK, 4], I32)
        if stop_phase >= 6:
            with ExitStack() as es:
                zt, r_zt = mk(es, "zt", [128, DM], BF16)
                S.op("dve", lambda: nc.vector.memset(zt[:], 0.0), [], [r_zt])
                for i in range(NROWS // 128):
                    dma("sp", XG[i * 128:(i + 1) * 128, :], zt[:], reads=[r_zt])
                S.barrier()

        def rstd(out_ap, var_ap, r_mv):
            S.op("dve", lambda: nc.vector.tensor_scalar(out=out_ap, in0=var_ap, scalar1=EPS, scalar2=None, op0=ALU.add), [r_mv], [r_mv])
            S.op("act", lambda: nc.scalar.sqrt(out=out_ap, in_=out_ap), [r_mv], [r_mv])
            S.op("dve", lambda: nc.vector.reciprocal(out=out_ap, in_=out_ap), [r_mv], [r_mv])

        def layer_norm_tile(es_unused, u, r_u, gt, r_gt, bt, r_bt, st, r_st, mv, r_mv, outt, r_out):
            for c4 in range(4):
                S.op("dve", lambda c4=c4: nc.vector.bn_stats(out=st[:, c4, :], in_=u[:, c4 * 512:(c4 + 1) * 512]),
                     [r_u], [r_st])
            S.op("dve", lambda: nc.vector.bn_aggr(out=mv[:, 0:2], in_=st[:].rearrange("p a b -> p (a b)")), [r_st], [r_mv])
            rstd(mv[:, 2:3], mv[:, 1:2], r_mv)
            S.op("dve", lambda: nc.vector.tensor_scalar(out=outt[:], in0=u[:], scalar1=mv[:, 0:1], scalar2=mv[:, 2:3],
                                                        op0=ALU.subtract, op1=ALU.mult), [r_u, r_mv], [r_out])
            S.op("pool", lambda: nc.gpsimd.tensor_tensor(out=outt[:], in0=outt[:], in1=gt[:], op=ALU.mult),
                 [r_out, r_gt], [r_out])
            S.op("pool", lambda: nc.gpsimd.tensor_tensor(out=outt[:], in0=outt[:], in1=bt[:], op=ALU.add),
                 [r_out, r_bt], [r_out])

        def head_norm(o_ap, nh, st, r_st, mv, r_mv, outt, r_out, r_o, col0):
            for h in range(nh):
                S.op("dve", lambda h=h: nc.vector.bn_stats(out=st[:, h, :], in_=o_ap(h)), [r_o], [r_st])
                S.op("dve", lambda h=h: nc.vector.bn_aggr(out=mv[:, h, 0:2], in_=st[:, h, :]), [r_st], [r_mv])
            rstd(mv[:, 0:nh, 2:3], mv[:, 0:nh, 1:2], r_mv)
            for h in range(nh):
                S.op("dve", lambda h=h: nc.vector.tensor_scalar(
                    out=outt[:, col0 + h * 128: col0 + (h + 1) * 128], in0=o_ap(h), scalar1=mv[:, h, 0:1],
                    scalar2=mv[:, h, 2:3], op0=ALU.subtract, op1=ALU.mult), [r_o, r_mv], [r_out])

        XCUR = x_in
        for l in range(n_layers):
            XNEXT = y_out if l == n_layers - 1 else (XA if l % 2 == 0 else XB_)
            with ExitStack() as es:
                xT, r_xT = mk(es, "xT", [128, 16, SEQ], BF16)
                xb = mk(es, "xb", [128, DM], BF16, 2)
                wt = mk(es, "wt", [128, 16, 512], BF16, 2)
                stg = mk(es, "stg", [128, 4, 512], BF16, 2)
                stgf, r_stgf = mk(es, "stgf", [16, 512], F32)
                tp = mkp(es, "tp", [128, 512], BF16, 2)
                ps = mkp(es, "ps", [128, 512], F32, 4)
                for j in range(NT):
                    xbt, r_xb = xb.next()
                    dma("pool", xbt[:], XCUR[j * 128:(j + 1) * 128, :], writes=[r_xb])
                    for g4 in range(4):
                        tpt, r_tp = tp.next()
                        for k in range(4):
                            kc = g4 * 4 + k
                            S.op("pe", lambda kc=kc, k=k, tpt=tpt, xbt=xbt: nc.tensor.transpose(
                                out=tpt[:, k * 128:(k + 1) * 128], in_=xbt[:, kc * 128:(kc + 1) * 128], identity=ident_b[:]),
                                [r_xb, r_identb], [r_tp], signal=(k == 3))
                        eng = alt("act", "dve")
                        S.op(eng, lambda eng=eng, tpt=tpt, g4=g4, j=j: ecopy(
                            eng, xT[:, g4 * 4:(g4 + 1) * 4, j * 128:(j + 1) * 128],
                            tpt[:].rearrange("p (k t) -> p k t", k=4)), [r_tp], [r_xT])
                if stop_phase >= 1:
                    tm_tiles = [(c0, c0) for c0 in range(0, 2048, 512)] + [(c0, c0 - 1536) for c0 in range(3584, 6656, 512)]
                    for (wc, pc) in tm_tiles:
                        wtt, r_wt = wt.next()
                        dma("pool", wtt[:], w_in[l, :, wc:wc + 512].rearrange("(k p) n -> p k n", p=128), writes=[r_wt])
                        for j4 in range(NT // 4):
                            sg, r_sg = stg.next()
                            for jj in range(4):
                                j = j4 * 4 + jj
                                pst, r_ps = ps.next()
                                for kc in range(16):
                                    S.op("pe", lambda kc=kc, pst=pst, j=j, wtt=wtt: nc.tensor.matmul(
                                        pst[:], lhsT=xT[:, kc, j * 128:(j + 1) * 128], rhs=wtt[:, kc, :],
                                        start=(kc == 0), stop=(kc == 15)), [r_xT, r_wt], [r_ps], signal=(kc == 15))
                                eng = alt("act", "dve")
                                S.op(eng, lambda eng=eng, sg=sg, jj=jj, pst=pst: ecopy(eng, sg[:, jj, :], pst[:]), [r_ps], [r_sg])
                            dma("sp", P[j4 * 512:(j4 + 1) * 512, pc:pc + 512].rearrange("(j p) n -> p j n", p=128), sg[:],
                                reads=[r_sg])
                    for g in range(3):
                        wtt, r_wt = wt.next()
                        wc = 2048 + g * 512
                        dma("pool", wtt[:], w_in[l, :, wc:wc + 512].rearrange("(k p) n -> p k n", p=128), writes=[r_wt])
                        for cc in range(4):
                            row0 = g * 512 + cc * 128
                            for s4 in range(2):
                                sg, r_sg = stg.next()
                                for ss in range(4):
                                    s = s4 * 4 + ss
                                    pst, r_ps = ps.next()
                                    for kc in range(16):
                                        S.op("pe", lambda kc=kc, pst=pst, s=s, wtt=wtt, cc=cc: nc.tensor.matmul(
                                            pst[:], lhsT=wtt[:, kc, cc * 128:(cc + 1) * 128], rhs=xT[:, kc, s * 512:(s + 1) * 512],
                                            start=(kc == 0), stop=(kc == 15)), [r_xT, r_wt], [r_ps], signal=(kc == 15))
                                    eng = alt("act", "dve")
                                    if row0 < 768:
                                        if eng == "act":
                                            S.op(eng, lambda sg=sg, ss=ss, pst=pst: nc.scalar.activation(out=sg[:, ss, :], in_=pst[:], func=AF.Copy, scale=float(128 ** -0.5)), [r_ps], [r_sg])
                                        else:
                                            S.op(eng, lambda sg=sg, ss=ss, pst=pst: nc.vector.tensor_scalar(out=sg[:, ss, :], in0=pst[:], scalar1=float(128 ** -0.5), scalar2=None, op0=ALU.mult), [r_ps], [r_sg])
                                    else:
                                        S.op(eng, lambda eng=eng, sg=sg, ss=ss, pst=pst: ecopy(eng, sg[:, ss, :], pst[:]), [r_ps], [r_sg])
                                dma("sp", PT[row0:row0 + 128, s4 * 2048:(s4 + 1) * 2048], sg[:].rearrange("p a b -> p (a b)"),
                                    reads=[r_sg])
                    wtt, r_wt = wt.next()
                    dma("pool", wtt[:, :, 0:16], w_in[l, :, 6656:6672].rearrange("(k p) n -> p k n", p=128), writes=[r_wt])
                    for s in range(8):
                        pst, r_ps = ps.next()
                        for kc in range(16):
                            S.op("pe", lambda kc=kc, pst=pst, s=s, wtt=wtt: nc.tensor.matmul(
                                pst[0:16, :], lhsT=wtt[:, kc, 0:16], rhs=xT[:, kc, s * 512:(s + 1) * 512],
                                start=(kc == 0), stop=(kc == 15)), [r_xT, r_wt], [r_ps], signal=(kc == 15))
                        S.op("act", lambda pst=pst: nc.scalar.copy(out=stgf[:], in_=pst[0:16, :]), [r_ps], [r_stgf])
                        dma("sp", GAT[:, s * 512:(s + 1) * 512], stgf[:], reads=[r_stgf])
                S.barrier()
            if stop_phase <= 1:
                break

            with ExitStack() as es:
                rope = mk(es, "rope", [128, 4, 256], F32, 2)
                qk = mk(es, "qk", [128, 1024], BF16, 2)
                rv = mk(es, "rv", [128, 512], BF16, 2)
                rg = mk(es, "rg", [128, 512], BF16, 2)
                qkr_ring = mk(es, "qkr", [128, 1024], BF16, 2)
                t1_ring = mk(es, "t1", [128, 4, 64], F32, 2)
                t2_ring = mk(es, "t2", [128, 4, 64], F32, 2)
                t3_ring = mk(es, "t3", [128, 4, 64], F32, 2)
                t4_ring = mk(es, "t4", [128, 4, 64], F32, 2)
                qkT_ring = mk(es, "qkT", [128, 8, 128], BF16, 2)
                sm = mk(es, "sm", [128, 128], BF16, 2)
                Sf = [mk(es, "Sf%d" % h, [128, 128], F32) for h in range(4)]
                Sb = [mk(es, "Sb%d" % h, [128, 128], BF16) for h in range(4)]
                Tt, r_Tt = mk(es, "Tt", [128, 128], F32)
                st_ring = mk(es, "st", [128, 4, 6], F32, 2)
                mv_ring = mk(es, "mv", [128, 4, 3], F32, 2)
                nrm_ring = mk(es, "nrm", [128, 512], F32, 2)
                sil_ring = mk(es, "sil", [128, 512], F32, 2)
                mixo = mk(es, "mixo", [128, 512], BF16, 2)
                gvec, r_gvec = mk(es, "gvec", [128, 512], F32)
                tp_ring = mkp(es, "tp2", [128, 8, 128], BF16, 2)
                sT = mkp(es, "sT", [128, 128], F32, 2)
                po_ring = mkp(es, "po", [128, 512], F32, 2)
                kv = mkp(es, "kv", [128, 128], F32, 2)
                dma("sp", gvec[:], ret_g[l:l + 1, :].partition_broadcast(128), writes=[r_gvec])
                for h in range(4):
                    S.op("dve", lambda h=h: nc.vector.memset(Sf[h][0][:], 0.0), [], [Sf[h][1]])
                    S.op("dve", lambda h=h: nc.vector.memset(Sb[h][0][:], 0.0), [], [Sb[h][1]])
                for j in range(NT):
                    rows = slice(j * 128, (j + 1) * 128)
                    qkr, r_qkr = qkr_ring.next()
                    t1, r_t1 = t1_ring.next()
                    t2, r_t2 = t2_ring.next()
                    t3, r_t3 = t3_ring.next()
                    t4, r_t4 = t4_ring.next()
                    qkT, r_qkT = qkT_ring.next()
                    st, r_st = st_ring.next()
                    mv, r_mv = mv_ring.next()
                    nrm, r_nrm = nrm_ring.next()
                    sil, r_sil = sil_ring.next()
                    tp, r_tp = tp_ring.next()
                    po, r_po = po_ring.next()
                    ropt, r_rop = rope.next()
                    dma("sp", ropt[:], c_rope[:, rows, :].rearrange("a p n -> p a n"), writes=[r_rop])
                    qkt, r_qk = qk.next()
                    dma("sp", qkt[:], P[rows, 0:1024], writes=[r_qk])
                    rvt, r_rv = rv.next()
                    dma("sp", rvt[:], P[rows, 1024:1536], writes=[r_rv])
                    rgt, r_rg = rg.next()
                    dma("sp", rgt[:], P[rows, 1536:2048], writes=[r_rg])
                    for qi in range(2):
                        src = qkt[:, qi * 512:(qi + 1) * 512].rearrange("p (h d) -> p h d", h=4)
                        dst = qkr[:, qi * 512:(qi + 1) * 512].rearrange("p (h d) -> p h d", h=4)
                        a1, a2 = src[:, :, 0:64], src[:, :, 64:128]
                        cosv = ropt[:, 2 * qi, :].rearrange("p (h d) -> p h d", h=4)
                        sinv = ropt[:, 2 * qi + 1, :].rearrange("p (h d) -> p h d", h=4)
                        e1 = "dve" if qi == 0 else "pool"
                        S.op(e1, lambda e1=e1, a1=a1, cosv=cosv: E[e1].tensor_tensor(out=t1[:], in0=a1, in1=cosv, op=ALU.mult), [r_qk, r_rop], [r_t1])
                        S.op(e1, lambda e1=e1, a2=a2, sinv=sinv: E[e1].tensor_tensor(out=t2[:], in0=a2, in1=sinv, op=ALU.mult), [r_qk, r_rop], [r_t2])
                        S.op(e1, lambda e1=e1, dst=dst: E[e1].tensor_tensor(out=dst[:, :, 0:64], in0=t1[:], in1=t2[:], op=ALU.subtract), [r_t1, r_t2], [r_qkr])
                        S.op(e1, lambda e1=e1, a1=a1, sinv=sinv: E[e1].tensor_tensor(out=t3[:], in0=a1, in1=sinv, op=ALU.mult), [r_qk, r_rop], [r_t3])
                        S.op(e1, lambda e1=e1, a2=a2, cosv=cosv: E[e1].tensor_tensor(out=t4[:], in0=a2, in1=cosv, op=ALU.mult), [r_qk, r_rop], [r_t4])
                        S.op(e1, lambda e1=e1, dst=dst: E[e1].tensor_tensor(out=dst[:, :, 64:128], in0=t3[:], in1=t4[:], op=ALU.add), [r_t3, r_t4], [r_qkr])
                    for k in range(8):
                        S.op("pe", lambda k=k: nc.tensor.transpose(out=tp[:, k, :], in_=qkr[:, k * 128:(k + 1) * 128], identity=ident_b[:]),
                             [r_qkr, r_identb], [r_tp], signal=(k == 7))
                    S.op("act", lambda: nc.scalar.copy(out=qkT[:], in_=tp[:]), [r_tp], [r_qkT])
                    S.op("act", lambda rgt=rgt: nc.scalar.activation(out=sil[:], in_=rgt[:], func=AF.Silu), [r_rg], [r_sil])
                    for h in range(4):
                        sTt, r_sT = sT.next()
                        S.op("pe", lambda h=h, sTt=sTt: nc.tensor.matmul(sTt[:], lhsT=qkT[:, 4 + h, :], rhs=qkT[:, h, :], start=True, stop=True),
                             [r_qkT], [r_sT])
                        smt, r_sm = sm.next()
                        S.op("dve", lambda sTt=sTt, smt=smt: nc.vector.tensor_tensor(out=smt[:], in0=sTt[:], in1=mask01[:], op=ALU.mult),
                             [r_sT, r_mask01], [r_sm])
                        S.op("pe", lambda h=h, smt=smt, rvt=rvt: nc.tensor.matmul(po[:, h * 128:(h + 1) * 128], lhsT=smt[:], rhs=rvt[:, h * 128:(h + 1) * 128],
                                                                         start=True, stop=False), [r_sm, r_rv], [r_po], signal=False)
                        S.op("pe", lambda h=h: nc.tensor.matmul(po[:, h * 128:(h + 1) * 128], lhsT=qkT[:, h, :], rhs=Sb[h][0][:],
                                                                start=False, stop=True), [r_qkT, Sb[h][1]], [r_po])
                        kvt, r_kv = kv.next()
                        S.op("pe", lambda h=h, kvt=kvt, rvt=rvt: nc.tensor.matmul(kvt[:], lhsT=qkr[:, 512 + h * 128:512 + (h + 1) * 128], rhs=rvt[:, h * 128:(h + 1) * 128],
                                                                         start=True, stop=True), [r_qkr, r_rv], [r_kv])
                        S.op("dve", lambda h=h, kvt=kvt: nc.vector.tensor_tensor(out=Tt[:], in0=Sf[h][0][:], in1=kvt[:], op=ALU.add),
                             [Sf[h][1], r_kv], [r_Tt])
                        S.op("act", lambda h=h: nc.scalar.activation(out=Sf[h][0][:], in_=Tt[:], func=AF.Copy, scale=g128[h]), [r_Tt], [Sf[h][1]])
                        S.op("act", lambda h=h: nc.scalar.activation(out=Sb[h][0][:], in_=Tt[:], func=AF.Copy, scale=g128[h]), [r_Tt], [Sb[h][1]])
                    head_norm(lambda h: po[:, h * 128:(h + 1) * 128], 4, st, r_st, mv, r_mv, nrm, r_nrm, r_po, 0)
                    S.op("pool", lambda: nc.gpsimd.tensor_tensor(out=nrm[:], in0=nrm[:], in1=gvec[:], op=ALU.mult), [r_nrm, r_gvec], [r_nrm])
                    mo, r_mo = mixo.next()
                    S.op("pool", lambda mo=mo: nc.gpsimd.tensor_tensor(out=mo[:], in0=nrm[:], in1=sil[:], op=ALU.mult), [r_nrm, r_sil], [r_mo])
                    dma("sp", MIX[rows, 0:512], mo[:], reads=[r_mo])
                S.barrier()
            if stop_phase <= 2:
                break

            with ExitStack() as es:
                wg, r_wg = mk(es, "wgg", [16, 384], F32)
                bg, r_bg = mk(es, "bgg", [1, 384], F32)
                gvec, r_gvec = mk(es, "gvec3", [128, 768], F32)
                gat = mk(es, "gat", [16, 128], F32, 2)
                gqk = mk(es, "gqk", [128, 768], BF16, 2)
                gv = mk(es, "gv", [128, 768], BF16, 2)
                gr = mk(es, "gr", [128, 768], BF16, 2)
                ez_ring = mk(es, "ez", [128, 384], F32, 2)
                lz_ring = mk(es, "lz", [128, 384], F32, 2)
                eb_ring = mk(es, "eb", [128, 384], F32, 2)
                enb_ring = mk(es, "enb", [128, 384], F32, 2)
                qkh_ring = mk(es, "qkh", [128, 768], BF16, 2)
                qkT_ring = mk(es, "qkT3", [128, 6, 128], BF16, 2)
                dec_ring = mk(es, "dec", [128, 4], F32, 2)
                sm = mk(es, "sm3", [128, 128], BF16, 2)
                Sf = [mk(es, "Sg%d" % p_, [128, 256], F32) for p_ in range(3)]
                Sb = [mk(es, "Sgb%d" % p_, [128, 256], BF16) for p_ in range(3)]
                Tt, r_Tt = mk(es, "Tt3", [128, 256], F32)
                st_ring = mk(es, "st3", [128, 6, 6], F32, 2)
                mv_ring = mk(es, "mv3", [128, 6, 3], F32, 2)
                nrm_ring = mk(es, "nrm3", [128, 768], F32, 2)
                sil_ring = mk(es, "sil3", [128, 768], F32, 2)
                mixo = mk(es, "mixo3", [128, 768], BF16, 2)
                pz, r_pz = mkp(es, "pz", [128, 512], F32)
                pl, r_pl = mkp(es, "pl", [128, 512], F32)
                tp, r_tp = mkp(es, "tp3", [128, 8, 128], BF16)
                pm, r_pm = mkp(es, "pm3", [128, 512], F32)
                poA, r_poA = mkp(es, "poA", [128, 512], F32)
                poB, r_poB = mkp(es, "poB", [128, 512], F32)
                pkv, r_pkv = mkp(es, "pkv", [128, 512], F32)
                r_sT = [Res(), Res()]
                r_bl = Res()
                r_kvh = [Res(), Res()]
                dma("sp", wg[:], w_gg[l], writes=[r_wg])
                dma("sp", bg[:], b_gg[l:l + 1, :], writes=[r_bg])
                dma("sp", gvec[:], gla_g[l:l + 1, :].partition_broadcast(128), writes=[r_gvec])
                for p_ in range(3):
                    S.op("dve", lambda p_=p_: nc.vector.memset(Sf[p_][0][:], 0.0), [], [Sf[p_][1]])
                    S.op("dve", lambda p_=p_: nc.vector.memset(Sb[p_][0][:], 0.0), [], [Sb[p_][1]])

                def po_ap(h):
                    return poA[:, h * 128:(h + 1) * 128] if h < 4 else poB[:, (h - 4) * 128:(h - 3) * 128]

                def r_poh(h):
                    return r_poA if h < 4 else r_poB
                for j in range(NT):
                    rows = slice(j * 128, (j + 1) * 128)
                    ez, r_ez = ez_ring.next()
                    lz, r_lz = lz_ring.next()
                    eb, r_eb = eb_ring.next()
                    enb, r_enb = enb_ring.next()
                    qkh, r_qkh = qkh_ring.next()
                    qkT, r_qkT = qkT_ring.next()
                    dec, r_dec = dec_ring.next()
                    st, r_st = st_ring.next()
                    mv, r_mv = mv_ring.next()
                    nrm, r_nrm = nrm_ring.next()
                    sil, r_sil = sil_ring.next()
                    gatt, r_gat = gat.next()
                    dma("sp", gatt[:], GAT[:, rows], writes=[r_gat])
                    gqkt, r_gqk = gqk.next()
                    dma("sp", gqkt[:], P[rows, 2816:3584], writes=[r_gqk])
                    gvt, r_gv = gv.next()
                    dma("sp", gvt[:], P[rows, 3584:4352], writes=[r_gv])
                    grt, r_gr = gr.next()
                    dma("sp", grt[:], P[rows, 4352:5120], writes=[r_gr])
                    S.op("pe", lambda gatt=gatt: nc.tensor.matmul(pz[:, 0:384], lhsT=gatt[:], rhs=wg[:], start=True, stop=False),
                         [r_gat, r_wg], [r_pz], signal=False)
                    S.op("pe", lambda: nc.tensor.matmul(pz[:, 0:384], lhsT=ones_f[0:1, :], rhs=bg[:], start=False, stop=True),
                         [r_onesf, r_bg], [r_pz])
                    S.op("act", lambda: nc.scalar.activation(out=ez[:], in_=pz[:, 0:384], func=AF.Exp, scale=-1.0), [r_pz], [r_ez])
                    S.op("act", lambda: nc.scalar.activation(out=lz[:], in_=ez[:], func=AF.Ln, bias=1.0, scale=1.0), [r_ez], [r_lz])
                    S.op("pe", lambda: nc.tensor.matmul(pl[:, 0:384], lhsT=mask01[:], rhs=lz[:], start=True, stop=True),
                         [r_mask01, r_lz], [r_pl])
                    for p_ in range(3):
                        S.op("pe", lambda p_=p_: nc.tensor.matmul(pm[:, 256 + p_:257 + p_], lhsT=lz[:, p_ * 128:(p_ + 1) * 128], rhs=ones_f[:, 0:1],
                                                                  start=True, stop=True), [r_lz, r_onesf], [r_bl], signal=(p_ == 2))
                    S.op("act", lambda: nc.scalar.activation(out=eb[:], in_=pl[:, 0:384], func=AF.Exp, scale=-1.0 / 16.0), [r_pl], [r_eb])
                    S.op("act", lambda: nc.scalar.activation(out=enb[:], in_=pl[:, 0:384], func=AF.Exp, scale=1.0 / 16.0), [r_pl], [r_enb])
                    S.op("act", lambda: nc.scalar.activation(out=dec[:, 0:3], in_=pm[:, 256:259], func=AF.Exp, scale=-1.0 / 16.0), [r_bl], [r_dec])
                    S.op("dve", lambda gqkt=gqkt: nc.vector.scalar_tensor_tensor(out=qkh[:, 0:384], in0=gqkt[:, 0:384], scalar=0.125, in1=eb[:],
                                                                                op0=ALU.mult, op1=ALU.mult), [r_gqk, r_eb], [r_qkh])
                    S.op("dve", lambda gqkt=gqkt: nc.vector.tensor_tensor(out=qkh[:, 384:768], in0=gqkt[:, 384:768], in1=enb[:], op=ALU.mult),
                         [r_gqk, r_enb], [r_qkh])
                    for k in range(6):
                        S.op("pe", lambda k=k: nc.tensor.transpose(out=tp[:, k, :], in_=qkh[:, k * 128:(k + 1) * 128], identity=ident_b[:]),
                             [r_qkh, r_identb], [r_tp], signal=(k == 5))
                    S.op("act", lambda: nc.scalar.copy(out=qkT[:], in_=tp[:, 0:6, :]), [r_tp], [r_qkT])
                    S.op("act", lambda grt=grt: nc.scalar.activation(out=sil[:], in_=grt[:], func=AF.Silu), [r_gr], [r_sil])
                    for h in range(6):
                        p_, hh = h // 2, h % 2
                        R = slice(hh * 64, (hh + 1) * 64)
                        sTa = pm[:, (h % 2) * 128:(h % 2 + 1) * 128]
                        rs = r_sT[h % 2]
                        S.op("pe", lambda p_=p_, R=R, sTa=sTa: nc.tensor.matmul(sTa, lhsT=qkT[R, 3 + p_, :], rhs=qkT[R, p_, :], start=True, stop=True),
                             [r_qkT], [rs])
                        smt, r_sm = sm.next()
                        S.op("dve", lambda sTa=sTa, smt=smt: nc.vector.tensor_tensor(out=smt[:], in0=sTa, in1=mask01[:], op=ALU.mult),
                             [rs, r_mask01], [r_sm])
                        S.op("pe", lambda h=h, smt=smt, gvt=gvt: nc.tensor.matmul(po_ap(h), lhsT=smt[:], rhs=gvt[:, h * 128:(h + 1) * 128],
                                                                         start=True, stop=False), [r_sm, r_gv], [r_poh(h)], signal=False)
                        S.op("pe", lambda h=h, p_=p_, R=R, hh=hh: nc.tensor.matmul(po_ap(h), lhsT=qkT[R, p_, :], rhs=Sb[p_][0][R, hh * 128:(hh + 1) * 128],
                                                                          start=False, stop=True), [r_qkT, Sb[p_][1]], [r_poh(h)])
                    for p_ in range(3):
                        kva = pkv[:, (p_ % 2) * 256:(p_ % 2 + 1) * 256]
                        rk = r_kvh[p_ % 2]
                        S.op("pe", lambda p_=p_, kva=kva, gvt=gvt: nc.tensor.matmul(kva, lhsT=qkh[:, 384 + p_ * 128:384 + (p_ + 1) * 128], rhs=gvt[:, p_ * 256:(p_ + 1) * 256],
                                                                          start=True, stop=True), [r_qkh, r_gv], [rk])
                        S.op("dve", lambda p_=p_, kva=kva: nc.vector.tensor_tensor(out=Tt[:], in0=Sf[p_][0][:], in1=kva, op=ALU.add),
                             [Sf[p_][1], rk], [r_Tt])
                        S.op("act", lambda p_=p_: nc.scalar.activation(out=Sf[p_][0][:], in_=Tt[:], func=AF.Copy, scale=dec[:, p_:p_ + 1]), [r_Tt, r_dec], [Sf[p_][1]])
                        S.op("act", lambda p_=p_: nc.scalar.activation(out=Sb[p_][0][:], in_=Tt[:], func=AF.Copy, scale=dec[:, p_:p_ + 1]), [r_Tt, r_dec], [Sb[p_][1]])
                    r_pob = Res()
                    for h in range(6):
                        S.op("dve", lambda h=h: nc.vector.bn_stats(out=st[:, h, :], in_=po_ap(h)), [r_poh(h)], [r_st])
                        S.op("dve", lambda h=h: nc.vector.bn_aggr(out=mv[:, h, 0:2], in_=st[:, h, :]), [r_st], [r_mv])
                    rstd(mv[:, :, 2:3], mv[:, :, 1:2], r_mv)
                    for h in range(6):
                        S.op("dve", lambda h=h: nc.vector.tensor_scalar(out=nrm[:, h * 128:(h + 1) * 128], in0=po_ap(h), scalar1=mv[:, h, 0:1],
                                                                        scalar2=mv[:, h, 2:3], op0=ALU.subtract, op1=ALU.mult),
                             [r_poh(h), r_mv], [r_nrm])
                    S.op("pool", lambda: nc.gpsimd.tensor_tensor(out=nrm[:], in0=nrm[:], in1=gvec[:], op=ALU.mult), [r_nrm, r_gvec], [r_nrm])
                    mo, r_mo = mixo.next()
                    S.op("pool", lambda mo=mo: nc.gpsimd.tensor_tensor(out=mo[:], in0=nrm[:], in1=sil[:], op=ALU.mult), [r_nrm, r_sil], [r_mo])
                    dma("sp", MIX[rows, 1280:2048], mo[:], reads=[r_mo])
                S.barrier()
            if stop_phase <= 3:
                break

            with ExitStack() as es:
                QT, r_QT = mk(es, "QT", [128, 6, SEQ], BF16)
                KT, r_KT = mk(es, "KT", [128, 6, SEQ], BF16)
                dm0, r_dm0 = mk(es, "dm0", [128, 256], F32)
                dm1, r_dm1 = mk(es, "dm1", [128, 256], F32)
                mb0, r_mb0 = mk(es, "mb0", [128, 256], BF16)
                mb1, r_mb1 = mk(es, "mb1", [128, 256], BF16)
                V = mk(es, "V", [128, 768], BF16, 5)
                negm = mk(es, "negm", [128, 3], F32, 3)
                pexp = mk(es, "pexp", [128, 3, 256], BF16, 3)
                pTs = mk(es, "pTs", [128, 3, 256], BF16, 3)
                stage = Ring([(es.enter_context(nc.sbuf_tensor("dstage%d_%d" % (k, l), [128, 6, 130], F32)), [Res() for _ in range(2)]) for k in range(4)])
                ps_s = mkp(es, "ps_s", [128, 4, 256], F32, 2)
                ps_t = mkp(es, "ps_t", [128, 4, 256], BF16, 2)
                ps_o = mkp(es, "ps_o", [128, 4, 128], F32, 2)
                dma("sp", dm0[:], c_dmask0, writes=[r_dm0])
                dma("sp", dm1[:], c_dmask1, writes=[r_dm1])
                S.op("dve", lambda: nc.vector.tensor_copy(out=mb0[:], in_=dm0[:]), [r_dm0], [r_mb0])
                S.op("dve", lambda: nc.vector.tensor_copy(out=mb1[:], in_=dm1[:]), [r_dm1], [r_mb1])
                for h in range(6):
                    dma("sp", QT[:, h, :], PT[h * 128:(h + 1) * 128, :], writes=[r_QT])
                    dma("sp", KT[:, h, :], PT[768 + h * 128:768 + (h + 1) * 128, :], writes=[r_KT])
                batches = []
                for pi, dil in enumerate((1, 4, 16)):
                    nb = SEQ // (dil * 128)
                    for r in range(dil):
                        for n in range(nb):
                            for hb in range(2):
                                batches.append((pi, dil, r, n, hb))
                ctxs = {}
                ust = {}

                def sA(b):
                    pi, dil, r, n, hb = batches[b]
                    c = ctxs[b] = {}
                    row0 = n * 128 * dil + r
                    rsl = slice(row0, row0 + 127 * dil + 1, dil)
                    c["rsl"] = rsl
                    if hb == 0:
                        vt, r_v = V.next()
                        dma("sp", vt[:], P[rsl, 2048:2816], writes=[r_v])
                        vprev = ust.get("vprev") if n > 0 else (vt, r_v)
                        stg, r_stg = stage.next()
                        ust["cur"] = (vt, r_v, vprev, stg, r_stg)
                        ust["vprev"] = (vt, r_v)
                    c["u"] = ust["cur"]
                    h0 = hb * 3
                    pst, r_ps = ps_s.next()
                    c["pst"] = (pst, r_ps)
                    for hh in range(3):
                        h = h0 + hh
                        q_ap = QT[:, h, rsl]
                        if n == 0:
                            S.op("pe", lambda: nc.tensor.matmul(pst[:, hh, 128:256], lhsT=q_ap, rhs=KT[:, h, rsl], start=True, stop=False),
                                 [r_QT, r_KT], [r_ps], signal=False)
                            S.op("pe", lambda: nc.tensor.matmul(pst[:, hh, :], lhsT=ident_b[:], rhs=mb0[:], start=False, stop=True),
                                 [r_identb, r_mb0], [r_ps], signal=(hh == 2))
                        else:
                            ksl = slice(row0 - 128 * dil, row0 + 127 * dil + 1, dil)
                            S.op("pe", lambda: nc.tensor.matmul(pst[:, hh, :], lhsT=q_ap, rhs=KT[:, h, ksl], start=True, stop=False),
                                 [r_QT, r_KT], [r_ps], signal=False)
                            S.op("pe", lambda: nc.tensor.matmul(pst[:, hh, :], lhsT=ident_b[:], rhs=mb1[:], start=False, stop=True),
                                 [r_identb, r_mb1], [r_ps], signal=(hh == 2))

                def sB(b):
                    pi, dil, r, n, hb = batches[b]
                    c = ctxs[b]
                    h0 = hb * 3
                    pst, r_ps = c["pst"]
                    vt, r_v, vprev, stg, r_stg = c["u"]
                    S.op("dve", lambda: nc.vector.reduce_max(out=stg[:, h0:h0 + 3, 128], in_=pst[:, 0:3, :], axis=AX.X), [r_ps], [r_stg[hb]])
                    ngt, r_ng = negm.next()
                    S.op("dve", lambda: nc.vector.tensor_scalar(out=ngt[:], in0=stg[:, h0:h0 + 3, 128], scalar1=-1.0, scalar2=None, op0=ALU.mult),
                         [r_stg[hb]], [r_ng])
                    pet, r_pe = pexp.next()
                    c["pet"] = (pet, r_pe)
                    for hh in range(3):
                        S.op("act", lambda: nc.scalar.activation(out=pet[:, hh, :], in_=pst[:, hh, :], func=AF.Exp, bias=ngt[:, hh:hh + 1], scale=1.0,
                                                                 accum_out=stg[:, h0 + hh, 129:130]),
                             [r_ps, r_ng], [r_pe, r_stg[hb]])

                def sC(b):
                    c = ctxs[b]
                    pet, r_pe = c["pet"]
                    ptt, r_pt = ps_t.next()
                    for hh in range(3):
                        for kk in range(2):
                            S.op("pe", lambda: nc.tensor.transpose(out=ptt[:, hh, kk * 128:(kk + 1) * 128], in_=pet[:, hh, kk * 128:(kk + 1) * 128], identity=ident_b[:]),
                                 [r_pe, r_identb], [r_pt], signal=(hh == 2 and kk == 1))
                    pts, r_pts = pTs.next()
                    c["pts"] = (pts, r_pts)
                    S.op("dve", lambda: nc.vector.tensor_copy(out=pts[:], in_=ptt[:, 0:3, :]), [r_pt], [r_pts])

                def sD(b):
                    pi, dil, r, n, hb = batches[b]
                    c = ctxs.pop(b)
                    h0 = hb * 3
                    pts, r_pts = c["pts"]
                    vt, r_v, vprev, stg, r_stg = c["u"]
                    pot, r_po2 = ps_o.next()
                    for hh in range(3):
                        h = h0 + hh
                        S.op("pe", lambda: nc.tensor.matmul(pot[:, hh, :], lhsT=pts[:, hh, 0:128], rhs=vprev[0][:, h * 128:(h + 1) * 128], start=True, stop=False),
                             [r_pts, vprev[1]], [r_po2], signal=False)
                        S.op("pe", lambda: nc.tensor.matmul(pot[:, hh, :], lhsT=pts[:, hh, 128:256], rhs=vt[:, h * 128:(h + 1) * 128], start=False, stop=True),
                             [r_pts, r_v], [r_po2], signal=(hh == 2))
                    S.op("act", lambda: nc.scalar.copy(out=stg[:, h0:h0 + 3, 0:128], in_=pot[:, 0:3, :]), [r_po2], [r_stg[hb]])
                    if hb == 1:
                        dma("sp", DO[pi, c["rsl"], :], stg[:].rearrange("p a b -> p (a b)"), reads=r_stg)

                nbt = len(batches)
                for t in range(nbt + 3):
                    if 0 <= t - 3 < nbt:
                        sD(t - 3)
                    if 0 <= t - 2 < nbt:
                        sC(t - 2)
                    if 0 <= t - 1 < nbt:
                        sB(t - 1)
                    if t < nbt:
                        sA(t)
                S.barrier()
            with ExitStack() as es:
                D3 = mk(es, "D3", [128, 3, 780], F32, 2)
                mxx, r_mxx = mk(es, "mxx", [128, 6], F32)
                e3, r_e3 = mk(es, "e3", [128, 3, 6], F32)
                w3, r_w3 = mk(es, "w3", [128, 3, 6], F32)
                dn, r_dn = mk(es, "dn", [128, 6], F32)
                cf, r_cf = mk(es, "cf", [128, 3, 6], F32)
                acc = [mk(es, "dacc%d" % h, [128, 128], F32) for h in range(6)]
                outb = Ring([(es.enter_context(nc.sbuf_tensor("doutb%d_%d" % (k, l), [128, 768], BF16)), [Res() for _ in range(6)]) for k in range(2)])
                for j in range(NT):
                    rows = slice(j * 128, (j + 1) * 128)
                    d3, r_d3 = D3.next()
                    dma("sp", d3[:], DO[:, rows, :].rearrange("a p n -> p a n"), writes=[r_d3])
                    d4 = d3[:].rearrange("p a (h c) -> p a h c", h=6)
                    S.op("dve", lambda d4=d4: nc.vector.tensor_tensor(out=mxx[:], in0=d4[:, 0, :, 128], in1=d4[:, 1, :, 128], op=ALU.max), [r_d3], [r_mxx])
                    S.op("dve", lambda d4=d4: nc.vector.tensor_tensor(out=mxx[:], in0=mxx[:], in1=d4[:, 2, :, 128], op=ALU.max), [r_d3, r_mxx], [r_mxx])
                    for p_ in range(3):
                        S.op("dve", lambda d4=d4, p_=p_: nc.vector.tensor_tensor(out=e3[:, p_, :], in0=d4[:, p_, :, 128], in1=mxx[:], op=ALU.subtract), [r_d3, r_mxx], [r_e3])
                    S.op("act", lambda: nc.scalar.activation(out=e3[:], in_=e3[:], func=AF.Exp), [r_e3], [r_e3])
                    for p_ in range(3):
                        S.op("dve", lambda d4=d4, p_=p_: nc.vector.tensor_tensor(out=w3[:, p_, :], in0=e3[:, p_, :], in1=d4[:, p_, :, 129], op=ALU.mult), [r_d3, r_e3], [r_w3])
                    S.op("dve", lambda: nc.vector.tensor_tensor(out=dn[:], in0=w3[:, 0, :], in1=w3[:, 1, :], op=ALU.add), [r_w3], [r_dn])
                    S.op("dve", lambda: nc.vector.tensor_tensor(out=dn[:], in0=dn[:], in1=w3[:, 2, :], op=ALU.add), [r_w3, r_dn], [r_dn])
                    S.op("dve", lambda: nc.vector.reciprocal(out=dn[:], in_=dn[:]), [r_dn], [r_dn])
                    for p_ in range(3):
                        S.op("dve", lambda p_=p_: nc.vector.tensor_tensor(out=cf[:, p_, :], in0=e3[:, p_, :], in1=dn[:], op=ALU.mult), [r_e3, r_dn], [r_cf])
                    ob, r_ob = outb.next()
                    for h in range(6):
                        eng = "dve"
                        at, r_at = acc[h]
                        S.op(eng, lambda eng=eng, at=at, d4=d4, h=h: E[eng].tensor_scalar(out=at[:], in0=d4[:, 0, h, 0:128], scalar1=cf[:, 0, h:h + 1], scalar2=None, op0=ALU.mult),
                             [r_d3, r_cf], [r_at])
                        S.op(eng, lambda eng=eng, at=at, d4=d4, h=h: E[eng].scalar_tensor_tensor(out=at[:], in0=d4[:, 1, h, 0:128], scalar=cf[:, 1, h:h + 1], in1=at[:], op0=ALU.mult, op1=ALU.add),
                             [r_d3, r_cf, r_at], [r_at])
                        S.op(eng, lambda eng=eng, at=at, d4=d4, h=h, ob=ob: E[eng].scalar_tensor_tensor(out=ob[:, h * 128:(h + 1) * 128], in0=d4[:, 2, h, 0:128], scalar=cf[:, 2, h:h + 1], in1=at[:], op0=ALU.mult, op1=ALU.add),
                             [r_d3, r_cf, r_at], [r_ob[h]])
                    dma("sp", MIX[rows, 512:1280], ob[:], reads=r_ob)
                S.barrier()
            if stop_phase <= 4:
                break

            with ExitStack() as es:
                wo, r_wo = mk(es, "wo", [128, 16, DM], BF16)
                g1, r_g1 = mk(es, "g1", [128, DM], F32)
                b1, r_b1 = mk(es, "b1", [128, DM], F32)
                wr, r_wr = mk(es, "wr", [128, 16, 36], F32)
                br, r_br = mk(es, "br", [1, 36], F32)
                mixl = mk(es, "mixl", [128, DM], BF16, 2)
                mT_ring = mk(es, "mT", [128, 16, 128], BF16, 2)
                xr = mk(es, "xr", [128, DM], F32, 2)
                u_ring = mk(es, "u5", [128, DM], F32, 2)
                x1 = mk(es, "x1t", [128, DM], F32, 2)
                x1b = mk(es, "x1bt", [128, DM], BF16, 2)
                x1T_ring = mk(es, "x1T", [128, 16, 128], F32, 2)
                st_ring = mk(es, "st5", [128, 4, 6], F32, 2)
                mv_ring = mk(es, "mv5", [128, 3], F32, 2)
                L_ring = mk(es, "L", [128, 36], F32, 2)
                sc_ring = mk(es, "sc5", [128, 16], F32, 2)
                goh_ring = mk(es, "goh", [128, 4], F32, 2)
                gex_ring = mk(es, "gex", [128, 4], F32, 2)
                pen_ring = mk(es, "pen", [128, 4], F32, 2)
                em_ring = mk(es, "em", [128, 32], F32, 2)
                em2_ring = mk(es, "em2", [128, 32], F32, 2)
                tp = mkp(es, "tp5", [128, 512], BF16, 2)
                po = mkp(es, "po5", [128, 512], F32, 4)
                tpf, r_tpf = mkp(es, "tpf", [128, 512], F32)
                plog, r_plog = mkp(es, "plog", [128, 64], F32)
                dma("pool", wo[:], w_out[l].rearrange("(k p) n -> p k n", p=128), writes=[r_wo])
                dma("sp", g1[:], ln1_g[l:l + 1, :].partition_broadcast(128), writes=[r_g1])
                dma("sp", b1[:], ln1_b[l:l + 1, :].partition_broadcast(128), writes=[r_b1])
                dma("sp", wr[:], w_rt[l].rearrange("(k p) n -> p k n", p=128), writes=[r_wr])
                dma("sp", br[:], b_rt[l:l + 1, :], writes=[r_br])
                for j in range(NT):
                    rows = slice(j * 128, (j + 1) * 128)
                    mT, r_mT = mT_ring.next()
                    u, r_u = u_ring.next()
                    x1T, r_x1T = x1T_ring.next()
                    st, r_st = st_ring.next()
                    mv, r_mv = mv_ring.next()
                    L, r_L = L_ring.next()
                    sc, r_sc = sc_ring.next()
                    goh, r_goh = goh_ring.next()
                    gex, r_gex = gex_ring.next()
                    pen, r_pen = pen_ring.next()
                    em, r_em = em_ring.next()
                    em2, r_em2 = em2_ring.next()
                    ml, r_ml = mixl.next()
                    dma("sp", ml[:], MIX[rows, :], writes=[r_ml])
                    xrt, r_xr = xr.next()
                    dma("sp", xrt[:], XCUR[rows, :], writes=[r_xr])
                    for g4 in range(4):
                        tpt, r_tp = tp.next()
                        for k in range(4):
                            kc = g4 * 4 + k
                            S.op("pe", lambda kc=kc, k=k, tpt=tpt, ml=ml: nc.tensor.transpose(out=tpt[:, k * 128:(k + 1) * 128], in_=ml[:, kc * 128:(kc + 1) * 128], identity=ident_b[:]),
                                 [r_ml, r_identb], [r_tp], signal=(k == 3))
                        eng = alt("act", "dve")
                        S.op(eng, lambda eng=eng, tpt=tpt, g4=g4: ecopy(eng, mT[:, g4 * 4:(g4 + 1) * 4, :], tpt[:].rearrange("p (k t) -> p k t", k=4)), [r_tp], [r_mT])
                    for n4 in range(4):
                        pot, r_po5 = po.next()
                        for kc in range(16):
                            S.op("pe", lambda kc=kc, pot=pot, n4=n4: nc.tensor.matmul(pot[:], lhsT=mT[:, kc, :], rhs=wo[:, kc, n4 * 512:(n4 + 1) * 512], start=(kc == 0), stop=(kc == 15)),
                                 [r_mT, r_wo], [r_po5], signal=(kc == 15))
                        S.op("dve", lambda pot=pot, n4=n4, xrt=xrt: nc.vector.scalar_tensor_tensor(out=u[:, n4 * 512:(n4 + 1) * 512], in0=xrt[:, n4 * 512:(n4 + 1) * 512], scalar=ALPHA,
                                                                                                  in1=pot[:], op0=ALU.mult, op1=ALU.add), [r_xr, r_po5], [r_u])
                    x1t, r_x1 = x1.next()
                    layer_norm_tile(None, u, r_u, g1, r_g1, b1, r_b1, st, r_st, mv, r_mv, x1t, r_x1)
                    dma("sp", X1[rows, :], x1t[:], reads=[r_x1])
                    xbt, r_xb1 = x1b.next()
                    S.op("act", lambda xbt=xbt, x1t=x1t: nc.scalar.copy(out=xbt[:], in_=x1t[:]), [r_x1], [r_xb1])
                    dma("sp", X1B[rows, :], xbt[:], reads=[r_xb1])
                    for g4 in range(4):
                        for k in range(4):
                            kc = g4 * 4 + k
                            S.op("pe", lambda kc=kc, k=k, x1t=x1t: nc.tensor.transpose(out=tpf[:, k * 128:(k + 1) * 128], in_=x1t[:, kc * 128:(kc + 1) * 128], identity=ident_f[:]),
                                 [r_x1, r_identf], [r_tpf], signal=(k == 3))
                        eng = alt("act", "dve")
                        S.op(eng, lambda eng=eng, g4=g4: ecopy(eng, x1T[:, g4 * 4:(g4 + 1) * 4, :], tpf[:].rearrange("p (k t) -> p k t", k=4)), [r_tpf], [r_x1T])
                    for kc in range(16):
                        S.op("pe", lambda kc=kc: nc.tensor.matmul(plog[:, 0:36], lhsT=x1T[:, kc, :], rhs=wr[:, kc, :], start=(kc == 0), stop=False), [r_x1T, r_wr], [r_plog], signal=False)
                    S.op("pe", lambda: nc.tensor.matmul(plog[:, 0:36], lhsT=ones_f[0:1, :], rhs=br[:], start=False, stop=True), [r_onesf, r_br], [r_plog])
                    S.op("act", lambda: nc.scalar.copy(out=L[:], in_=plog[:, 0:36]), [r_plog], [r_L])
                    V_ = nc.vector
                    S.op("dve", lambda: V_.reduce_max(out=sc[:, 0:1], in_=L[:, 0:4], axis=AX.X), [r_L], [r_sc])
                    S.op("dve", lambda: V_.tensor_scalar(out=goh[:], in0=L[:, 0:4], scalar1=sc[:, 0:1], scalar2=None, op0=ALU.is_ge), [r_L, r_sc], [r_goh])
                    S.op("dve", lambda: V_.tensor_scalar(out=sc[:, 1:2], in0=sc[:, 0:1], scalar1=-1.0, scalar2=None, op0=ALU.mult), [r_sc], [r_sc])
                    S.op("act", lambda: nc.scalar.activation(out=gex[:], in_=L[:, 0:4], func=AF.Exp, bias=sc[:, 1:2], scale=1.0, accum_out=sc[:, 2:3]), [r_L, r_sc], [r_gex, r_sc])
                    S.op("dve", lambda: V_.reciprocal(out=sc[:, 3:4], in_=sc[:, 2:3]), [r_sc], [r_sc])
                    S.op("dve", lambda: V_.tensor_scalar(out=pen[:], in0=goh[:], scalar1=BIG, scalar2=-BIG, op0=ALU.mult, op1=ALU.add), [r_goh], [r_pen])
                    for g in range(4):
                        S.op("dve", lambda g=g: V_.tensor_scalar(out=em[:, g * 8:(g + 1) * 8], in0=L[:, 4 + g * 8:4 + (g + 1) * 8], scalar1=pen[:, g:g + 1], scalar2=None, op0=ALU.add),
                             [r_L, r_pen], [r_em])
                    S.op("dve", lambda: V_.reduce_max(out=sc[:, 4:5], in_=em[:], axis=AX.X), [r_em], [r_sc])
                    S.op("dve", lambda j=j: V_.tensor_scalar(out=OH[:, j, 0, :], in0=em[:], scalar1=sc[:, 4:5], scalar2=None, op0=ALU.is_ge), [r_em, r_sc], [r_OH])
                    S.op("dve", lambda j=j: V_.scalar_tensor_tensor(out=em2[:], in0=OH[:, j, 0, :], scalar=-BIG, in1=em[:], op0=ALU.mult, op1=ALU.add), [r_OH, r_em], [r_em2])
                    S.op("dve", lambda: V_.reduce_max(out=sc[:, 5:6], in_=em2[:], axis=AX.X), [r_em2], [r_sc])
                    S.op("dve", lambda j=j: V_.tensor_scalar(out=OH[:, j, 1, :], in0=em2[:], scalar1=sc[:, 5:6], scalar2=None, op0=ALU.is_ge), [r_em2, r_sc], [r_OH])
                    S.op("dve", lambda: V_.tensor_tensor(out=sc[:, 6:7], in0=sc[:, 5:6], in1=sc[:, 4:5], op=ALU.subtract), [r_sc], [r_sc])
                    S.op("act", lambda: nc.scalar.activation(out=sc[:, 7:8], in_=sc[:, 6:7], func=AF.Exp), [r_sc], [r_sc])
                    S.op("dve", lambda: V_.tensor_scalar(out=sc[:, 8:9], in0=sc[:, 7:8], scalar1=1.0, scalar2=None, op0=ALU.add), [r_sc], [r_sc])
                    S.op("dve", lambda: V_.reciprocal(out=sc[:, 9:10], in_=sc[:, 8:9]), [r_sc], [r_sc])
                    S.op("dve", lambda j=j: V_.tensor_tensor(out=GATE[:, j, 0:1], in0=sc[:, 3:4], in1=sc[:, 9:10], op=ALU.mult), [r_sc], [r_GATE])
                    S.op("dve", lambda: V_.tensor_tensor(out=sc[:, 10:11], in0=sc[:, 7:8], in1=sc[:, 9:10], op=ALU.mult), [r_sc], [r_sc])
                    S.op("dve", lambda j=j: V_.tensor_tensor(out=GATE[:, j, 1:2], in0=sc[:, 3:4], in1=sc[:, 10:11], op=ALU.mult), [r_sc], [r_GATE])
                    S.op("dve", lambda j=j: V_.tensor_tensor(out=A_b[:, j, :], in0=OH[:, j, 0, :], in1=OH[:, j, 1, :], op=ALU.add), [r_OH], [r_Ab])
                S.barrier()
            if stop_phase <= 5:
                break

            with ExitStack() as es:
                V_ = nc.vector
                cnt, r_cnt = mk(es, "cnt", [128, 32], F32)
                pc, r_pc = mk(es, "pc", [128, 32], F32)
                pa, r_pa = mk(es, "pa", [128, 32], F32)
                pb, r_pb = mk(es, "pb", [128, 32], F32)
                pstart, r_pstart = mk(es, "pstart", [128, 32], F32)
                carry, r_carry = mk(es, "carry", [128, 32], F32)
                pos, r_pos = mk(es, "pos", [128, 32], F32)
                tmp, r_tmp = mk(es, "tmp6", [128, 32], F32)
                destf, r_destf = mk(es, "destf", [128, NT, 2], F32)
                EB, r_EB = mk(es, "EB", [128, NBLK], F32)
                bsg, r_bsg = mk(es, "bsg", [128, NBLK], F32)
                bsd, r_bsd = mk(es, "bsd", [128, NBLK], F32)
                ioi, r_ioi = mk(es, "ioi", [128, 1], I32)
                iof, r_iof = mk(es, "iof", [128, 1], F32)
                iof4, r_iof4 = mk(es, "iof4", [128, 1], F32)
                strib, r_strib = mk(es, "strib", [128, 128], BF16)
                strif, r_strif = mk(es, "strif", [128, 128], F32)
                xs = mk(es, "xs6", [128, DM], BF16, 3)
                ptot, r_ptot = mkp(es, "ptot", [128, 64], F32)
                prk = mkp(es, "prk", [128, 64], F32, 2)
                dma("sp", strif[:], c_stri, writes=[r_strif])
                S.op("dve", lambda: V_.tensor_copy(out=strib[:], in_=strif[:]), [r_strif], [r_strib])
                S.op("pool", lambda: nc.gpsimd.iota(ioi[:], pattern=[[0, 1]], base=0, channel_multiplier=1), [], [r_ioi])
                S.op("dve", lambda: V_.tensor_copy(out=iof[:], in_=ioi[:]), [r_ioi], [r_iof])
                for j in range(NT):
                    S.op("pe", lambda j=j: nc.tensor.matmul(ptot[:, 0:32], lhsT=ones_b[:], rhs=A_b[:, j, :], start=(j == 0), stop=(j == NT - 1)), [r_onesb, r_Ab], [r_ptot], signal=(j == NT - 1))
                S.op("dve", lambda: V_.tensor_copy(out=cnt[:], in_=ptot[:, 0:32]), [r_ptot], [r_cnt])
                S.op("dve", lambda: V_.memset(tmp[:], 0.0), [], [r_tmp])
                for m_ in range(-(-SEQ // BLK)):
                    S.op("dve", lambda m_=m_: V_.scalar_tensor_tensor(out=tmp[:], in0=cnt[:], scalar=float(m_ * BLK), in1=tmp[:], op0=ALU.is_gt, op1=ALU.add), [r_cnt, r_tmp], [r_tmp])
                S.op("dve", lambda: V_.tensor_scalar(out=pc[:], in0=tmp[:], scalar1=float(BLK), scalar2=None, op0=ALU.mult), [r_tmp], [r_pc])
                S.op("dve", lambda: V_.tensor_copy(out=pa[:], in_=pc[:]), [r_pc], [r_pa])
                cur, r_cur, oth, r_oth = pa, r_pa, pb, r_pb
                for sh in (1, 2, 4, 8, 16):
                    S.op("dve", lambda cur=cur, oth=oth, sh=sh: V_.tensor_copy(out=oth[:, 0:sh], in_=cur[:, 0:sh]), [r_cur], [r_oth])
                    S.op("dve", lambda cur=cur, oth=oth, sh=sh: V_.tensor_tensor(out=oth[:, sh:32], in0=cur[:, sh:32], in1=cur[:, 0:32 - sh], op=ALU.add), [r_cur], [r_oth])
                    cur, r_cur, oth, r_oth = oth, r_oth, cur, r_cur
                pend, r_pend = cur, r_cur
                S.op("dve", lambda: V_.tensor_tensor(out=pstart[:], in0=pend[:], in1=pc[:], op=ALU.subtract), [r_pend, r_pc], [r_pstart])
                S.op("dve", lambda: V_.memset(carry[:], 0.0), [], [r_carry])
                for j in range(NT):
                    prt, r_pr = prk.next()
                    S.op("pe", lambda prt=prt, j=j: nc.tensor.matmul(prt[:, 0:32], lhsT=strib[:], rhs=A_b[:, j, :], start=True, stop=True), [r_strib, r_Ab], [r_pr], signal=False)
                    S.op("pe", lambda prt=prt, j=j: nc.tensor.matmul(prt[:, 32:64], lhsT=ones_b[:], rhs=A_b[:, j, :], start=True, stop=True), [r_onesb, r_Ab], [r_pr])
                    S.op("dve", lambda prt=prt: V_.tensor_tensor(out=pos[:], in0=prt[:, 0:32], in1=carry[:], op=ALU.add), [r_pr, r_carry], [r_pos])
                    S.op("dve", lambda: V_.tensor_tensor(out=pos[:], in0=pos[:], in1=pstart[:], op=ALU.add), [r_pos, r_pstart], [r_pos])
                    for k in range(2):
                        S.op("dve", lambda j=j, k=k: V_.tensor_tensor(out=tmp[:], in0=OH[:, j, k, :], in1=pos[:], op=ALU.mult), [r_OH, r_pos], [r_tmp])
                        S.op("dve", lambda j=j, k=k: V_.reduce_sum(out=destf[:, j, k:k + 1], in_=tmp[:], axis=AX.X), [r_tmp], [r_destf])
                    S.op("dve", lambda prt=prt: V_.tensor_tensor(out=carry[:], in0=carry[:], in1=prt[:, 32:64], op=ALU.add), [r_pr, r_carry], [r_carry])
                S.op("dve", lambda: V_.tensor_copy(out=DESTI[:], in_=destf[:]), [r_destf], [r_DESTI])
                for j in range(NT):
                    rows = slice(j * 128, (j + 1) * 128)
                    xst, r_xs = xs.next()
                    dma("sp", xst[:], X1B[rows, :], writes=[r_xs])
                    for k in range(2):
                        S.dma("pool", lambda xst=xst, j=j, k=k: nc.gpsimd.indirect_dma_start(
                            out=XG, out_offset=bass.IndirectOffsetOnAxis(ap=DESTI[:, j, k:k + 1], axis=0), in_=xst[:], in_offset=None),
                            [r_xs, r_DESTI], [])
                for i in range(NBLK):
                    S.op("dve", lambda i=i: V_.tensor_scalar(out=tmp[:], in0=pend[:], scalar1=float(i * BLK), scalar2=None, op0=ALU.is_le), [r_pend], [r_tmp])
                    S.op("dve", lambda i=i: V_.reduce_sum(out=EB[:, i:i + 1], in_=tmp[:], axis=AX.X), [r_tmp], [r_EB])
                S.op("dve", lambda: V_.tensor_scalar(out=iof4[:], in0=iof[:], scalar1=4.0, scalar2=None, op0=ALU.mult), [r_iof], [r_iof4])
                S.op("dve", lambda: V_.tensor_scalar(out=bsg[:], in0=EB[:], scalar1=512.0, scalar2=iof4[:, 0:1], op0=ALU.mult, op1=ALU.add), [r_EB, r_iof4], [r_bsg])
                S.op("dve", lambda: V_.tensor_scalar(out=bsd[:], in0=EB[:], scalar1=512.0, scalar2=iof[:, 0:1], op0=ALU.mult, op1=ALU.add), [r_EB, r_iof], [r_bsd])
                for q4 in range(4):
                    S.op("dve", lambda q4=q4: V_.tensor_scalar(out=IDXG[:, :, q4], in0=bsg[:], scalar1=float(q4 + l * 32 * 512), scalar2=None, op0=ALU.add), [r_bsg], [r_IDXG])
                for fc in range(4):
                    S.op("dve", lambda fc=fc: V_.tensor_scalar(out=IDXD[:, :, fc], in0=bsd[:], scalar1=float(fc * 128 + l * 32 * 512), scalar2=None, op0=ALU.add), [r_bsd], [r_IDXD])
                S.barrier()
            if stop_phase <= 6:
                break

            with ExitStack() as es:
                def wring(name, shape, nres):
                    return Ring([(es.enter_context(nc.sbuf_tensor("%s%d_%d" % (name, k, l), shape, BF16)), [Res() for _ in range(nres)]) for k in range(2)])
                wgr = wring("wgt", [128, 16, 512], 16)
                wur = wring("wut", [128, 16, 512], 16)
                wdr = wring("wdt", [128, 4, DM], 4)
                xg = mk(es, "xg", [128, DM], BF16, 2)
                xgT = mk(es, "xgT", [128, 16, BLK], BF16, 2)
                sgm = mk(es, "sgm", [128, BLK], F32, 2)
                hT = mk(es, "hT", [128, 4, BLK], BF16, 2)
                ys = mk(es, "ys", [128, DM], F32, 2)
                tp = mkp(es, "tp7", [128, 512], BF16, 2)
                pg = mkp(es, "pg", [128, BLK], F32, 2)
                pu = mkp(es, "pu", [128, BLK], F32, 2)
                py = mkp(es, "py", [128, 512], F32, 2)
                bc_g = nc.gpsimd.to_reg((l + 1) * 32 * 512 - 1)
                bc_d = nc.gpsimd.to_reg((l + 1) * 32 * 512 - 1)
                for (ring_, nres_) in ((wgr, 16), (wur, 16), (wdr, 4)):
                    for (t_, rs_) in ring_.items:
                        S.op("pool", lambda t_=t_: nc.gpsimd.memset(t_[:], 0.0), [], rs_)
                for i in range(NBLK):
                    wgt, r_wg = wgr.next()
                    wut, r_wu = wur.next()
                    wdt, r_wd = wdr.next()
                    for q4 in range(4):
                        S.dma("pool", lambda wgt=wgt, i=i, q4=q4: nc.gpsimd.indirect_dma_start(
                            out=wgt[:, 4 * q4:4 * q4 + 4, :].rearrange("p a b -> p (a b)"), out_offset=None, in_=w_eg, in_offset=bass.IndirectOffsetOnAxis(ap=IDXG[:, i, q4:q4 + 1], axis=0), bounds_check=bc_g, oob_is_err=False),
                            [r_IDXG], [r_wg[q4]])
                        S.dma("pool", lambda wut=wut, i=i, q4=q4: nc.gpsimd.indirect_dma_start(
                            out=wut[:, 4 * q4:4 * q4 + 4, :].rearrange("p a b -> p (a b)"), out_offset=None, in_=w_eu, in_offset=bass.IndirectOffsetOnAxis(ap=IDXG[:, i, q4:q4 + 1], axis=0), bounds_check=bc_g, oob_is_err=False),
                            [r_IDXG], [r_wu[q4]])
                    for fc in range(4):
                        S.dma("pool", lambda wdt=wdt, i=i, fc=fc: nc.gpsimd.indirect_dma_start(
                            out=wdt[:, fc, :], out_offset=None, in_=w_ed, in_offset=bass.IndirectOffsetOnAxis(ap=IDXD[:, i, fc:fc + 1], axis=0), bounds_check=bc_d, oob_is_err=False),
                            [r_IDXD], [r_wd[fc]])
                    xTt, r_xgT = xgT.next()
                    for sb in range(BLK // 128):
                        xgt, r_xg = xg.next()
                        r0 = i * BLK + sb * 128
                        dma("sp", xgt[:], XG[r0:r0 + 128, :], writes=[r_xg])
                        for g4 in range(4):
                            tpt, r_tp = tp.next()
                            for k in range(4):
                                kc = g4 * 4 + k
                                S.op("pe", lambda kc=kc, k=k, tpt=tpt, xgt=xgt: nc.tensor.transpose(out=tpt[:, k * 128:(k + 1) * 128], in_=xgt[:, kc:kc + 127 * 16 + 1:16], identity=ident_b[:]),
                                     [r_xg, r_identb], [r_tp], signal=(k == 3))
                            eng = alt("act", "dve")
                            S.op(eng, lambda eng=eng, tpt=tpt, g4=g4, sb=sb, xTt=xTt: ecopy(eng, xTt[:, g4 * 4:(g4 + 1) * 4, sb * 128:(sb + 1) * 128], tpt[:].rearrange("p (k t) -> p k t", k=4)),
                                 [r_tp], [r_xgT])
                    hTt, r_hT = hT.next()
                    for fc in range(4):
                        pgt, r_pg = pg.next()
                        put, r_pu = pu.next()
                        for kc in range(16):
                            S.op("pe", lambda kc=kc, fc=fc, pgt=pgt, wgt=wgt, xTt=xTt: nc.tensor.matmul(pgt[:], lhsT=wgt[:, kc, fc * 128:(fc + 1) * 128], rhs=xTt[:, kc, :], start=(kc == 0), stop=(kc == 15)),
                                 [r_wg[kc // 4], r_xgT], [r_pg], signal=(kc == 15))
                        for kc in range(16):
                            S.op("pe", lambda kc=kc, fc=fc, put=put, wut=wut, xTt=xTt: nc.tensor.matmul(put[:], lhsT=wut[:, kc, fc * 128:(fc + 1) * 128], rhs=xTt[:, kc, :], start=(kc == 0), stop=(kc == 15)),
                                 [r_wu[kc // 4], r_xgT], [r_pu], signal=(kc == 15))
                        sgt, r_sg = sgm.next()
                        S.op("act", lambda sgt=sgt, pgt=pgt: nc.scalar.activation(out=sgt[:], in_=pgt[:], func=AF.Silu), [r_pg], [r_sg])
                        S.op("dve", lambda sgt=sgt, put=put, hTt=hTt, fc=fc: nc.vector.tensor_tensor(out=hTt[:, fc, :], in0=sgt[:], in1=put[:], op=ALU.mult), [r_sg, r_pu], [r_hT])
                    for sb in range(BLK // 128):
                        yst, r_ys = ys.next()
                        for n4 in range(4):
                            pyt, r_py = py.next()
                            for fc in range(4):
                                S.op("pe", lambda fc=fc, pyt=pyt, hTt=hTt, wdt=wdt, sb=sb, n4=n4: nc.tensor.matmul(pyt[:], lhsT=hTt[:, fc, sb * 128:(sb + 1) * 128], rhs=wdt[:, fc, n4 * 512:(n4 + 1) * 512], start=(fc == 0), stop=(fc == 3)),
                                     [r_hT, r_wd[fc]], [r_py], signal=(fc == 3))
                            eng = alt("act", "dve")
                            S.op(eng, lambda eng=eng, yst=yst, pyt=pyt, n4=n4: ecopy(eng, yst[:, n4 * 512:(n4 + 1) * 512], pyt[:]), [r_py], [r_ys])
                        r0 = i * BLK + sb * 128
                        dma("sp", YB[r0:r0 + 128, :], yst[:], reads=[r_ys])
                S.barrier()
                nc.gpsimd.free_register(bc_g)
                nc.gpsimd.free_register(bc_d)
            if stop_phase <= 7:
                break

            with ExitStack() as es:
                g2, r_g2 = mk(es, "g2", [128, DM], F32)
                b2, r_b2 = mk(es, "b2", [128, DM], F32)
                y1 = mk(es, "y1", [128, DM], F32, 2)
                y2 = mk(es, "y2", [128, DM], F32, 2)
                x1r = mk(es, "x1r", [128, DM], F32, 2)
                u_ring = mk(es, "u8", [128, DM], F32, 2)
                xo = mk(es, "xo", [128, DM], F32, 2)
                st_ring = mk(es, "st8", [128, 4, 6], F32, 2)
                mv_ring = mk(es, "mv8", [128, 3], F32, 2)
                dma("sp", g2[:], ln2_g[l:l + 1, :].partition_broadcast(128), writes=[r_g2])
                dma("sp", b2[:], ln2_b[l:l + 1, :].partition_broadcast(128), writes=[r_b2])
                for j in range(NT):
                    rows = slice(j * 128, (j + 1) * 128)
                    u, r_u = u_ring.next()
                    st, r_st = st_ring.next()
                    mv, r_mv = mv_ring.next()
                    y1t, r_y1 = y1.next()
                    y2t, r_y2 = y2.next()
                    S.dma("pool", lambda y1t=y1t, j=j: nc.gpsimd.indirect_dma_start(out=y1t[:], out_offset=None, in_=YB, in_offset=bass.IndirectOffsetOnAxis(ap=DESTI[:, j, 0:1], axis=0)),
                          [r_DESTI], [r_y1])
                    S.dma("pool", lambda y2t=y2t, j=j: nc.gpsimd.indirect_dma_start(out=y2t[:], out_offset=None, in_=YB, in_offset=bass.IndirectOffsetOnAxis(ap=DESTI[:, j, 1:2], axis=0)),
                          [r_DESTI], [r_y2])
                    xt_, r_x1r = x1r.next()
                    dma("sp", xt_[:], X1[rows, :], writes=[r_x1r])
                    S.op("dve", lambda y1t=y1t, j=j: nc.vector.tensor_scalar(out=u[:], in0=y1t[:], scalar1=GATE[:, j, 0:1], scalar2=None, op0=ALU.mult), [r_y1, r_GATE], [r_u])
                    S.op("dve", lambda y2t=y2t, j=j: nc.vector.scalar_tensor_tensor(out=u[:], in0=y2t[:], scalar=GATE[:, j, 1:2], in1=u[:], op0=ALU.mult, op1=ALU.add), [r_y2, r_GATE, r_u], [r_u])
                    S.op("dve", lambda xt_=xt_: nc.vector.scalar_tensor_tensor(out=u[:], in0=xt_[:], scalar=ALPHA, in1=u[:], op0=ALU.mult, op1=ALU.add), [r_x1r, r_u], [r_u])
                    xot, r_xo = xo.next()
                    layer_norm_tile(None, u, r_u, g2, r_g2, b2, r_b2, st, r_st, mv, r_mv, xot, r_xo)
                    dma("sp", XNEXT[rows, :], xot[:], reads=[r_xo])
                S.barrier()
            XCUR = XNEXT
    return nc, S


def make_inputs(inputs, core, stop_phase=99):
    hc = host_consts()
    m = {}
    m["x"] = np.ascontiguousarray(inputs["x"][core])
    for k in ("w_in", "w_gla_gate", "b_gla_gate", "ret_norm_g", "gla_norm_g", "w_out", "ln1_g", "ln1_b", "ln2_g", "ln2_b"):
        m[k] = np.ascontiguousarray(inputs[k])
    m["w_router"] = np.ascontiguousarray(np.concatenate([inputs["w_router_group"], inputs["w_router_expert"]], axis=-1))
    m["b_router"] = np.ascontiguousarray(np.concatenate([inputs["b_router_group"], inputs["b_router_expert"]], axis=-1))
    if stop_phase >= 7:
        m["w_expert_gate"] = np.ascontiguousarray(inputs["w_expert_gate"]).reshape(DEPTH * 32 * 128 * 4, 2048)
        m["w_expert_up"] = np.ascontiguousarray(inputs["w_expert_up"]).reshape(DEPTH * 32 * 128 * 4, 2048)
        m["w_expert_down"] = np.ascontiguousarray(inputs["w_expert_down"]).reshape(DEPTH * 32 * 512, DM)
    m["c_ident"] = hc["ident"]
    m["c_mask01"] = hc["mask01"]
    m["c_stri"] = hc["stri"]
    m["c_dmask0"] = hc["dmask0"]
    m["c_dmask1"] = hc["dmask1"]
    m["c_rope"] = hc["rope"]
    return m


def kernel(**inputs):
    inputs = {k: np.asarray(v) for k, v in inputs.items()}
    nc, _ = build()
    in_maps = [make_inputs(inputs, c) for c in range(8)]
    res = run_bass_kernel_spmd(nc, in_maps, core_ids=list(range(8)))
    return np.stack([r["y"] for r in res.results], axis=0).astype(np.float32)
```

```python
import numpy as np
from contextlib import ExitStack
import concourse.bass as bass
import concourse.mybir as mybir
from concourse.bass_utils import run_bass_kernel_spmd

F32 = mybir.dt.float32
BF16 = mybir.dt.bfloat16
I32 = mybir.dt.int32
AF = mybir.ActivationFunctionType
ALU = mybir.AluOpType
AX = mybir.AxisListType

ND = 6
SEQ = 4096
DM = 2048
NT = SEQ // 128
DEPTH = 4
INW = 6672
ALPHA = float((2 * DEPTH) ** 0.25)
EPS = 1e-5
BLK = 384
NBLK = -(-(2 * SEQ + 32 * (BLK - 1)) // BLK)
NROWS = NBLK * BLK
BIG = 30000.0


class Res:
    __slots__ = ("name", "w", "r")

    def __init__(self, name=""):
        self.name = name
        self.w = {}
        self.r = {}


class Sched:
    def __init__(self, nc, es, same_sync=True):
        self.nc = nc
        self.same_sync = same_sync
        self.e = dict(pe=nc.tensor, act=nc.scalar, dve=nc.vector, pool=nc.gpsimd, sp=nc.sync)
        self.sem = {k: es.enter_context(nc.semaphore("s_" + k)) for k in ("pe", "act", "dve", "pool")}
        self.cnt = {k: 0 for k in self.sem}
        self.pending = {k: False for k in self.sem}
        self.dsem = {q: [es.enter_context(nc.semaphore("d_%s%d" % (q, i))) for i in range(ND)]
                     for q in ("sp", "pool", "act")}
        self.dcnt = {q: [0] * ND for q in self.dsem}
        self.dnext = {q: 0 for q in self.dsem}
        self.known = {k: {} for k in self.e}
        self.n_ins = 0
        self.n_wait = 0

    def _semof(self, key):
        if key[0] == "c":
            return self.sem[key[1]], 1
        return self.dsem[key[1]][key[2]], 16

    def _wait(self, eng, key, val):
        if self.known[eng].get(key, 0) >= val:
            return
        sem, mult = self._semof(key)
        self.e[eng].wait_ge(sem, val * mult)
        self.known[eng][key] = val
        self.n_wait += 1

    def _deps(self, eng, reads, writes):
        deps = {}
        own = ("c", eng)
        for r in reads:
            for k, v in r.w.items():
                if deps.get(k, 0) < v:
                    deps[k] = v
        for w in writes:
            for d in (w.w, w.r):
                for k, v in d.items():
                    if k == own:
                        continue
                    if deps.get(k, 0) < v:
                        deps[k] = v
        for k, v in deps.items():
            if k == own and (eng == "pe" or not self.same_sync):
                continue
            self._wait(eng, k, v)

    def _mark(self, key, val, reads, writes):
        for r in reads:
            if r.r.get(key, 0) < val:
                r.r[key] = val
        for w in writes:
            w.w = {key: val}
            w.r = {}

    def op(self, eng, fn, reads=(), writes=(), signal=True):
        self._deps(eng, reads, writes)
        ins = fn()
        self.n_ins += 1
        if signal:
            self.cnt[eng] += 1
            ins.then_inc(self.sem[eng], 1)
            self.pending[eng] = False
            val = self.cnt[eng]
        else:
            self.pending[eng] = True
            val = self.cnt[eng] + 1
        self._mark(("c", eng), val, reads, writes)
        return ins

    def dma(self, q, fn, reads=(), writes=()):
        slot = self.dnext[q]
        self.dnext[q] = (slot + 1) % ND
        key = ("d", q, slot)
        if self.dcnt[q][slot] > 0:
            self._wait(q, key, self.dcnt[q][slot])
        self._deps(q, reads, writes)
        ins = fn()
        self.n_ins += 1
        self.dcnt[q][slot] += 1
        ins.then_inc(self.dsem[q][slot], 16)
        self._mark(key, self.dcnt[q][slot], reads, writes)
        return ins

    def barrier(self):
        for k in self.pending:
            assert not self.pending[k], "pending unsignaled instruction on " + k
        for eng in self.e.keys():
            for k in self.cnt:
                if self.cnt[k] > 0:
                    self._wait(eng, ("c", k), self.cnt[k])
            for q in self.dcnt:
                for i in range(ND):
                    if self.dcnt[q][i] > 0:
                        self._wait(eng, ("d", q, i), self.dcnt[q][i])


class Ring:
    def __init__(self, items):
        self.items = items
        self.i = 0

    def next(self):
        it = self.items[self.i]
        self.i = (self.i + 1) % len(self.items)
        return it


def host_consts():
    c = {}
    i = np.arange(128)
    c["ident"] = np.eye(128, dtype=np.float32)
    c["mask01"] = (i[:, None] <= i[None, :]).astype(np.float32)
    c["stri"] = (i[:, None] < i[None, :]).astype(np.float32)
    j = np.arange(256)
    band = (j[None, :] >= i[:, None]) & (j[None, :] <= i[:, None] + 128)
    c["dmask1"] = np.where(band, 0.0, -BIG).astype(np.float32)
    c["dmask0"] = np.where(band & (j[None, :] >= 128), 0.0, -BIG).astype(np.float32)
    half = 64
    inv = (np.float32(10000.0) ** (-np.arange(half, dtype=np.float32) / np.float32(half))).astype(np.float32)
    pos = np.arange(SEQ, dtype=np.float32)
    ang = (pos[:, None] * inv[None, :]).astype(np.float32)
    cos = np.cos(ang).astype(np.float32)
    sin = np.sin(ang).astype(np.float32)
    h = np.arange(4, dtype=np.float32)
    log_g = np.log1p(-np.exp2(-5.0 - h)).astype(np.float64)
    cc = (np.arange(SEQ) % 128).astype(np.float64)
    qd = np.exp((cc[:, None] + 1.0) * log_g[None, :])
    kd = np.exp(-(cc[:, None] + 1.0) * log_g[None, :]) * (128.0 ** -0.5)
    rope = np.zeros((4, SEQ, 4, 64), np.float32)
    rope[0] = cos[:, None, :] * qd[:, :, None]
    rope[1] = sin[:, None, :] * qd[:, :, None]
    rope[2] = cos[:, None, :] * kd[:, :, None]
    rope[3] = sin[:, None, :] * kd[:, :, None]
    c["rope"] = rope.reshape(4, SEQ, 256)
    c["g128"] = [float(np.exp(128.0 * lg)) for lg in log_g]
    return c


def build(n_layers=DEPTH, debug=(), stop_phase=99):
    nc = bass.Bass("TRN2", target_bir_lowering=False)
    hc = host_consts()
    g128 = hc["g128"]

    def din(name, shape, dt=F32):
        return nc.dram_tensor(name, list(shape), dt, kind="ExternalInput").ap()

    def dscr(name, shape, dt):
        return nc.dram_tensor(name, list(shape), dt, kind=("ExternalOutput" if name in debug else "Internal")).ap()

    x_in = din("x", [SEQ, DM])
    w_in = din("w_in", [DEPTH, DM, INW])
    w_gg = din("w_gla_gate", [DEPTH, 16, 384])
    b_gg = din("b_gla_gate", [DEPTH, 384])
    ret_g = din("ret_norm_g", [DEPTH, 512])
    gla_g = din("gla_norm_g", [DEPTH, 768])
    w_out = din("w_out", [DEPTH, DM, DM])
    ln1_g = din("ln1_g", [DEPTH, DM])
    ln1_b = din("ln1_b", [DEPTH, DM])
    w_rt = din("w_router", [DEPTH, DM, 36])
    b_rt = din("b_router", [DEPTH, 36])
    if stop_phase >= 7:
        w_eg = din("w_expert_gate", [DEPTH * 32 * 128 * 4, 2048])
        w_eu = din("w_expert_up", [DEPTH * 32 * 128 * 4, 2048])
        w_ed = din("w_expert_down", [DEPTH * 32 * 512, DM])
    ln2_g = din("ln2_g", [DEPTH, DM])
    ln2_b = din("ln2_b", [DEPTH, DM])
    c_ident = din("c_ident", [128, 128])
    c_mask01 = din("c_mask01", [128, 128])
    c_stri = din("c_stri", [128, 128])
    c_dmask0 = din("c_dmask0", [128, 256])
    c_dmask1 = din("c_dmask1", [128, 256])
    c_rope = din("c_rope", [4, SEQ, 256])
    y_out = nc.dram_tensor("y", [SEQ, DM], F32, kind="ExternalOutput").ap()

    P = dscr("P", [SEQ, 5120], BF16)
    PT = dscr("PT", [1536, SEQ], BF16)
    GAT = dscr("GAT", [16, SEQ], F32)
    DO = dscr("DO", [3, SEQ, 6 * 130], F32)
    MIX = dscr("MIX", [SEQ, DM], BF16)
    X1 = dscr("X1", [SEQ, DM], F32)
    X1B = dscr("X1B", [SEQ, DM], BF16)
    XG = dscr("XG", [NROWS, DM], BF16)
    YB = dscr("YB", [NROWS, DM], F32)
    XA = dscr("XA", [SEQ, DM], F32)
    XB_ = dscr("XBb", [SEQ, DM], F32)

    with ExitStack() as es0:
        S = Sched(nc, es0)
        E = S.e
        rr = {"i": 0}

        def alt(*engs):
            rr["i"] += 1
            return engs[rr["i"] % len(engs)]

        def ecopy(eng, out, in_):
            if eng == "act":
                return nc.scalar.copy(out=out, in_=in_)
            return E[eng].tensor_copy(out=out, in_=in_)

        def dma(q, out, in_, reads=(), writes=()):
            return S.dma(q, lambda: E[q].dma_start(out=out, in_=in_), reads, writes)

        uid = [0]

        def mk(es, name, shape, dt, n=1):
            items = []
            for k in range(n):
                uid[0] += 1
                t = es.enter_context(nc.sbuf_tensor("%s%d_%d" % (name, k, uid[0]), list(shape), dt))
                items.append((t, Res(name)))
            return items[0] if n == 1 else Ring(items)

        def mkp(es, name, shape, dt, n=1):
            items = []
            for k in range(n):
                uid[0] += 1
                t = es.enter_context(nc.psum_tensor("%s%d_%d" % (name, k, uid[0]), list(shape), dt))
                items.append((t, Res(name)))
            return items[0] if n == 1 else Ring(items)

        ident_f, r_identf = mk(es0, "identf", [128, 128], F32)
        ident_b, r_identb = mk(es0, "identb", [128, 128], BF16)
        mask01, r_mask01 = mk(es0, "mask01", [128, 128], F32)
        ones_b, r_onesb = mk(es0, "onesb", [128, 128], BF16)
        ones_f, r_onesf = mk(es0, "onesf", [128, 128], F32)
        dma("sp", ident_f[:], c_ident, writes=[r_identf])
        dma("sp", mask01[:], c_mask01, writes=[r_mask01])
        S.op("dve", lambda: nc.vector.tensor_copy(out=ident_b[:], in_=ident_f[:]), [r_identf], [r_identb])
        S.op("dve", lambda: nc.vector.memset(ones_b[:], 1.0), [], [r_onesb])
        S.op("dve", lambda: nc.vector.memset(ones_f[:], 1.0), [], [r_onesf])

        OH, r_OH = mk(es0, "OH", [128, NT, 2, 32], F32)
        A_b, r_Ab = mk(es0, "A_b", [128, NT, 32], BF16)
        GATE, r_GATE = mk(es0, "GATE", [128, NT, 2], F32)
        DESTI, r_DESTI = mk(es0, "DESTI", [128, NT, 2], I32)
        IDXG, r_IDXG = mk(es0, "IDXG", [128, NBLK, 4], I32)
        IDXD, r_IDXD = mk(es0, "IDXD", [128, NBLK, 4], I32)
        if stop_phase >= 6:
            with ExitStack() as es:
                zt, r_zt = mk(es, "zt", [128, DM], BF16)
                S.op("dve", lambda: nc.vector.memset(zt[:], 0.0), [], [r_zt])
                for i in range(NROWS // 128):
                    dma("sp", XG[i * 128:(i + 1) * 128, :], zt[:], reads=[r_zt])
                S.barrier()

        def run_skewed(tile_fn, n):
            gens = []
            t = 0
            while t < n or gens:
                if t < n:
                    gens.append(tile_fn(t))
                for g in list(gens):
                    try:
                        next(g)
                    except StopIteration:
                        gens.remove(g)
                t += 1

        def rstd(out_ap, var_ap, r_mv):
            S.op("dve", lambda: nc.vector.tensor_scalar(out=out_ap, in0=var_ap, scalar1=EPS, scalar2=None, op0=ALU.add), [r_mv], [r_mv])
            S.op("act", lambda: nc.scalar.sqrt(out=out_ap, in_=out_ap), [r_mv], [r_mv])
            S.op("dve", lambda: nc.vector.reciprocal(out=out_ap, in_=out_ap), [r_mv], [r_mv])

        def layer_norm_tile(es_unused, u, r_u, gt, r_gt, bt, r_bt, st, r_st, mv, r_mv, outt, r_out):
            for c4 in range(4):
                S.op("dve", lambda c4=c4: nc.vector.bn_stats(out=st[:, c4, :], in_=u[:, c4 * 512:(c4 + 1) * 512]),
                     [r_u], [r_st])
            S.op("dve", lambda: nc.vector.bn_aggr(out=mv[:, 0:2], in_=st[:].rearrange("p a b -> p (a b)")), [r_st], [r_mv])
            rstd(mv[:, 2:3], mv[:, 1:2], r_mv)
            S.op("dve", lambda: nc.vector.tensor_scalar(out=outt[:], in0=u[:], scalar1=mv[:, 0:1], scalar2=mv[:, 2:3],
                                                        op0=ALU.subtract, op1=ALU.mult), [r_u, r_mv], [r_out])
            S.op("pool", lambda: nc.gpsimd.tensor_tensor(out=outt[:], in0=outt[:], in1=gt[:], op=ALU.mult),
                 [r_out, r_gt], [r_out])
            S.op("pool", lambda: nc.gpsimd.tensor_tensor(out=outt[:], in0=outt[:], in1=bt[:], op=ALU.add),
                 [r_out, r_bt], [r_out])

        def head_norm(o_ap, nh, st, r_st, mv, r_mv, outt, r_out, r_o, col0):
            for h in range(nh):
                S.op("dve", lambda h=h: nc.vector.bn_stats(out=st[:, h, :], in_=o_ap(h)), [r_o], [r_st])
                S.op("dve", lambda h=h: nc.vector.bn_aggr(out=mv[:, h, 0:2], in_=st[:, h, :]), [r_st], [r_mv])
            rstd(mv[:, 0:nh, 2:3], mv[:, 0:nh, 1:2], r_mv)
            for h in range(nh):
                S.op("dve", lambda h=h: nc.vector.tensor_scalar(
                    out=outt[:, col0 + h * 128: col0 + (h + 1) * 128], in0=o_ap(h), scalar1=mv[:, h, 0:1],
                    scalar2=mv[:, h, 2:3], op0=ALU.subtract, op1=ALU.mult), [r_o, r_mv], [r_out])

        XCUR = x_in
        for l in range(n_layers):
            XNEXT = y_out if l == n_layers - 1 else (XA if l % 2 == 0 else XB_)
            with ExitStack() as es:
                xT, r_xT = mk(es, "xT", [128, 16, SEQ], BF16)
                xb = mk(es, "xb", [128, DM], BF16, 2)
                wt = mk(es, "wt", [128, 16, 512], BF16, 2)
                stg = mk(es, "stg", [128, 4, 512], BF16, 2)
                stgf, r_stgf = mk(es, "stgf", [16, 512], F32)
                tp = mkp(es, "tp", [128, 512], BF16, 2)
                ps = mkp(es, "ps", [128, 512], F32, 4)
                for j in range(NT):
                    xbt, r_xb = xb.next()
                    dma("pool", xbt[:], XCUR[j * 128:(j + 1) * 128, :], writes=[r_xb])
                    for g4 in range(4):
                        tpt, r_tp = tp.next()
                        for k in range(4):
                            kc = g4 * 4 + k
                            S.op("pe", lambda kc=kc, k=k, tpt=tpt, xbt=xbt: nc.tensor.transpose(
                                out=tpt[:, k * 128:(k + 1) * 128], in_=xbt[:, kc * 128:(kc + 1) * 128], identity=ident_b[:]),
                                [r_xb, r_identb], [r_tp], signal=(k == 3))
                        eng = alt("act", "dve")
                        S.op(eng, lambda eng=eng, tpt=tpt, g4=g4, j=j: ecopy(
                            eng, xT[:, g4 * 4:(g4 + 1) * 4, j * 128:(j + 1) * 128],
                            tpt[:].rearrange("p (k t) -> p k t", k=4)), [r_tp], [r_xT])
                if stop_phase >= 1:
                    tm_tiles = [(c0, c0) for c0 in range(0, 2048, 512)] + [(c0, c0 - 1536) for c0 in range(3584, 6656, 512)]
                    for (wc, pc) in tm_tiles:
                        wtt, r_wt = wt.next()
                        dma("pool", wtt[:], w_in[l, :, wc:wc + 512].rearrange("(k p) n -> p k n", p=128), writes=[r_wt])
                        for j4 in range(NT // 4):
                            sg, r_sg = stg.next()
                            for jj in range(4):
                                j = j4 * 4 + jj
                                pst, r_ps = ps.next()
                                for kc in range(16):
                                    S.op("pe", lambda kc=kc, pst=pst, j=j, wtt=wtt: nc.tensor.matmul(
                                        pst[:], lhsT=xT[:, kc, j * 128:(j + 1) * 128], rhs=wtt[:, kc, :],
                                        start=(kc == 0), stop=(kc == 15)), [r_xT, r_wt], [r_ps], signal=(kc == 15))
                                eng = alt("act", "dve")
                                S.op(eng, lambda eng=eng, sg=sg, jj=jj, pst=pst: ecopy(eng, sg[:, jj, :], pst[:]), [r_ps], [r_sg])
                            dma("sp", P[j4 * 512:(j4 + 1) * 512, pc:pc + 512].rearrange("(j p) n -> p j n", p=128), sg[:],
                                reads=[r_sg])
                    for g in range(3):
                        wtt, r_wt = wt.next()
                        wc = 2048 + g * 512
                        dma("pool", wtt[:], w_in[l, :, wc:wc + 512].rearrange("(k p) n -> p k n", p=128), writes=[r_wt])
                        for cc in range(4):
                            row0 = g * 512 + cc * 128
                            for s4 in range(2):
                                sg, r_sg = stg.next()
                                for ss in range(4):
                                    s = s4 * 4 + ss
                                    pst, r_ps = ps.next()
                                    for kc in range(16):
                                        S.op("pe", lambda kc=kc, pst=pst, s=s, wtt=wtt, cc=cc: nc.tensor.matmul(
                                            pst[:], lhsT=wtt[:, kc, cc * 128:(cc + 1) * 128], rhs=xT[:, kc, s * 512:(s + 1) * 512],
                                            start=(kc == 0), stop=(kc == 15)), [r_xT, r_wt], [r_ps], signal=(kc == 15))
                                    eng = alt("act", "dve")
                                    if row0 < 768:
                                        if eng == "act":
                                            S.op(eng, lambda sg=sg, ss=ss, pst=pst: nc.scalar.activation(out=sg[:, ss, :], in_=pst[:], func=AF.Copy, scale=float(128 ** -0.5)), [r_ps], [r_sg])
                                        else:
                                            S.op(eng, lambda sg=sg, ss=ss, pst=pst: nc.vector.tensor_scalar(out=sg[:, ss, :], in0=pst[:], scalar1=float(128 ** -0.5), scalar2=None, op0=ALU.mult), [r_ps], [r_sg])
                                    else:
                                        S.op(eng, lambda eng=eng, sg=sg, ss=ss, pst=pst: ecopy(eng, sg[:, ss, :], pst[:]), [r_ps], [r_sg])
                                dma("sp", PT[row0:row0 + 128, s4 * 2048:(s4 + 1) * 2048], sg[:].rearrange("p a b -> p (a b)"),
                                    reads=[r_sg])
                    wtt, r_wt = wt.next()
                    dma("pool", wtt[:, :, 0:16], w_in[l, :, 6656:6672].rearrange("(k p) n -> p k n", p=128), writes=[r_wt])
                    for s in range(8):
                        pst, r_ps = ps.next()
                        for kc in range(16):
                            S.op("pe", lambda kc=kc, pst=pst, s=s, wtt=wtt: nc.tensor.matmul(
                                pst[0:16, :], lhsT=wtt[:, kc, 0:16], rhs=xT[:, kc, s * 512:(s + 1) * 512],
                                start=(kc == 0), stop=(kc == 15)), [r_xT, r_wt], [r_ps], signal=(kc == 15))
                        S.op("act", lambda pst=pst: nc.scalar.copy(out=stgf[:], in_=pst[0:16, :]), [r_ps], [r_stgf])
                        dma("sp", GAT[:, s * 512:(s + 1) * 512], stgf[:], reads=[r_stgf])
                S.barrier()
            if stop_phase <= 1:
                break

            with ExitStack() as es:
                rope = mk(es, "rope", [128, 4, 256], F32, 2)
                qk = mk(es, "qk", [128, 1024], BF16, 2)
                rv = mk(es, "rv", [128, 512], BF16, 2)
                rg = mk(es, "rg", [128, 512], BF16, 2)
                qkr_ring = mk(es, "qkr", [128, 1024], BF16, 2)
                t1_ring = mk(es, "t1", [128, 4, 64], F32, 2)
                t2_ring = mk(es, "t2", [128, 4, 64], F32, 2)
                t3_ring = mk(es, "t3", [128, 4, 64], F32, 2)
                t4_ring = mk(es, "t4", [128, 4, 64], F32, 2)
                qkT_ring = mk(es, "qkT", [128, 8, 128], BF16, 2)
                sm = mk(es, "sm", [128, 128], BF16, 2)
                Sf = [mk(es, "Sf%d" % h, [128, 128], F32) for h in range(4)]
                Sb = [mk(es, "Sb%d" % h, [128, 128], BF16) for h in range(4)]
                Tt, r_Tt = mk(es, "Tt", [128, 128], F32)
                st_ring = mk(es, "st", [128, 4, 6], F32, 2)
                mv_ring = mk(es, "mv", [128, 4, 3], F32, 2)
                nrm_ring = mk(es, "nrm", [128, 512], F32, 2)
                sil_ring = mk(es, "sil", [128, 512], F32, 2)
                mixo = mk(es, "mixo", [128, 512], BF16, 2)
                gvec, r_gvec = mk(es, "gvec", [128, 512], F32)
                tp_ring = mkp(es, "tp2", [128, 8, 128], BF16, 2)
                sT = mkp(es, "sT", [128, 128], F32, 2)
                po_ring = mkp(es, "po", [128, 512], F32, 2)
                kv = mkp(es, "kv", [128, 128], F32, 2)
                dma("sp", gvec[:], ret_g[l:l + 1, :].partition_broadcast(128), writes=[r_gvec])
                for h in range(4):
                    S.op("dve", lambda h=h: nc.vector.memset(Sf[h][0][:], 0.0), [], [Sf[h][1]])
                    S.op("dve", lambda h=h: nc.vector.memset(Sb[h][0][:], 0.0), [], [Sb[h][1]])
                def tile2(j):
                    rows = slice(j * 128, (j + 1) * 128)
                    qkr, r_qkr = qkr_ring.next()
                    t1, r_t1 = t1_ring.next()
                    t2, r_t2 = t2_ring.next()
                    t3, r_t3 = t3_ring.next()
                    t4, r_t4 = t4_ring.next()
                    qkT, r_qkT = qkT_ring.next()
                    st, r_st = st_ring.next()
                    mv, r_mv = mv_ring.next()
                    nrm, r_nrm = nrm_ring.next()
                    sil, r_sil = sil_ring.next()
                    tp, r_tp = tp_ring.next()
                    po, r_po = po_ring.next()
                    ropt, r_rop = rope.next()
                    dma("sp", ropt[:], c_rope[:, rows, :].rearrange("a p n -> p a n"), writes=[r_rop])
                    qkt, r_qk = qk.next()
                    dma("sp", qkt[:], P[rows, 0:1024], writes=[r_qk])
                    rvt, r_rv = rv.next()
                    dma("sp", rvt[:], P[rows, 1024:1536], writes=[r_rv])
                    rgt, r_rg = rg.next()
                    dma("sp", rgt[:], P[rows, 1536:2048], writes=[r_rg])
                    for qi in range(2):
                        src = qkt[:, qi * 512:(qi + 1) * 512].rearrange("p (h d) -> p h d", h=4)
                        dst = qkr[:, qi * 512:(qi + 1) * 512].rearrange("p (h d) -> p h d", h=4)
                        a1, a2 = src[:, :, 0:64], src[:, :, 64:128]
                        cosv = ropt[:, 2 * qi, :].rearrange("p (h d) -> p h d", h=4)
                        sinv = ropt[:, 2 * qi + 1, :].rearrange("p (h d) -> p h d", h=4)
                        e1 = "dve" if qi == 0 else "pool"
                        S.op(e1, lambda e1=e1, a1=a1, cosv=cosv: E[e1].tensor_tensor(out=t1[:], in0=a1, in1=cosv, op=ALU.mult), [r_qk, r_rop], [r_t1])
                        S.op(e1, lambda e1=e1, a2=a2, sinv=sinv: E[e1].tensor_tensor(out=t2[:], in0=a2, in1=sinv, op=ALU.mult), [r_qk, r_rop], [r_t2])
                        S.op(e1, lambda e1=e1, dst=dst: E[e1].tensor_tensor(out=dst[:, :, 0:64], in0=t1[:], in1=t2[:], op=ALU.subtract), [r_t1, r_t2], [r_qkr])
                        S.op(e1, lambda e1=e1, a1=a1, sinv=sinv: E[e1].tensor_tensor(out=t3[:], in0=a1, in1=sinv, op=ALU.mult), [r_qk, r_rop], [r_t3])
                        S.op(e1, lambda e1=e1, a2=a2, cosv=cosv: E[e1].tensor_tensor(out=t4[:], in0=a2, in1=cosv, op=ALU.mult), [r_qk, r_rop], [r_t4])
                        S.op(e1, lambda e1=e1, dst=dst: E[e1].tensor_tensor(out=dst[:, :, 64:128], in0=t3[:], in1=t4[:], op=ALU.add), [r_t3, r_t4], [r_qkr])
                    for k in range(8):
                        S.op("pe", lambda k=k: nc.tensor.transpose(out=tp[:, k, :], in_=qkr[:, k * 128:(k + 1) * 128], identity=ident_b[:]),
                             [r_qkr, r_identb], [r_tp], signal=(k == 7))
                    S.op("act", lambda: nc.scalar.copy(out=qkT[:], in_=tp[:]), [r_tp], [r_qkT])
                    S.op("act", lambda rgt=rgt: nc.scalar.activation(out=sil[:], in_=rgt[:], func=AF.Silu), [r_rg], [r_sil])
                    yield
                    for h in range(4):
                        sTt, r_sT = sT.next()
                        S.op("pe", lambda h=h, sTt=sTt: nc.tensor.matmul(sTt[:], lhsT=qkT[:, 4 + h, :], rhs=qkT[:, h, :], start=True, stop=True),
                             [r_qkT], [r_sT])
                        smt, r_sm = sm.next()
                        S.op("dve", lambda sTt=sTt, smt=smt: nc.vector.tensor_tensor(out=smt[:], in0=sTt[:], in1=mask01[:], op=ALU.mult),
                             [r_sT, r_mask01], [r_sm])
                        S.op("pe", lambda h=h, smt=smt, rvt=rvt: nc.tensor.matmul(po[:, h * 128:(h + 1) * 128], lhsT=smt[:], rhs=rvt[:, h * 128:(h + 1) * 128],
                                                                         start=True, stop=False), [r_sm, r_rv], [r_po], signal=False)
                        S.op("pe", lambda h=h: nc.tensor.matmul(po[:, h * 128:(h + 1) * 128], lhsT=qkT[:, h, :], rhs=Sb[h][0][:],
                                                                start=False, stop=True), [r_qkT, Sb[h][1]], [r_po])
                        kvt, r_kv = kv.next()
                        S.op("pe", lambda h=h, kvt=kvt, rvt=rvt: nc.tensor.matmul(kvt[:], lhsT=qkr[:, 512 + h * 128:512 + (h + 1) * 128], rhs=rvt[:, h * 128:(h + 1) * 128],
                                                                         start=True, stop=True), [r_qkr, r_rv], [r_kv])
                        S.op("dve", lambda h=h, kvt=kvt: nc.vector.tensor_tensor(out=Tt[:], in0=Sf[h][0][:], in1=kvt[:], op=ALU.add),
                             [Sf[h][1], r_kv], [r_Tt])
                        S.op("act", lambda h=h: nc.scalar.activation(out=Sf[h][0][:], in_=Tt[:], func=AF.Copy, scale=g128[h]), [r_Tt], [Sf[h][1]])
                        S.op("act", lambda h=h: nc.scalar.activation(out=Sb[h][0][:], in_=Tt[:], func=AF.Copy, scale=g128[h]), [r_Tt], [Sb[h][1]])
                    yield
                    head_norm(lambda h: po[:, h * 128:(h + 1) * 128], 4, st, r_st, mv, r_mv, nrm, r_nrm, r_po, 0)
                    S.op("pool", lambda: nc.gpsimd.tensor_tensor(out=nrm[:], in0=nrm[:], in1=gvec[:], op=ALU.mult), [r_nrm, r_gvec], [r_nrm])
                    mo, r_mo = mixo.next()
                    S.op("pool", lambda mo=mo: nc.gpsimd.tensor_tensor(out=mo[:], in0=nrm[:], in1=sil[:], op=ALU.mult), [r_nrm, r_sil], [r_mo])
                    dma("sp", MIX[rows, 0:512], mo[:], reads=[r_mo])
                run_skewed(tile2, NT)
                S.barrier()
            if stop_phase <= 2:
                break

            with ExitStack() as es:
                wg, r_wg = mk(es, "wgg", [16, 384], F32)
                bg, r_bg = mk(es, "bgg", [1, 384], F32)
                gvec, r_gvec = mk(es, "gvec3", [128, 768], F32)
                gat = mk(es, "gat", [16, 128], F32, 2)
                gqk = mk(es, "gqk", [128, 768], BF16, 2)
                gv = mk(es, "gv", [128, 768], BF16, 2)
                gr = mk(es, "gr", [128, 768], BF16, 2)
                ez_ring = mk(es, "ez", [128, 384], F32, 2)
                lz_ring = mk(es, "lz", [128, 384], F32, 2)
                eb_ring = mk(es, "eb", [128, 384], F32, 2)
                enb_ring = mk(es, "enb", [128, 384], F32, 2)
                qkh_ring = mk(es, "qkh", [128, 768], BF16, 2)
                qkT_ring = mk(es, "qkT3", [128, 6, 128], BF16, 2)
                dec_ring = mk(es, "dec", [128, 4], F32, 2)
                sm = mk(es, "sm3", [128, 128], BF16, 2)
                Sf = [mk(es, "Sg%d" % p_, [128, 256], F32) for p_ in range(3)]
                Sb = [mk(es, "Sgb%d" % p_, [128, 256], BF16) for p_ in range(3)]
                Tt, r_Tt = mk(es, "Tt3", [128, 256], F32)
                st_ring = mk(es, "st3", [128, 6, 6], F32, 2)
                mv_ring = mk(es, "mv3", [128, 6, 3], F32, 2)
                nrm_ring = mk(es, "nrm3", [128, 768], F32, 2)
                sil_ring = mk(es, "sil3", [128, 768], F32, 2)
                mixo = mk(es, "mixo3", [128, 768], BF16, 2)
                pz, r_pz = mkp(es, "pz", [128, 512], F32)
                pl, r_pl = mkp(es, "pl", [128, 512], F32)
                tp, r_tp = mkp(es, "tp3", [128, 8, 128], BF16)
                pm, r_pm = mkp(es, "pm3", [128, 512], F32)
                poA, r_poA = mkp(es, "poA", [128, 512], F32)
                poB, r_poB = mkp(es, "poB", [128, 512], F32)
                pkv, r_pkv = mkp(es, "pkv", [128, 512], F32)
                r_sT = [Res(), Res()]
                r_bl = Res()
                r_kvh = [Res(), Res()]
                dma("sp", wg[:], w_gg[l], writes=[r_wg])
                dma("sp", bg[:], b_gg[l:l + 1, :], writes=[r_bg])
                dma("sp", gvec[:], gla_g[l:l + 1, :].partition_broadcast(128), writes=[r_gvec])
                for p_ in range(3):
                    S.op("dve", lambda p_=p_: nc.vector.memset(Sf[p_][0][:], 0.0), [], [Sf[p_][1]])
                    S.op("dve", lambda p_=p_: nc.vector.memset(Sb[p_][0][:], 0.0), [], [Sb[p_][1]])

                def po_ap(h):
                    return poA[:, h * 128:(h + 1) * 128] if h < 4 else poB[:, (h - 4) * 128:(h - 3) * 128]

                def r_poh(h):
                    return r_poA if h < 4 else r_poB
                def tile3(j):
                    rows = slice(j * 128, (j + 1) * 128)
                    ez, r_ez = ez_ring.next()
                    lz, r_lz = lz_ring.next()
                    eb, r_eb = eb_ring.next()
                    enb, r_enb = enb_ring.next()
                    qkh, r_qkh = qkh_ring.next()
                    qkT, r_qkT = qkT_ring.next()
                    dec, r_dec = dec_ring.next()
                    st, r_st = st_ring.next()
                    mv, r_mv = mv_ring.next()
                    nrm, r_nrm = nrm_ring.next()
                    sil, r_sil = sil_ring.next()
                    gatt, r_gat = gat.next()
                    dma("sp", gatt[:], GAT[:, rows], writes=[r_gat])
                    gqkt, r_gqk = gqk.next()
                    dma("sp", gqkt[:], P[rows, 2816:3584], writes=[r_gqk])
                    gvt, r_gv = gv.next()
                    dma("sp", gvt[:], P[rows, 3584:4352], writes=[r_gv])
                    grt, r_gr = gr.next()
                    dma("sp", grt[:], P[rows, 4352:5120], writes=[r_gr])
                    S.op("pe", lambda gatt=gatt: nc.tensor.matmul(pz[:, 0:384], lhsT=gatt[:], rhs=wg[:], start=True, stop=False),
                         [r_gat, r_wg], [r_pz], signal=False)
                    S.op("pe", lambda: nc.tensor.matmul(pz[:, 0:384], lhsT=ones_f[0:1, :], rhs=bg[:], start=False, stop=True),
                         [r_onesf, r_bg], [r_pz])
                    S.op("act", lambda: nc.scalar.activation(out=ez[:], in_=pz[:, 0:384], func=AF.Exp, scale=-1.0), [r_pz], [r_ez])
                    S.op("act", lambda: nc.scalar.activation(out=lz[:], in_=ez[:], func=AF.Ln, bias=1.0, scale=1.0), [r_ez], [r_lz])
                    S.op("pe", lambda: nc.tensor.matmul(pl[:, 0:384], lhsT=mask01[:], rhs=lz[:], start=True, stop=True),
                         [r_mask01, r_lz], [r_pl])
                    for p_ in range(3):
                        S.op("pe", lambda p_=p_: nc.tensor.matmul(pm[:, 256 + p_:257 + p_], lhsT=lz[:, p_ * 128:(p_ + 1) * 128], rhs=ones_f[:, 0:1],
                                                                  start=True, stop=True), [r_lz, r_onesf], [r_bl], signal=(p_ == 2))
                    S.op("act", lambda: nc.scalar.activation(out=eb[:], in_=pl[:, 0:384], func=AF.Exp, scale=-1.0 / 16.0), [r_pl], [r_eb])
                    S.op("act", lambda: nc.scalar.activation(out=enb[:], in_=pl[:, 0:384], func=AF.Exp, scale=1.0 / 16.0), [r_pl], [r_enb])
                    S.op("act", lambda: nc.scalar.activation(out=dec[:, 0:3], in_=pm[:, 256:259], func=AF.Exp, scale=-1.0 / 16.0), [r_bl], [r_dec])
                    S.op("dve", lambda gqkt=gqkt: nc.vector.scalar_tensor_tensor(out=qkh[:, 0:384], in0=gqkt[:, 0:384], scalar=0.125, in1=eb[:],
                                                                                op0=ALU.mult, op1=ALU.mult), [r_gqk, r_eb], [r_qkh])
                    S.op("dve", lambda gqkt=gqkt: nc.vector.tensor_tensor(out=qkh[:, 384:768], in0=gqkt[:, 384:768], in1=enb[:], op=ALU.mult),
                         [r_gqk, r_enb], [r_qkh])
                    for k in range(6):
                        S.op("pe", lambda k=k: nc.tensor.transpose(out=tp[:, k, :], in_=qkh[:, k * 128:(k + 1) * 128], identity=ident_b[:]),
                             [r_qkh, r_identb], [r_tp], signal=(k == 5))
                    S.op("act", lambda: nc.scalar.copy(out=qkT[:], in_=tp[:, 0:6, :]), [r_tp], [r_qkT])
                    S.op("act", lambda grt=grt: nc.scalar.activation(out=sil[:], in_=grt[:], func=AF.Silu), [r_gr], [r_sil])
                    yield
                    for h in range(6):
                        p_, hh = h // 2, h % 2
                        R = slice(hh * 64, (hh + 1) * 64)
                        sTa = pm[:, (h % 2) * 128:(h % 2 + 1) * 128]
                        rs = r_sT[h % 2]
                        S.op("pe", lambda p_=p_, R=R, sTa=sTa: nc.tensor.matmul(sTa, lhsT=qkT[R, 3 + p_, :], rhs=qkT[R, p_, :], start=True, stop=True),
                             [r_qkT], [rs])
                        smt, r_sm = sm.next()
                        S.op("dve", lambda sTa=sTa, smt=smt: nc.vector.tensor_tensor(out=smt[:], in0=sTa, in1=mask01[:], op=ALU.mult),
                             [rs, r_mask01], [r_sm])
                        S.op("pe", lambda h=h, smt=smt, gvt=gvt: nc.tensor.matmul(po_ap(h), lhsT=smt[:], rhs=gvt[:, h * 128:(h + 1) * 128],
                                                                         start=True, stop=False), [r_sm, r_gv], [r_poh(h)], signal=False)
                        S.op("pe", lambda h=h, p_=p_, R=R, hh=hh: nc.tensor.matmul(po_ap(h), lhsT=qkT[R, p_, :], rhs=Sb[p_][0][R, hh * 128:(hh + 1) * 128],
                                                                          start=False, stop=True), [r_qkT, Sb[p_][1]], [r_poh(h)])
                    for p_ in range(3):
                        kva = pkv[:, (p_ % 2) * 256:(p_ % 2 + 1) * 256]
                        rk = r_kvh[p_ % 2]
                        S.op("pe", lambda p_=p_, kva=kva, gvt=gvt: nc.tensor.matmul(kva, lhsT=qkh[:, 384 + p_ * 128:384 + (p_ + 1) * 128], rhs=gvt[:, p_ * 256:(p_ + 1) * 256],
                                                                          start=True, stop=True), [r_qkh, r_gv], [rk])
                        S.op("dve", lambda p_=p_, kva=kva: nc.vector.tensor_tensor(out=Tt[:], in0=Sf[p_][0][:], in1=kva, op=ALU.add),
                             [Sf[p_][1], rk], [r_Tt])
                        S.op("act", lambda p_=p_: nc.scalar.activation(out=Sf[p_][0][:], in_=Tt[:], func=AF.Copy, scale=dec[:, p_:p_ + 1]), [r_Tt, r_dec], [Sf[p_][1]])
                        S.op("act", lambda p_=p_: nc.scalar.activation(out=Sb[p_][0][:], in_=Tt[:], func=AF.Copy, scale=dec[:, p_:p_ + 1]), [r_Tt, r_dec], [Sb[p_][1]])
                    yield
                    r_pob = Res()
                    for h in range(6):
                        S.op("dve", lambda h=h: nc.vector.bn_stats(out=st[:, h, :], in_=po_ap(h)), [r_poh(h)], [r_st])
                        S.op("dve", lambda h=h: nc.vector.bn_aggr(out=mv[:, h, 0:2], in_=st[:, h, :]), [r_st], [r_mv])
                    rstd(mv[:, :, 2:3], mv[:, :, 1:2], r_mv)
                    for h in range(6):
                        S.op("dve", lambda h=h: nc.vector.tensor_scalar(out=nrm[:, h * 128:(h + 1) * 128], in0=po_ap(h), scalar1=mv[:, h, 0:1],
                                                                        scalar2=mv[:, h, 2:3], op0=ALU.subtract, op1=ALU.mult),
                             [r_poh(h), r_mv], [r_nrm])
                    S.op("pool", lambda: nc.gpsimd.tensor_tensor(out=nrm[:], in0=nrm[:], in1=gvec[:], op=ALU.mult), [r_nrm, r_gvec], [r_nrm])
                    mo, r_mo = mixo.next()
                    S.op("pool", lambda mo=mo: nc.gpsimd.tensor_tensor(out=mo[:], in0=nrm[:], in1=sil[:], op=ALU.mult), [r_nrm, r_sil], [r_mo])
                    dma("sp", MIX[rows, 1280:2048], mo[:], reads=[r_mo])
                run_skewed(tile3, NT)
                S.barrier()
            if stop_phase <= 3:
                break

            with ExitStack() as es:
                QT, r_QT = mk(es, "QT", [128, 6, SEQ], BF16)
                KT, r_KT = mk(es, "KT", [128, 6, SEQ], BF16)
                dm0, r_dm0 = mk(es, "dm0", [128, 256], F32)
                dm1, r_dm1 = mk(es, "dm1", [128, 256], F32)
                mb0, r_mb0 = mk(es, "mb0", [128, 256], BF16)
                mb1, r_mb1 = mk(es, "mb1", [128, 256], BF16)
                V = mk(es, "V", [128, 768], BF16, 5)
                negm = mk(es, "negm", [128, 3], F32, 3)
                pexp = mk(es, "pexp", [128, 3, 256], BF16, 3)
                pTs = mk(es, "pTs", [128, 3, 256], BF16, 3)
                stage = Ring([(es.enter_context(nc.sbuf_tensor("dstage%d_%d" % (k, l), [128, 6, 130], F32)), [Res() for _ in range(2)]) for k in range(4)])
                ps_s = mkp(es, "ps_s", [128, 4, 256], F32, 2)
                ps_t = mkp(es, "ps_t", [128, 4, 256], BF16, 2)
                ps_o = mkp(es, "ps_o", [128, 4, 128], F32, 2)
                dma("sp", dm0[:], c_dmask0, writes=[r_dm0])
                dma("sp", dm1[:], c_dmask1, writes=[r_dm1])
                S.op("dve", lambda: nc.vector.tensor_copy(out=mb0[:], in_=dm0[:]), [r_dm0], [r_mb0])
                S.op("dve", lambda: nc.vector.tensor_copy(out=mb1[:], in_=dm1[:]), [r_dm1], [r_mb1])
                for h in range(6):
                    dma("sp", QT[:, h, :], PT[h * 128:(h + 1) * 128, :], writes=[r_QT])
                    dma("sp", KT[:, h, :], PT[768 + h * 128:768 + (h + 1) * 128, :], writes=[r_KT])
                batches = []
                for pi, dil in enumerate((1, 4, 16)):
                    nb = SEQ // (dil * 128)
                    for r in range(dil):
                        for n in range(nb):
                            for hb in range(2):
                                batches.append((pi, dil, r, n, hb))
                ctxs = {}
                ust = {}

                def sA(b):
                    pi, dil, r, n, hb = batches[b]
                    c = ctxs[b] = {}
                    row0 = n * 128 * dil + r
                    rsl = slice(row0, row0 + 127 * dil + 1, dil)
                    c["rsl"] = rsl
                    if hb == 0:
                        vt, r_v = V.next()
                        dma("sp", vt[:], P[rsl, 2048:2816], writes=[r_v])
                        vprev = ust.get("vprev") if n > 0 else (vt, r_v)
                        stg, r_stg = stage.next()
                        ust["cur"] = (vt, r_v, vprev, stg, r_stg)
                        ust["vprev"] = (vt, r_v)
                    c["u"] = ust["cur"]
                    h0 = hb * 3
                    pst, r_ps = ps_s.next()
                    c["pst"] = (pst, r_ps)
                    for hh in range(3):
                        h = h0 + hh
                        q_ap = QT[:, h, rsl]
                        if n == 0:
                            S.op("pe", lambda: nc.tensor.matmul(pst[:, hh, 128:256], lhsT=q_ap, rhs=KT[:, h, rsl], start=True, stop=False),
                                 [r_QT, r_KT], [r_ps], signal=False)
                            S.op("pe", lambda: nc.tensor.matmul(pst[:, hh, :], lhsT=ident_b[:], rhs=mb0[:], start=False, stop=True),
                                 [r_identb, r_mb0], [r_ps], signal=(hh == 2))
                        else:
                            ksl = slice(row0 - 128 * dil, row0 + 127 * dil + 1, dil)
                            S.op("pe", lambda: nc.tensor.matmul(pst[:, hh, :], lhsT=q_ap, rhs=KT[:, h, ksl], start=True, stop=False),
                                 [r_QT, r_KT], [r_ps], signal=False)
                            S.op("pe", lambda: nc.tensor.matmul(pst[:, hh, :], lhsT=ident_b[:], rhs=mb1[:], start=False, stop=True),
                                 [r_identb, r_mb1], [r_ps], signal=(hh == 2))

                def sB(b):
                    pi, dil, r, n, hb = batches[b]
                    c = ctxs[b]
                    h0 = hb * 3
                    pst, r_ps = c["pst"]
                    vt, r_v, vprev, stg, r_stg = c["u"]
                    S.op("dve", lambda: nc.vector.reduce_max(out=stg[:, h0:h0 + 3, 128], in_=pst[:, 0:3, :], axis=AX.X), [r_ps], [r_stg[hb]])
                    ngt, r_ng = negm.next()
                    S.op("dve", lambda: nc.vector.tensor_scalar(out=ngt[:], in0=stg[:, h0:h0 + 3, 128], scalar1=-1.0, scalar2=None, op0=ALU.mult),
                         [r_stg[hb]], [r_ng])
                    pet, r_pe = pexp.next()
                    c["pet"] = (pet, r_pe)
                    for hh in range(3):
                        S.op("act", lambda: nc.scalar.activation(out=pet[:, hh, :], in_=pst[:, hh, :], func=AF.Exp, bias=ngt[:, hh:hh + 1], scale=1.0,
                                                                 accum_out=stg[:, h0 + hh, 129:130]),
                             [r_ps, r_ng], [r_pe, r_stg[hb]])

                def sC(b):
                    c = ctxs[b]
                    pet, r_pe = c["pet"]
                    ptt, r_pt = ps_t.next()
                    for hh in range(3):
                        for kk in range(2):
                            S.op("pe", lambda: nc.tensor.transpose(out=ptt[:, hh, kk * 128:(kk + 1) * 128], in_=pet[:, hh, kk * 128:(kk + 1) * 128], identity=ident_b[:]),
                                 [r_pe, r_identb], [r_pt], signal=(hh == 2 and kk == 1))
                    pts, r_pts = pTs.next()
                    c["pts"] = (pts, r_pts)
                    S.op("dve", lambda: nc.vector.tensor_copy(out=pts[:], in_=ptt[:, 0:3, :]), [r_pt], [r_pts])

                def sD(b):
                    pi, dil, r, n, hb = batches[b]
                    c = ctxs.pop(b)
                    h0 = hb * 3
                    pts, r_pts = c["pts"]
                    vt, r_v, vprev, stg, r_stg = c["u"]
                    pot, r_po2 = ps_o.next()
                    for hh in range(3):
                        h = h0 + hh
                        S.op("pe", lambda: nc.tensor.matmul(pot[:, hh, :], lhsT=pts[:, hh, 0:128], rhs=vprev[0][:, h * 128:(h + 1) * 128], start=True, stop=False),
                             [r_pts, vprev[1]], [r_po2], signal=False)
                        S.op("pe", lambda: nc.tensor.matmul(pot[:, hh, :], lhsT=pts[:, hh, 128:256], rhs=vt[:, h * 128:(h + 1) * 128], start=False, stop=True),
                             [r_pts, r_v], [r_po2], signal=(hh == 2))
                    S.op("act", lambda: nc.scalar.copy(out=stg[:, h0:h0 + 3, 0:128], in_=pot[:, 0:3, :]), [r_po2], [r_stg[hb]])
                    if hb == 1:
                        dma("sp", DO[pi, c["rsl"], :], stg[:].rearrange("p a b -> p (a b)"), reads=r_stg)

                nbt = len(batches)
                for t in range(nbt + 3):
                    if 0 <= t - 3 < nbt:
                        sD(t - 3)
                    if 0 <= t - 2 < nbt:
                        sC(t - 2)
                    if 0 <= t - 1 < nbt:
                        sB(t - 1)
                    if t < nbt:
                        sA(t)
                S.barrier()
            with ExitStack() as es:
                D3 = mk(es, "D3", [128, 3, 780], F32, 2)
                mxx, r_mxx = mk(es, "mxx", [128, 6], F32)
                e3, r_e3 = mk(es, "e3", [128, 3, 6], F32)
                w3, r_w3 = mk(es, "w3", [128, 3, 6], F32)
                dn, r_dn = mk(es, "dn", [128, 6], F32)
                cf, r_cf = mk(es, "cf", [128, 3, 6], F32)
                acc = [mk(es, "dacc%d" % h, [128, 128], F32) for h in range(6)]
                outb = Ring([(es.enter_context(nc.sbuf_tensor("doutb%d_%d" % (k, l), [128, 768], BF16)), [Res() for _ in range(6)]) for k in range(2)])
                for j in range(NT):
                    rows = slice(j * 128, (j + 1) * 128)
                    d3, r_d3 = D3.next()
                    dma("sp", d3[:], DO[:, rows, :].rearrange("a p n -> p a n"), writes=[r_d3])
                    d4 = d3[:].rearrange("p a (h c) -> p a h c", h=6)
                    S.op("dve", lambda d4=d4: nc.vector.tensor_tensor(out=mxx[:], in0=d4[:, 0, :, 128], in1=d4[:, 1, :, 128], op=ALU.max), [r_d3], [r_mxx])
                    S.op("dve", lambda d4=d4: nc.vector.tensor_tensor(out=mxx[:], in0=mxx[:], in1=d4[:, 2, :, 128], op=ALU.max), [r_d3, r_mxx], [r_mxx])
                    for p_ in range(3):
                        S.op("dve", lambda d4=d4, p_=p_: nc.vector.tensor_tensor(out=e3[:, p_, :], in0=d4[:, p_, :, 128], in1=mxx[:], op=ALU.subtract), [r_d3, r_mxx], [r_e3])
                    S.op("act", lambda: nc.scalar.activation(out=e3[:], in_=e3[:], func=AF.Exp), [r_e3], [r_e3])
                    for p_ in range(3):
                        S.op("dve", lambda d4=d4, p_=p_: nc.vector.tensor_tensor(out=w3[:, p_, :], in0=e3[:, p_, :], in1=d4[:, p_, :, 129], op=ALU.mult), [r_d3, r_e3], [r_w3])
                    S.op("dve", lambda: nc.vector.tensor_tensor(out=dn[:], in0=w3[:, 0, :], in1=w3[:, 1, :], op=ALU.add), [r_w3], [r_dn])
                    S.op("dve", lambda: nc.vector.tensor_tensor(out=dn[:], in0=dn[:], in1=w3[:, 2, :], op=ALU.add), [r_w3, r_dn], [r_dn])
                    S.op("dve", lambda: nc.vector.reciprocal(out=dn[:], in_=dn[:]), [r_dn], [r_dn])
                    for p_ in range(3):
                        S.op("dve", lambda p_=p_: nc.vector.tensor_tensor(out=cf[:, p_, :], in0=e3[:, p_, :], in1=dn[:], op=ALU.mult), [r_e3, r_dn], [r_cf])
                    ob, r_ob = outb.next()
                    for h in range(6):
                        eng = "dve"
                        at, r_at = acc[h]
                        S.op(eng, lambda eng=eng, at=at, d4=d4, h=h: E[eng].tensor_scalar(out=at[:], in0=d4[:, 0, h, 0:128], scalar1=cf[:, 0, h:h + 1], scalar2=None, op0=ALU.mult),
                             [r_d3, r_cf], [r_at])
                        S.op(eng, lambda eng=eng, at=at, d4=d4, h=h: E[eng].scalar_tensor_tensor(out=at[:], in0=d4[:, 1, h, 0:128], scalar=cf[:, 1, h:h + 1], in1=at[:], op0=ALU.mult, op1=ALU.add),
                             [r_d3, r_cf, r_at], [r_at])
                        S.op(eng, lambda eng=eng, at=at, d4=d4, h=h, ob=ob: E[eng].scalar_tensor_tensor(out=ob[:, h * 128:(h + 1) * 128], in0=d4[:, 2, h, 0:128], scalar=cf[:, 2, h:h + 1], in1=at[:], op0=ALU.mult, op1=ALU.add),
                             [r_d3, r_cf, r_at], [r_ob[h]])
                    dma("sp", MIX[rows, 512:1280], ob[:], reads=r_ob)
                S.barrier()
            if stop_phase <= 4:
                break

            with ExitStack() as es:
                wo, r_wo = mk(es, "wo", [128, 16, DM], BF16)
                g1, r_g1 = mk(es, "g1", [128, DM], F32)
                b1, r_b1 = mk(es, "b1", [128, DM], F32)
                wr, r_wr = mk(es, "wr", [128, 16, 36], F32)
                br, r_br = mk(es, "br", [1, 36], F32)
                mixl = mk(es, "mixl", [128, DM], BF16, 2)
                mT_ring = mk(es, "mT", [128, 16, 128], BF16, 2)
                xr = mk(es, "xr", [128, DM], F32, 2)
                u_ring = mk(es, "u5", [128, DM], F32, 2)
                x1 = mk(es, "x1t", [128, DM], F32, 3)
                x1b = mk(es, "x1bt", [128, DM], BF16, 2)
                x1T_ring = mk(es, "x1T", [128, 16, 128], F32, 2)
                st_ring = mk(es, "st5", [128, 4, 6], F32, 2)
                mv_ring = mk(es, "mv5", [128, 3], F32, 2)
                L_ring = mk(es, "L", [128, 36], F32, 2)
                sc_ring = mk(es, "sc5", [128, 16], F32, 2)
                goh_ring = mk(es, "goh", [128, 4], F32, 2)
                gex_ring = mk(es, "gex", [128, 4], F32, 2)
                pen_ring = mk(es, "pen", [128, 4], F32, 2)
                em_ring = mk(es, "em", [128, 32], F32, 2)
                em2_ring = mk(es, "em2", [128, 32], F32, 2)
                tp = mkp(es, "tp5", [128, 512], BF16, 2)
                po = mkp(es, "po5", [128, 512], F32, 4)
                tpf, r_tpf = mkp(es, "tpf", [128, 512], F32)
                plog, r_plog = mkp(es, "plog", [128, 64], F32)
                dma("pool", wo[:], w_out[l].rearrange("(k p) n -> p k n", p=128), writes=[r_wo])
                dma("sp", g1[:], ln1_g[l:l + 1, :].partition_broadcast(128), writes=[r_g1])
                dma("sp", b1[:], ln1_b[l:l + 1, :].partition_broadcast(128), writes=[r_b1])
                dma("sp", wr[:], w_rt[l].rearrange("(k p) n -> p k n", p=128), writes=[r_wr])
                dma("sp", br[:], b_rt[l:l + 1, :], writes=[r_br])
                def tile5(j):
                    rows = slice(j * 128, (j + 1) * 128)
                    mT, r_mT = mT_ring.next()
                    u, r_u = u_ring.next()
                    x1T, r_x1T = x1T_ring.next()
                    st, r_st = st_ring.next()
                    mv, r_mv = mv_ring.next()
                    L, r_L = L_ring.next()
                    sc, r_sc = sc_ring.next()
                    goh, r_goh = goh_ring.next()
                    gex, r_gex = gex_ring.next()
                    pen, r_pen = pen_ring.next()
                    em, r_em = em_ring.next()
                    em2, r_em2 = em2_ring.next()
                    ml, r_ml = mixl.next()
                    dma("sp", ml[:], MIX[rows, :], writes=[r_ml])
                    xrt, r_xr = xr.next()
                    dma("sp", xrt[:], XCUR[rows, :], writes=[r_xr])
                    for g4 in range(4):
                        tpt, r_tp = tp.next()
                        for k in range(4):
                            kc = g4 * 4 + k
                            S.op("pe", lambda kc=kc, k=k, tpt=tpt, ml=ml: nc.tensor.transpose(out=tpt[:, k * 128:(k + 1) * 128], in_=ml[:, kc * 128:(kc + 1) * 128], identity=ident_b[:]),
                                 [r_ml, r_identb], [r_tp], signal=(k == 3))
                        eng = alt("act", "dve")
                        S.op(eng, lambda eng=eng, tpt=tpt, g4=g4: ecopy(eng, mT[:, g4 * 4:(g4 + 1) * 4, :], tpt[:].rearrange("p (k t) -> p k t", k=4)), [r_tp], [r_mT])
                    for n4 in range(4):
                        pot, r_po5 = po.next()
                        for kc in range(16):
                            S.op("pe", lambda kc=kc, pot=pot, n4=n4: nc.tensor.matmul(pot[:], lhsT=mT[:, kc, :], rhs=wo[:, kc, n4 * 512:(n4 + 1) * 512], start=(kc == 0), stop=(kc == 15)),
                                 [r_mT, r_wo], [r_po5], signal=(kc == 15))
                        S.op("dve", lambda pot=pot, n4=n4, xrt=xrt: nc.vector.scalar_tensor_tensor(out=u[:, n4 * 512:(n4 + 1) * 512], in0=xrt[:, n4 * 512:(n4 + 1) * 512], scalar=ALPHA,
                                                                                                  in1=pot[:], op0=ALU.mult, op1=ALU.add), [r_xr, r_po5], [r_u])
                    yield
                    x1t, r_x1 = x1.next()
                    layer_norm_tile(None, u, r_u, g1, r_g1, b1, r_b1, st, r_st, mv, r_mv, x1t, r_x1)
                    dma("sp", X1[rows, :], x1t[:], reads=[r_x1])
                    xbt, r_xb1 = x1b.next()
                    S.op("act", lambda xbt=xbt, x1t=x1t: nc.scalar.copy(out=xbt[:], in_=x1t[:]), [r_x1], [r_xb1])
                    dma("sp", X1B[rows, :], xbt[:], reads=[r_xb1])
                    yield
                    for g4 in range(4):
                        for k in range(4):
                            kc = g4 * 4 + k
                            S.op("pe", lambda kc=kc, k=k, x1t=x1t: nc.tensor.transpose(out=tpf[:, k * 128:(k + 1) * 128], in_=x1t[:, kc * 128:(kc + 1) * 128], identity=ident_f[:]),
                                 [r_x1, r_identf], [r_tpf], signal=(k == 3))
                        eng = alt("act", "dve")
                        S.op(eng, lambda eng=eng, g4=g4: ecopy(eng, x1T[:, g4 * 4:(g4 + 1) * 4, :], tpf[:].rearrange("p (k t) -> p k t", k=4)), [r_tpf], [r_x1T])
                    for kc in range(16):
                        S.op("pe", lambda kc=kc: nc.tensor.matmul(plog[:, 0:36], lhsT=x1T[:, kc, :], rhs=wr[:, kc, :], start=(kc == 0), stop=False), [r_x1T, r_wr], [r_plog], signal=False)
                    S.op("pe", lambda: nc.tensor.matmul(plog[:, 0:36], lhsT=ones_f[0:1, :], rhs=br[:], start=False, stop=True), [r_onesf, r_br], [r_plog])
                    S.op("act", lambda: nc.scalar.copy(out=L[:], in_=plog[:, 0:36]), [r_plog], [r_L])
                    yield
                    V_ = nc.vector
                    S.op("dve", lambda: V_.reduce_max(out=sc[:, 0:1], in_=L[:, 0:4], axis=AX.X), [r_L], [r_sc])
                    S.op("dve", lambda: V_.tensor_scalar(out=goh[:], in0=L[:, 0:4], scalar1=sc[:, 0:1], scalar2=None, op0=ALU.is_ge), [r_L, r_sc], [r_goh])
                    S.op("dve", lambda: V_.tensor_scalar(out=sc[:, 1:2], in0=sc[:, 0:1], scalar1=-1.0, scalar2=None, op0=ALU.mult), [r_sc], [r_sc])
                    S.op("act", lambda: nc.scalar.activation(out=gex[:], in_=L[:, 0:4], func=AF.Exp, bias=sc[:, 1:2], scale=1.0, accum_out=sc[:, 2:3]), [r_L, r_sc], [r_gex, r_sc])
                    S.op("dve", lambda: V_.reciprocal(out=sc[:, 3:4], in_=sc[:, 2:3]), [r_sc], [r_sc])
                    S.op("dve", lambda: V_.tensor_scalar(out=pen[:], in0=goh[:], scalar1=BIG, scalar2=-BIG, op0=ALU.mult, op1=ALU.add), [r_goh], [r_pen])
                    for g in range(4):
                        S.op("dve", lambda g=g: V_.tensor_scalar(out=em[:, g * 8:(g + 1) * 8], in0=L[:, 4 + g * 8:4 + (g + 1) * 8], scalar1=pen[:, g:g + 1], scalar2=None, op0=ALU.add),
                             [r_L, r_pen], [r_em])
                    S.op("dve", lambda: V_.reduce_max(out=sc[:, 4:5], in_=em[:], axis=AX.X), [r_em], [r_sc])
                    S.op("dve", lambda j=j: V_.tensor_scalar(out=OH[:, j, 0, :], in0=em[:], scalar1=sc[:, 4:5], scalar2=None, op0=ALU.is_ge), [r_em, r_sc], [r_OH])
                    S.op("dve", lambda j=j: V_.scalar_tensor_tensor(out=em2[:], in0=OH[:, j, 0, :], scalar=-BIG, in1=em[:], op0=ALU.mult, op1=ALU.add), [r_OH, r_em], [r_em2])
                    S.op("dve", lambda: V_.reduce_max(out=sc[:, 5:6], in_=em2[:], axis=AX.X), [r_em2], [r_sc])
                    S.op("dve", lambda j=j: V_.tensor_scalar(out=OH[:, j, 1, :], in0=em2[:], scalar1=sc[:, 5:6], scalar2=None, op0=ALU.is_ge), [r_em2, r_sc], [r_OH])
                    S.op("dve", lambda: V_.tensor_tensor(out=sc[:, 6:7], in0=sc[:, 5:6], in1=sc[:, 4:5], op=ALU.subtract), [r_sc], [r_sc])
                    S.op("act", lambda: nc.scalar.activation(out=sc[:, 7:8], in_=sc[:, 6:7], func=AF.Exp), [r_sc], [r_sc])
                    S.op("dve", lambda: V_.tensor_scalar(out=sc[:, 8:9], in0=sc[:, 7:8], scalar1=1.0, scalar2=None, op0=ALU.add), [r_sc], [r_sc])
                    S.op("dve", lambda: V_.reciprocal(out=sc[:, 9:10], in_=sc[:, 8:9]), [r_sc], [r_sc])
                    S.op("dve", lambda j=j: V_.tensor_tensor(out=GATE[:, j, 0:1], in0=sc[:, 3:4], in1=sc[:, 9:10], op=ALU.mult), [r_sc], [r_GATE])
                    S.op("dve", lambda: V_.tensor_tensor(out=sc[:, 10:11], in0=sc[:, 7:8], in1=sc[:, 9:10], op=ALU.mult), [r_sc], [r_sc])
                    S.op("dve", lambda j=j: V_.tensor_tensor(out=GATE[:, j, 1:2], in0=sc[:, 3:4], in1=sc[:, 10:11], op=ALU.mult), [r_sc], [r_GATE])
                    S.op("dve", lambda j=j: V_.tensor_tensor(out=A_b[:, j, :], in0=OH[:, j, 0, :], in1=OH[:, j, 1, :], op=ALU.add), [r_OH], [r_Ab])
                run_skewed(tile5, NT)
                S.barrier()
            if stop_phase <= 5:
                break

            with ExitStack() as es:
                V_ = nc.vector
                cnt, r_cnt = mk(es, "cnt", [128, 32], F32)
                pc, r_pc = mk(es, "pc", [128, 32], F32)
                pa, r_pa = mk(es, "pa", [128, 32], F32)
                pb, r_pb = mk(es, "pb", [128, 32], F32)
                pstart, r_pstart = mk(es, "pstart", [128, 32], F32)
                carry, r_carry = mk(es, "carry", [128, 32], F32)
                pos, r_pos = mk(es, "pos", [128, 32], F32)
                tmp, r_tmp = mk(es, "tmp6", [128, 32], F32)
                destf, r_destf = mk(es, "destf", [128, NT, 2], F32)
                EB, r_EB = mk(es, "EB", [128, NBLK], F32)
                bsg, r_bsg = mk(es, "bsg", [128, NBLK], F32)
                bsd, r_bsd = mk(es, "bsd", [128, NBLK], F32)
                ioi, r_ioi = mk(es, "ioi", [128, 1], I32)
                iof, r_iof = mk(es, "iof", [128, 1], F32)
                iof4, r_iof4 = mk(es, "iof4", [128, 1], F32)
                strib, r_strib = mk(es, "strib", [128, 128], BF16)
                strif, r_strif = mk(es, "strif", [128, 128], F32)
                xs = mk(es, "xs6", [128, DM], BF16, 3)
                ptot, r_ptot = mkp(es, "ptot", [128, 64], F32)
                prk = mkp(es, "prk", [128, 64], F32, 2)
                dma("sp", strif[:], c_stri, writes=[r_strif])
                S.op("dve", lambda: V_.tensor_copy(out=strib[:], in_=strif[:]), [r_strif], [r_strib])
                S.op("pool", lambda: nc.gpsimd.iota(ioi[:], pattern=[[0, 1]], base=0, channel_multiplier=1), [], [r_ioi])
                S.op("dve", lambda: V_.tensor_copy(out=iof[:], in_=ioi[:]), [r_ioi], [r_iof])
                for j in range(NT):
                    S.op("pe", lambda j=j: nc.tensor.matmul(ptot[:, 0:32], lhsT=ones_b[:], rhs=A_b[:, j, :], start=(j == 0), stop=(j == NT - 1)), [r_onesb, r_Ab], [r_ptot], signal=(j == NT - 1))
                S.op("dve", lambda: V_.tensor_copy(out=cnt[:], in_=ptot[:, 0:32]), [r_ptot], [r_cnt])
                S.op("dve", lambda: V_.memset(tmp[:], 0.0), [], [r_tmp])
                for m_ in range(-(-SEQ // BLK)):
                    S.op("dve", lambda m_=m_: V_.scalar_tensor_tensor(out=tmp[:], in0=cnt[:], scalar=float(m_ * BLK), in1=tmp[:], op0=ALU.is_gt, op1=ALU.add), [r_cnt, r_tmp], [r_tmp])
                S.op("dve", lambda: V_.tensor_scalar(out=pc[:], in0=tmp[:], scalar1=float(BLK), scalar2=None, op0=ALU.mult), [r_tmp], [r_pc])
                S.op("dve", lambda: V_.tensor_copy(out=pa[:], in_=pc[:]), [r_pc], [r_pa])
                cur, r_cur, oth, r_oth = pa, r_pa, pb, r_pb
                for sh in (1, 2, 4, 8, 16):
                    S.op("dve", lambda cur=cur, oth=oth, sh=sh: V_.tensor_copy(out=oth[:, 0:sh], in_=cur[:, 0:sh]), [r_cur], [r_oth])
                    S.op("dve", lambda cur=cur, oth=oth, sh=sh: V_.tensor_tensor(out=oth[:, sh:32], in0=cur[:, sh:32], in1=cur[:, 0:32 - sh], op=ALU.add), [r_cur], [r_oth])
                    cur, r_cur, oth, r_oth = oth, r_oth, cur, r_cur
                pend, r_pend = cur, r_cur
                S.op("dve", lambda: V_.tensor_tensor(out=pstart[:], in0=pend[:], in1=pc[:], op=ALU.subtract), [r_pend, r_pc], [r_pstart])
                S.op("dve", lambda: V_.memset(carry[:], 0.0), [], [r_carry])
                for j in range(NT):
                    prt, r_pr = prk.next()
                    S.op("pe", lambda prt=prt, j=j: nc.tensor.matmul(prt[:, 0:32], lhsT=strib[:], rhs=A_b[:, j, :], start=True, stop=True), [r_strib, r_Ab], [r_pr], signal=False)
                    S.op("pe", lambda prt=prt, j=j: nc.tensor.matmul(prt[:, 32:64], lhsT=ones_b[:], rhs=A_b[:, j, :], start=True, stop=True), [r_onesb, r_Ab], [r_pr])
                    S.op("dve", lambda prt=prt: V_.tensor_tensor(out=pos[:], in0=prt[:, 0:32], in1=carry[:], op=ALU.add), [r_pr, r_carry], [r_pos])
                    S.op("dve", lambda: V_.tensor_tensor(out=pos[:], in0=pos[:], in1=pstart[:], op=ALU.add), [r_pos, r_pstart], [r_pos])
                    for k in range(2):
                        S.op("dve", lambda j=j, k=k: V_.tensor_tensor(out=tmp[:], in0=OH[:, j, k, :], in1=pos[:], op=ALU.mult), [r_OH, r_pos], [r_tmp])
                        S.op("dve", lambda j=j, k=k: V_.reduce_sum(out=destf[:, j, k:k + 1], in_=tmp[:], axis=AX.X), [r_tmp], [r_destf])
                    S.op("dve", lambda prt=prt: V_.tensor_tensor(out=carry[:], in0=carry[:], in1=prt[:, 32:64], op=ALU.add), [r_pr, r_carry], [r_carry])
                S.op("dve", lambda: V_.tensor_copy(out=DESTI[:], in_=destf[:]), [r_destf], [r_DESTI])
                for j in range(NT):
                    rows = slice(j * 128, (j + 1) * 128)
                    xst, r_xs = xs.next()
                    dma("sp", xst[:], X1B[rows, :], writes=[r_xs])
                    for k in range(2):
                        S.dma("pool", lambda xst=xst, j=j, k=k: nc.gpsimd.indirect_dma_start(
                            out=XG, out_offset=bass.IndirectOffsetOnAxis(ap=DESTI[:, j, k:k + 1], axis=0), in_=xst[:], in_offset=None),
                            [r_xs, r_DESTI], [])
                for i in range(NBLK):
                    S.op("dve", lambda i=i: V_.tensor_scalar(out=tmp[:], in0=pend[:], scalar1=float(i * BLK), scalar2=None, op0=ALU.is_le), [r_pend], [r_tmp])
                    S.op("dve", lambda i=i: V_.reduce_sum(out=EB[:, i:i + 1], in_=tmp[:], axis=AX.X), [r_tmp], [r_EB])
                S.op("dve", lambda: V_.tensor_scalar(out=iof4[:], in0=iof[:], scalar1=4.0, scalar2=None, op0=ALU.mult), [r_iof], [r_iof4])
                S.op("dve", lambda: V_.tensor_scalar(out=bsg[:], in0=EB[:], scalar1=512.0, scalar2=iof4[:, 0:1], op0=ALU.mult, op1=ALU.add), [r_EB, r_iof4], [r_bsg])
                S.op("dve", lambda: V_.tensor_scalar(out=bsd[:], in0=EB[:], scalar1=512.0, scalar2=iof[:, 0:1], op0=ALU.mult, op1=ALU.add), [r_EB, r_iof], [r_bsd])
                for q4 in range(4):
                    S.op("dve", lambda q4=q4: V_.tensor_scalar(out=IDXG[:, :, q4], in0=bsg[:], scalar1=float(q4 + l * 32 * 512), scalar2=None, op0=ALU.add), [r_bsg], [r_IDXG])
                for fc in range(4):
                    S.op("dve", lambda fc=fc: V_.tensor_scalar(out=IDXD[:, :, fc], in0=bsd[:], scalar1=float(fc * 128 + l * 32 * 512), scalar2=None, op0=ALU.add), [r_bsd], [r_IDXD])
                S.barrier()
            if stop_phase <= 6:
                break

            with ExitStack() as es:
                def wring(name, shape, nres):
                    return Ring([(es.enter_context(nc.sbuf_tensor("%s%d_%d" % (name, k, l), shape, BF16)), [Res() for _ in range(nres)]) for k in range(2)])
                wgr = wring("wgt", [128, 16, 512], 16)
                wur = wring("wut", [128, 16, 512], 16)
                wdr = wring("wdt", [128, 4, DM], 4)
                xg = mk(es, "xg", [128, DM], BF16, 2)
                xgT = mk(es, "xgT", [128, 16, BLK], BF16, 2)
                sgm = mk(es, "sgm", [128, BLK], F32, 2)
                hT = mk(es, "hT", [128, 4, BLK], BF16, 2)
                ys = mk(es, "ys", [128, DM], F32, 2)
                tp = mkp(es, "tp7", [128, 512], BF16, 2)
                pg = mkp(es, "pg", [128, BLK], F32, 2)
                pu = mkp(es, "pu", [128, BLK], F32, 2)
                py = mkp(es, "py", [128, 512], F32, 2)
                bc_g = nc.gpsimd.to_reg((l + 1) * 32 * 512 - 1)
                bc_d = nc.gpsimd.to_reg((l + 1) * 32 * 512 - 1)
                for (ring_, nres_) in ((wgr, 16), (wur, 16), (wdr, 4)):
                    for (t_, rs_) in ring_.items:
                        S.op("pool", lambda t_=t_: nc.gpsimd.memset(t_[:], 0.0), [], rs_)
                for i in range(NBLK):
                    wgt, r_wg = wgr.next()
                    wut, r_wu = wur.next()
                    wdt, r_wd = wdr.next()
                    for q4 in range(4):
                        S.dma("pool", lambda wgt=wgt, i=i, q4=q4: nc.gpsimd.indirect_dma_start(
                            out=wgt[:, 4 * q4:4 * q4 + 4, :].rearrange("p a b -> p (a b)"), out_offset=None, in_=w_eg, in_offset=bass.IndirectOffsetOnAxis(ap=IDXG[:, i, q4:q4 + 1], axis=0), bounds_check=bc_g, oob_is_err=False),
                            [r_IDXG], [r_wg[q4]])
                        S.dma("pool", lambda wut=wut, i=i, q4=q4: nc.gpsimd.indirect_dma_start(
                            out=wut[:, 4 * q4:4 * q4 + 4, :].rearrange("p a b -> p (a b)"), out_offset=None, in_=w_eu, in_offset=bass.IndirectOffsetOnAxis(ap=IDXG[:, i, q4:q4 + 1], axis=0), bounds_check=bc_g, oob_is_err=False),
                            [r_IDXG], [r_wu[q4]])
                    for fc in range(4):
                        S.dma("pool", lambda wdt=wdt, i=i, fc=fc: nc.gpsimd.indirect_dma_start(
                            out=wdt[:, fc, :], out_offset=None, in_=w_ed, in_offset=bass.IndirectOffsetOnAxis(ap=IDXD[:, i, fc:fc + 1], axis=0), bounds_check=bc_d, oob_is_err=False),
                            [r_IDXD], [r_wd[fc]])
                    xTt, r_xgT = xgT.next()
                    for sb in range(BLK // 128):
                        xgt, r_xg = xg.next()
                        r0 = i * BLK + sb * 128
                        dma("sp", xgt[:], XG[r0:r0 + 128, :], writes=[r_xg])
                        for g4 in range(4):
                            tpt, r_tp = tp.next()
                            for k in range(4):
                                kc = g4 * 4 + k
                                S.op("pe", lambda kc=kc, k=k, tpt=tpt, xgt=xgt: nc.tensor.transpose(out=tpt[:, k * 128:(k + 1) * 128], in_=xgt[:, kc:kc + 127 * 16 + 1:16], identity=ident_b[:]),
                                     [r_xg, r_identb], [r_tp], signal=(k == 3))
                            eng = alt("act", "dve")
                            S.op(eng, lambda eng=eng, tpt=tpt, g4=g4, sb=sb, xTt=xTt: ecopy(eng, xTt[:, g4 * 4:(g4 + 1) * 4, sb * 128:(sb + 1) * 128], tpt[:].rearrange("p (k t) -> p k t", k=4)),
                                 [r_tp], [r_xgT])
                    hTt, r_hT = hT.next()
                    for fc in range(4):
                        pgt, r_pg = pg.next()
                        put, r_pu = pu.next()
                        for kc in range(16):
                            S.op("pe", lambda kc=kc, fc=fc, pgt=pgt, wgt=wgt, xTt=xTt: nc.tensor.matmul(pgt[:], lhsT=wgt[:, kc, fc * 128:(fc + 1) * 128], rhs=xTt[:, kc, :], start=(kc == 0), stop=(kc == 15)),
                                 [r_wg[kc // 4], r_xgT], [r_pg], signal=(kc == 15))
                        for kc in range(16):
                            S.op("pe", lambda kc=kc, fc=fc, put=put, wut=wut, xTt=xTt: nc.tensor.matmul(put[:], lhsT=wut[:, kc, fc * 128:(fc + 1) * 128], rhs=xTt[:, kc, :], start=(kc == 0), stop=(kc == 15)),
                                 [r_wu[kc // 4], r_xgT], [r_pu], signal=(kc == 15))
                        sgt, r_sg = sgm.next()
                        S.op("act", lambda sgt=sgt, pgt=pgt: nc.scalar.activation(out=sgt[:], in_=pgt[:], func=AF.Silu), [r_pg], [r_sg])
                        S.op("dve", lambda sgt=sgt, put=put, hTt=hTt, fc=fc: nc.vector.tensor_tensor(out=hTt[:, fc, :], in0=sgt[:], in1=put[:], op=ALU.mult), [r_sg, r_pu], [r_hT])
                    for sb in range(BLK // 128):
                        yst, r_ys = ys.next()
                        for n4 in range(4):
                            pyt, r_py = py.next()
                            for fc in range(4):
                                S.op("pe", lambda fc=fc, pyt=pyt, hTt=hTt, wdt=wdt, sb=sb, n4=n4: nc.tensor.matmul(pyt[:], lhsT=hTt[:, fc, sb * 128:(sb + 1) * 128], rhs=wdt[:, fc, n4 * 512:(n4 + 1) * 512], start=(fc == 0), stop=(fc == 3)),
                                     [r_hT, r_wd[fc]], [r_py], signal=(fc == 3))
                            eng = alt("act", "dve")
                            S.op(eng, lambda eng=eng, yst=yst, pyt=pyt, n4=n4: ecopy(eng, yst[:, n4 * 512:(n4 + 1) * 512], pyt[:]), [r_py], [r_ys])
                        r0 = i * BLK + sb * 128
                        dma("sp", YB[r0:r0 + 128, :], yst[:], reads=[r_ys])
                S.barrier()
                nc.gpsimd.free_register(bc_g)
                nc.gpsimd.free_register(bc_d)
            if stop_phase <= 7:
                break

            with ExitStack() as es:
                g2, r_g2 = mk(es, "g2", [128, DM], F32)
                b2, r_b2 = mk(es, "b2", [128, DM], F32)
                y1 = mk(es, "y1", [128, DM], F32, 2)
                y2 = mk(es, "y2", [128, DM], F32, 2)
                x1r = mk(es, "x1r", [128, DM], F32, 2)
                u_ring = mk(es, "u8", [128, DM], F32, 2)
                xo = mk(es, "xo", [128, DM], F32, 2)
                st_ring = mk(es, "st8", [128, 4, 6], F32, 2)
                mv_ring = mk(es, "mv8", [128, 3], F32, 2)
                dma("sp", g2[:], ln2_g[l:l + 1, :].partition_broadcast(128), writes=[r_g2])
                dma("sp", b2[:], ln2_b[l:l + 1, :].partition_broadcast(128), writes=[r_b2])
                def tile8(j):
                    rows = slice(j * 128, (j + 1) * 128)
                    u, r_u = u_ring.next()
                    st, r_st = st_ring.next()
                    mv, r_mv = mv_ring.next()
                    y1t, r_y1 = y1.next()
                    y2t, r_y2 = y2.next()
                    S.dma("pool", lambda y1t=y1t, j=j: nc.gpsimd.indirect_dma_start(out=y1t[:], out_offset=None, in_=YB, in_offset=bass.IndirectOffsetOnAxis(ap=DESTI[:, j, 0:1], axis=0)),
                          [r_DESTI], [r_y1])
                    S.dma("pool", lambda y2t=y2t, j=j: nc.gpsimd.indirect_dma_start(out=y2t[:], out_offset=None, in_=YB, in_offset=bass.IndirectOffsetOnAxis(ap=DESTI[:, j, 1:2], axis=0)),
                          [r_DESTI], [r_y2])
                    xt_, r_x1r = x1r.next()
                    dma("sp", xt_[:], X1[rows, :], writes=[r_x1r])
                    S.op("dve", lambda y1t=y1t, j=j: nc.vector.tensor_scalar(out=u[:], in0=y1t[:], scalar1=GATE[:, j, 0:1], scalar2=None, op0=ALU.mult), [r_y1, r_GATE], [r_u])
                    S.op("dve", lambda y2t=y2t, j=j: nc.vector.scalar_tensor_tensor(out=u[:], in0=y2t[:], scalar=GATE[:, j, 1:2], in1=u[:], op0=ALU.mult, op1=ALU.add), [r_y2, r_GATE, r_u], [r_u])
                    S.op("dve", lambda xt_=xt_: nc.vector.scalar_tensor_tensor(out=u[:], in0=xt_[:], scalar=ALPHA, in1=u[:], op0=ALU.mult, op1=ALU.add), [r_x1r, r_u], [r_u])
                    yield
                    xot, r_xo = xo.next()
                    layer_norm_tile(None, u, r_u, g2, r_g2, b2, r_b2, st, r_st, mv, r_mv, xot, r_xo)
                    dma("sp", XNEXT[rows, :], xot[:], reads=[r_xo])
                run_skewed(tile8, NT)
                S.barrier()
            XCUR = XNEXT
    return nc, S


def make_inputs(inputs, core, stop_phase=99):
    hc = host_consts()
    m = {}
    m["x"] = np.ascontiguousarray(inputs["x"][core])
    for k in ("w_in", "w_gla_gate", "b_gla_gate", "ret_norm_g", "gla_norm_g", "w_out", "ln1_g", "ln1_b", "ln2_g", "ln2_b"):
        m[k] = np.ascontiguousarray(inputs[k])
    m["w_router"] = np.ascontiguousarray(np.concatenate([inputs["w_router_group"], inputs["w_router_expert"]], axis=-1))
    m["b_router"] = np.ascontiguousarray(np.concatenate([inputs["b_router_group"], inputs["b_router_expert"]], axis=-1))
    if stop_phase >= 7:
        m["w_expert_gate"] = np.ascontiguousarray(inputs["w_expert_gate"]).reshape(DEPTH * 32 * 128 * 4, 2048)
        m["w_expert_up"] = np.ascontiguousarray(inputs["w_expert_up"]).reshape(DEPTH * 32 * 128 * 4, 2048)
        m["w_expert_down"] = np.ascontiguousarray(inputs["w_expert_down"]).reshape(DEPTH * 32 * 512, DM)
    m["c_ident"] = hc["ident"]
    m["c_mask01"] = hc["mask01"]
    m["c_stri"] = hc["stri"]
    m["c_dmask0"] = hc["dmask0"]
    m["c_dmask1"] = hc["dmask1"]
    m["c_rope"] = hc["rope"]
    return m


def kernel(**inputs):
    inputs = {k: np.asarray(v) for k, v in inputs.items()}
    nc, _ = build()
    in_maps = [make_inputs(inputs, c) for c in range(8)]
    res = run_bass_kernel_spmd(nc, in_maps, core_ids=list(range(8)))
    return np.stack([r["y"] for r in res.results], axis=0).astype(np.float32)
```

```python
import numpy as np
from contextlib import ExitStack
import concourse.bass as bass
import concourse.mybir as mybir
from concourse.bass_utils import run_bass_kernel_spmd

F32 = mybir.dt.float32
BF16 = mybir.dt.bfloat16
I32 = mybir.dt.int32
AF = mybir.ActivationFunctionType
ALU = mybir.AluOpType
AX = mybir.AxisListType

ND = 12
SEQ = 4096
DM = 2048
NT = SEQ // 128
DEPTH = 4
INW = 6672
ALPHA = float((2 * DEPTH) ** 0.25)
EPS = 1e-5
BLK = 384
NBLK = -(-(2 * SEQ + 32 * (BLK - 1)) // BLK)
NROWS = NBLK * BLK
BIG = 30000.0


class Res:
    __slots__ = ("name", "w", "r")

    def __init__(self, name=""):
        self.name = name
        self.w = {}
        self.r = {}


class Sched:
    def __init__(self, nc, es, same_sync=True):
        self.nc = nc
        self.same_sync = same_sync
        self.e = dict(pe=nc.tensor, act=nc.scalar, dve=nc.vector, pool=nc.gpsimd, sp=nc.sync)
        self.sem = {k: es.enter_context(nc.semaphore("s_" + k)) for k in ("pe", "act", "dve", "pool")}
        self.cnt = {k: 0 for k in self.sem}
        self.pending = {k: False for k in self.sem}
        self.dsem = {q: [es.enter_context(nc.semaphore("d_%s%d" % (q, i))) for i in range(ND)]
                     for q in ("sp", "pool", "act")}
        self.dcnt = {q: [0] * ND for q in self.dsem}
        self.dnext = {q: 0 for q in self.dsem}
        self.known = {k: {} for k in self.e}
        self.n_ins = 0
        self.n_wait = 0

    def _semof(self, key):
        if key[0] == "c":
            return self.sem[key[1]], 1
        return self.dsem[key[1]][key[2]], 16

    def _wait(self, eng, key, val):
        if self.known[eng].get(key, 0) >= val:
            return
        sem, mult = self._semof(key)
        self.e[eng].wait_ge(sem, val * mult)
        self.known[eng][key] = val
        self.n_wait += 1

    def _deps(self, eng, reads, writes):
        deps = {}
        own = ("c", eng)
        for r in reads:
            for k, v in r.w.items():
                if deps.get(k, 0) < v:
                    deps[k] = v
        for w in writes:
            for d in (w.w, w.r):
                for k, v in d.items():
                    if k == own:
                        continue
                    if deps.get(k, 0) < v:
                        deps[k] = v
        for k, v in deps.items():
            if k == own and (eng == "pe" or not self.same_sync):
                continue
            self._wait(eng, k, v)

    def _mark(self, key, val, reads, writes):
        for r in reads:
            if r.r.get(key, 0) < val:
                r.r[key] = val
        for w in writes:
            w.w = {key: val}
            w.r = {}

    def op(self, eng, fn, reads=(), writes=(), signal=True):
        self._deps(eng, reads, writes)
        ins = fn()
        self.n_ins += 1
        if signal:
            self.cnt[eng] += 1
            ins.then_inc(self.sem[eng], 1)
            self.pending[eng] = False
            val = self.cnt[eng]
        else:
            self.pending[eng] = True
            val = self.cnt[eng] + 1
        self._mark(("c", eng), val, reads, writes)
        return ins

    def dma(self, q, fn, reads=(), writes=()):
        slot = self.dnext[q]
        self.dnext[q] = (slot + 1) % ND
        key = ("d", q, slot)
        if self.dcnt[q][slot] > 0:
            self._wait(q, key, self.dcnt[q][slot])
        self._deps(q, reads, writes)
        ins = fn()
        self.n_ins += 1
        self.dcnt[q][slot] += 1
        ins.then_inc(self.dsem[q][slot], 16)
        self._mark(key, self.dcnt[q][slot], reads, writes)
        return ins

    def barrier(self):
        for k in self.pending:
            assert not self.pending[k], "pending unsignaled instruction on " + k
        for eng in self.e.keys():
            for k in self.cnt:
                if self.cnt[k] > 0:
                    self._wait(eng, ("c", k), self.cnt[k])
            for q in self.dcnt:
                for i in range(ND):
                    if self.dcnt[q][i] > 0:
                        self._wait(eng, ("d", q, i), self.dcnt[q][i])


class Ring:
    def __init__(self, items):
        self.items = items
        self.i = 0

    def next(self):
        it = self.items[self.i]
        self.i = (self.i + 1) % len(self.items)
        return it


def host_consts():
    c = {}
    i = np.arange(128)
    c["ident"] = np.eye(128, dtype=np.float32)
    c["mask01"] = (i[:, None] <= i[None, :]).astype(np.float32)
    c["stri"] = (i[:, None] < i[None, :]).astype(np.float32)
    j = np.arange(256)
    band = (j[None, :] >= i[:, None]) & (j[None, :] <= i[:, None] + 128)
    c["dmask1"] = np.where(band, 0.0, -BIG).astype(np.float32)
    c["dmask0"] = np.where(band & (j[None, :] >= 128), 0.0, -BIG).astype(np.float32)
    half = 64
    inv = (np.float32(10000.0) ** (-np.arange(half, dtype=np.float32) / np.float32(half))).astype(np.float32)
    pos = np.arange(SEQ, dtype=np.float32)
    ang = (pos[:, None] * inv[None, :]).astype(np.float32)
    cos = np.cos(ang).astype(np.float32)
    sin = np.sin(ang).astype(np.float32)
    h = np.arange(4, dtype=np.float32)
    log_g = np.log1p(-np.exp2(-5.0 - h)).astype(np.float64)
    cc = (np.arange(SEQ) % 128).astype(np.float64)
    qd = np.exp((cc[:, None] + 1.0) * log_g[None, :])
    kd = np.exp(-(cc[:, None] + 1.0) * log_g[None, :]) * (128.0 ** -0.5)
    rope = np.zeros((4, SEQ, 4, 64), np.float32)
    rope[0] = cos[:, None, :] * qd[:, :, None]
    rope[1] = sin[:, None, :] * qd[:, :, None]
    rope[2] = cos[:, None, :] * kd[:, :, None]
    rope[3] = sin[:, None, :] * kd[:, :, None]
    c["rope"] = rope.reshape(4, SEQ, 256)
    c["g128"] = [float(np.exp(128.0 * lg)) for lg in log_g]
    return c


def build(n_layers=DEPTH, debug=(), stop_phase=99):
    nc = bass.Bass("TRN2", target_bir_lowering=False)
    hc = host_consts()
    g128 = hc["g128"]

    def din(name, shape, dt=F32):
        return nc.dram_tensor(name, list(shape), dt, kind="ExternalInput").ap()

    def dscr(name, shape, dt):
        return nc.dram_tensor(name, list(shape), dt, kind=("ExternalOutput" if name in debug else "Internal")).ap()

    x_in = din("x", [SEQ, DM])
    w_in = din("w_in", [DEPTH, DM, INW])
    w_gg = din("w_gla_gate", [DEPTH, 16, 384])
    b_gg = din("b_gla_gate", [DEPTH, 384])
    ret_g = din("ret_norm_g", [DEPTH, 512])
    gla_g = din("gla_norm_g", [DEPTH, 768])
    w_out = din("w_out", [DEPTH, DM, DM])
    ln1_g = din("ln1_g", [DEPTH, DM])
    ln1_b = din("ln1_b", [DEPTH, DM])
    w_rt = din("w_router", [DEPTH, DM, 36])
    b_rt = din("b_router", [DEPTH, 36])
    if stop_phase >= 7:
        w_eg = din("w_expert_gate", [DEPTH * 32 * 128 * 4, 2048])
        w_eu = din("w_expert_up", [DEPTH * 32 * 128 * 4, 2048])
        w_ed = din("w_expert_down", [DEPTH * 32 * 512, DM])
    ln2_g = din("ln2_g", [DEPTH, DM])
    ln2_b = din("ln2_b", [DEPTH, DM])
    c_ident = din("c_ident", [128, 128])
    c_mask01 = din("c_mask01", [128, 128])
    c_stri = din("c_stri", [128, 128])
    c_dmask0 = din("c_dmask0", [128, 256])
    c_dmask1 = din("c_dmask1", [128, 256])
    c_rope = din("c_rope", [4, SEQ, 256])
    y_out = nc.dram_tensor("y", [SEQ, DM], F32, kind="ExternalOutput").ap()

    P = dscr("P", [SEQ, 5120], BF16)
    PT = dscr("PT", [1536, SEQ], BF16)
    GAT = dscr("GAT", [16, SEQ], F32)
    DO = dscr("DO", [3, SEQ, 6 * 130], F32)
    MIX = dscr("MIX", [SEQ, DM], BF16)
    X1 = dscr("X1", [SEQ, DM], F32)
    X1B = dscr("X1B", [SEQ, DM], BF16)
    XG = dscr("XG", [NROWS, DM], BF16)
    YB = dscr("YB", [NROWS, DM], F32)
    XA = dscr("XA", [SEQ, DM], F32)
    XB_ = dscr("XBb", [SEQ, DM], F32)

    with ExitStack() as es0:
        S = Sched(nc, es0)
        E = S.e
        rr = {"i": 0}

        def alt(*engs):
            rr["i"] += 1
            return engs[rr["i"] % len(engs)]

        def ecopy(eng, out, in_):
            if eng == "act":
                return nc.scalar.copy(out=out, in_=in_)
            return E[eng].tensor_copy(out=out, in_=in_)

        def dma(q, out, in_, reads=(), writes=()):
            return S.dma(q, lambda: E[q].dma_start(out=out, in_=in_), reads, writes)

        uid = [0]

        def mk(es, name, shape, dt, n=1):
            items = []
            for k in range(n):
                uid[0] += 1
                t = es.enter_context(nc.sbuf_tensor("%s%d_%d" % (name, k, uid[0]), list(shape), dt))
                items.append((t, Res(name)))
            return items[0] if n == 1 else Ring(items)

        def mkp(es, name, shape, dt, n=1):
            items = []
            for k in range(n):
                uid[0] += 1
                t = es.enter_context(nc.psum_tensor("%s%d_%d" % (name, k, uid[0]), list(shape), dt))
                items.append((t, Res(name)))
            return items[0] if n == 1 else Ring(items)

        ident_f, r_identf = mk(es0, "identf", [128, 128], F32)
        ident_b, r_identb = mk(es0, "identb", [128, 128], BF16)
        mask01, r_mask01 = mk(es0, "mask01", [128, 128], F32)
        ones_b, r_onesb = mk(es0, "onesb", [128, 128], BF16)
        ones_f, r_onesf = mk(es0, "onesf", [128, 128], F32)
        dma("sp", ident_f[:], c_ident, writes=[r_identf])
        dma("sp", mask01[:], c_mask01, writes=[r_mask01])
        S.op("dve", lambda: nc.vector.tensor_copy(out=ident_b[:], in_=ident_f[:]), [r_identf], [r_identb])
        S.op("dve", lambda: nc.vector.memset(ones_b[:], 1.0), [], [r_onesb])
        S.op("dve", lambda: nc.vector.memset(ones_f[:], 1.0), [], [r_onesf])

        OH, r_OH = mk(es0, "OH", [128, NT, 2, 32], F32)
        A_b, r_Ab = mk(es0, "A_b", [128, NT, 32], BF16)
        GATE, r_GATE = mk(es0, "GATE", [128, NT, 2], F32)
        DESTI, r_DESTI = mk(es0, "DESTI", [128, NT, 2], I32)
        IDXG, r_IDXG = mk(es0, "IDXG", [128, NBLK, 4], I32)
        IDXD, r_IDXD = mk(es0, "IDXD", [128, NBLK, 4], I32)
        if stop_phase >= 6:
            with ExitStack() as es:
                zt, r_zt = mk(es, "zt", [128, DM], BF16)
                S.op("dve", lambda: nc.vector.memset(zt[:], 0.0), [], [r_zt])
                for i in range(NROWS // 128):
                    dma("sp", XG[i * 128:(i + 1) * 128, :], zt[:], reads=[r_zt])
                S.barrier()

        def run_skewed(tile_fn, n):
            gens = []
            t = 0
            while t < n or gens:
                if t < n:
                    gens.append(tile_fn(t))
                for g in list(gens):
                    try:
                        next(g)
                    except StopIteration:
                        gens.remove(g)
                t += 1

        def rstd(out_ap, var_ap, r_mv):
            S.op("dve", lambda: nc.vector.tensor_scalar(out=out_ap, in0=var_ap, scalar1=EPS, scalar2=None, op0=ALU.add), [r_mv], [r_mv])
            S.op("act", lambda: nc.scalar.sqrt(out=out_ap, in_=out_ap), [r_mv], [r_mv])
            S.op("dve", lambda: nc.vector.reciprocal(out=out_ap, in_=out_ap), [r_mv], [r_mv])

        def layer_norm_tile(es_unused, u, r_u, gt, r_gt, bt, r_bt, st, r_st, mv, r_mv, outt, r_out):
            for c4 in range(4):
                S.op("dve", lambda c4=c4: nc.vector.bn_stats(out=st[:, c4, :], in_=u[:, c4 * 512:(c4 + 1) * 512]),
                     [r_u], [r_st])
            S.op("dve", lambda: nc.vector.bn_aggr(out=mv[:, 0:2], in_=st[:].rearrange("p a b -> p (a b)")), [r_st], [r_mv])
            rstd(mv[:, 2:3], mv[:, 1:2], r_mv)
            S.op("dve", lambda: nc.vector.tensor_scalar(out=outt[:], in0=u[:], scalar1=mv[:, 0:1], scalar2=mv[:, 2:3],
                                                        op0=ALU.subtract, op1=ALU.mult), [r_u, r_mv], [r_out])
            S.op("pool", lambda: nc.gpsimd.tensor_tensor(out=outt[:], in0=outt[:], in1=gt[:], op=ALU.mult),
                 [r_out, r_gt], [r_out])
            S.op("pool", lambda: nc.gpsimd.tensor_tensor(out=outt[:], in0=outt[:], in1=bt[:], op=ALU.add),
                 [r_out, r_bt], [r_out])

        def head_norm(o_ap, nh, st, r_st, mv, r_mv, outt, r_out, r_o, col0):
            for h in range(nh):
                S.op("dve", lambda h=h: nc.vector.bn_stats(out=st[:, h, :], in_=o_ap(h)), [r_o], [r_st])
                S.op("dve", lambda h=h: nc.vector.bn_aggr(out=mv[:, h, 0:2], in_=st[:, h, :]), [r_st], [r_mv])
            rstd(mv[:, 0:nh, 2:3], mv[:, 0:nh, 1:2], r_mv)
            for h in range(nh):
                S.op("dve", lambda h=h: nc.vector.tensor_scalar(
                    out=outt[:, col0 + h * 128: col0 + (h + 1) * 128], in0=o_ap(h), scalar1=mv[:, h, 0:1],
                    scalar2=mv[:, h, 2:3], op0=ALU.subtract, op1=ALU.mult), [r_o, r_mv], [r_out])

        XCUR = x_in
        for l in range(n_layers):
            XNEXT = y_out if l == n_layers - 1 else (XA if l % 2 == 0 else XB_)
            with ExitStack() as es:
                xT, r_xT = mk(es, "xT", [128, 16, SEQ], BF16)
                xb = mk(es, "xb", [128, DM], BF16, 2)
                wt = mk(es, "wt", [128, 16, 512], BF16, 2)
                stg = mk(es, "stg", [128, 4, 512], BF16, 2)
                stgf, r_stgf = mk(es, "stgf", [16, 512], F32)
                tp = mkp(es, "tp", [128, 512], BF16, 2)
                ps = mkp(es, "ps", [128, 512], F32, 4)
                for j in range(NT):
                    xbt, r_xb = xb.next()
                    dma("pool", xbt[:], XCUR[j * 128:(j + 1) * 128, :], writes=[r_xb])
                    for g4 in range(4):
                        tpt, r_tp = tp.next()
                        for k in range(4):
                            kc = g4 * 4 + k
                            S.op("pe", lambda kc=kc, k=k, tpt=tpt, xbt=xbt: nc.tensor.transpose(
                                out=tpt[:, k * 128:(k + 1) * 128], in_=xbt[:, kc * 128:(kc + 1) * 128], identity=ident_b[:]),
                                [r_xb, r_identb], [r_tp], signal=(k == 3))
                        eng = alt("act", "dve")
                        S.op(eng, lambda eng=eng, tpt=tpt, g4=g4, j=j: ecopy(
                            eng, xT[:, g4 * 4:(g4 + 1) * 4, j * 128:(j + 1) * 128],
                            tpt[:].rearrange("p (k t) -> p k t", k=4)), [r_tp], [r_xT])
                if stop_phase >= 1:
                    tm_tiles = [(c0, c0) for c0 in range(0, 2048, 512)] + [(c0, c0 - 1536) for c0 in range(3584, 6656, 512)]
                    for (wc, pc) in tm_tiles:
                        wtt, r_wt = wt.next()
                        dma("pool", wtt[:], w_in[l, :, wc:wc + 512].rearrange("(k p) n -> p k n", p=128), writes=[r_wt])
                        for j4 in range(NT // 4):
                            sg, r_sg = stg.next()
                            for jj in range(4):
                                j = j4 * 4 + jj
                                pst, r_ps = ps.next()
                                for kc in range(16):
                                    S.op("pe", lambda kc=kc, pst=pst, j=j, wtt=wtt: nc.tensor.matmul(
                                        pst[:], lhsT=xT[:, kc, j * 128:(j + 1) * 128], rhs=wtt[:, kc, :],
                                        start=(kc == 0), stop=(kc == 15)), [r_xT, r_wt], [r_ps], signal=(kc == 15))
                                eng = alt("act", "dve")
                                S.op(eng, lambda eng=eng, sg=sg, jj=jj, pst=pst: ecopy(eng, sg[:, jj, :], pst[:]), [r_ps], [r_sg])
                            dma("sp", P[j4 * 512:(j4 + 1) * 512, pc:pc + 512].rearrange("(j p) n -> p j n", p=128), sg[:],
                                reads=[r_sg])
                    for g in range(3):
                        wtt, r_wt = wt.next()
                        wc = 2048 + g * 512
                        dma("pool", wtt[:], w_in[l, :, wc:wc + 512].rearrange("(k p) n -> p k n", p=128), writes=[r_wt])
                        for cc in range(4):
                            row0 = g * 512 + cc * 128
                            for s4 in range(2):
                                sg, r_sg = stg.next()
                                for ss in range(4):
                                    s = s4 * 4 + ss
                                    pst, r_ps = ps.next()
                                    for kc in range(16):
                                        S.op("pe", lambda kc=kc, pst=pst, s=s, wtt=wtt, cc=cc: nc.tensor.matmul(
                                            pst[:], lhsT=wtt[:, kc, cc * 128:(cc + 1) * 128], rhs=xT[:, kc, s * 512:(s + 1) * 512],
                                            start=(kc == 0), stop=(kc == 15)), [r_xT, r_wt], [r_ps], signal=(kc == 15))
                                    eng = alt("act", "dve")
                                    if row0 < 768:
                                        if eng == "act":
                                            S.op(eng, lambda sg=sg, ss=ss, pst=pst: nc.scalar.activation(out=sg[:, ss, :], in_=pst[:], func=AF.Copy, scale=float(128 ** -0.5)), [r_ps], [r_sg])
                                        else:
                                            S.op(eng, lambda sg=sg, ss=ss, pst=pst: nc.vector.tensor_scalar(out=sg[:, ss, :], in0=pst[:], scalar1=float(128 ** -0.5), scalar2=None, op0=ALU.mult), [r_ps], [r_sg])
                                    else:
                                        S.op(eng, lambda eng=eng, sg=sg, ss=ss, pst=pst: ecopy(eng, sg[:, ss, :], pst[:]), [r_ps], [r_sg])
                                dma("sp", PT[row0:row0 + 128, s4 * 2048:(s4 + 1) * 2048], sg[:].rearrange("p a b -> p (a b)"),
                                    reads=[r_sg])
                    wtt, r_wt = wt.next()
                    dma("pool", wtt[:, :, 0:16], w_in[l, :, 6656:6672].rearrange("(k p) n -> p k n", p=128), writes=[r_wt])
                    for s in range(8):
                        pst, r_ps = ps.next()
                        for kc in range(16):
                            S.op("pe", lambda kc=kc, pst=pst, s=s, wtt=wtt: nc.tensor.matmul(
                                pst[0:16, :], lhsT=wtt[:, kc, 0:16], rhs=xT[:, kc, s * 512:(s + 1) * 512],
                                start=(kc == 0), stop=(kc == 15)), [r_xT, r_wt], [r_ps], signal=(kc == 15))
                        S.op("act", lambda pst=pst: nc.scalar.copy(out=stgf[:], in_=pst[0:16, :]), [r_ps], [r_stgf])
                        dma("sp", GAT[:, s * 512:(s + 1) * 512], stgf[:], reads=[r_stgf])
                S.barrier()
            if stop_phase <= 1:
                break

            with ExitStack() as es:
                rope = mk(es, "rope", [128, 4, 256], F32, 2)
                qk = mk(es, "qk", [128, 1024], BF16, 2)
                rv = mk(es, "rv", [128, 512], BF16, 2)
                rg = mk(es, "rg", [128, 512], BF16, 2)
                qkr_ring = mk(es, "qkr", [128, 1024], BF16, 2)
                t1_ring = mk(es, "t1", [128, 4, 64], F32, 2)
                t2_ring = mk(es, "t2", [128, 4, 64], F32, 2)
                t3_ring = mk(es, "t3", [128, 4, 64], F32, 2)
                t4_ring = mk(es, "t4", [128, 4, 64], F32, 2)
                qkT_ring = mk(es, "qkT", [128, 8, 128], BF16, 2)
                sm = mk(es, "sm", [128, 128], BF16, 2)
                Sf = [mk(es, "Sf%d" % h, [128, 128], F32) for h in range(4)]
                Sb = [mk(es, "Sb%d" % h, [128, 128], BF16) for h in range(4)]
                Tt, r_Tt = mk(es, "Tt", [128, 128], F32)
                st_ring = mk(es, "st", [128, 4, 6], F32, 2)
                mv_ring = mk(es, "mv", [128, 4, 3], F32, 2)
                nrm_ring = mk(es, "nrm", [128, 512], F32, 2)
                sil_ring = mk(es, "sil", [128, 512], F32, 2)
                mixo = mk(es, "mixo", [128, 512], BF16, 2)
                gvec, r_gvec = mk(es, "gvec", [128, 512], F32)
                tp_ring = mkp(es, "tp2", [128, 8, 128], BF16, 2)
                sT = mkp(es, "sT", [128, 128], F32, 2)
                po_ring = mkp(es, "po", [128, 512], F32, 2)
                kv = mkp(es, "kv", [128, 128], F32, 2)
                dma("sp", gvec[:], ret_g[l:l + 1, :].partition_broadcast(128), writes=[r_gvec])
                for h in range(4):
                    S.op("dve", lambda h=h: nc.vector.memset(Sf[h][0][:], 0.0), [], [Sf[h][1]])
                    S.op("dve", lambda h=h: nc.vector.memset(Sb[h][0][:], 0.0), [], [Sb[h][1]])
                def tile2(j):
                    rows = slice(j * 128, (j + 1) * 128)
                    qkr, r_qkr = qkr_ring.next()
                    t1, r_t1 = t1_ring.next()
                    t2, r_t2 = t2_ring.next()
                    t3, r_t3 = t3_ring.next()
                    t4, r_t4 = t4_ring.next()
                    qkT, r_qkT = qkT_ring.next()
                    st, r_st = st_ring.next()
                    mv, r_mv = mv_ring.next()
                    nrm, r_nrm = nrm_ring.next()
                    sil, r_sil = sil_ring.next()
                    tp, r_tp = tp_ring.next()
                    po, r_po = po_ring.next()
                    ropt, r_rop = rope.next()
                    dma("sp", ropt[:], c_rope[:, rows, :].rearrange("a p n -> p a n"), writes=[r_rop])
                    qkt, r_qk = qk.next()
                    dma("sp", qkt[:], P[rows, 0:1024], writes=[r_qk])
                    rvt, r_rv = rv.next()
                    dma("sp", rvt[:], P[rows, 1024:1536], writes=[r_rv])
                    rgt, r_rg = rg.next()
                    dma("sp", rgt[:], P[rows, 1536:2048], writes=[r_rg])
                    for qi in range(2):
                        src = qkt[:, qi * 512:(qi + 1) * 512].rearrange("p (h d) -> p h d", h=4)
                        dst = qkr[:, qi * 512:(qi + 1) * 512].rearrange("p (h d) -> p h d", h=4)
                        a1, a2 = src[:, :, 0:64], src[:, :, 64:128]
                        cosv = ropt[:, 2 * qi, :].rearrange("p (h d) -> p h d", h=4)
                        sinv = ropt[:, 2 * qi + 1, :].rearrange("p (h d) -> p h d", h=4)
                        e1 = "dve" if qi == 0 else "pool"
                        S.op(e1, lambda e1=e1, a1=a1, cosv=cosv: E[e1].tensor_tensor(out=t1[:], in0=a1, in1=cosv, op=ALU.mult), [r_qk, r_rop], [r_t1])
                        S.op(e1, lambda e1=e1, a2=a2, sinv=sinv: E[e1].tensor_tensor(out=t2[:], in0=a2, in1=sinv, op=ALU.mult), [r_qk, r_rop], [r_t2])
                        S.op(e1, lambda e1=e1, dst=dst: E[e1].tensor_tensor(out=dst[:, :, 0:64], in0=t1[:], in1=t2[:], op=ALU.subtract), [r_t1, r_t2], [r_qkr])
                        S.op(e1, lambda e1=e1, a1=a1, sinv=sinv: E[e1].tensor_tensor(out=t3[:], in0=a1, in1=sinv, op=ALU.mult), [r_qk, r_rop], [r_t3])
                        S.op(e1, lambda e1=e1, a2=a2, cosv=cosv: E[e1].tensor_tensor(out=t4[:], in0=a2, in1=cosv, op=ALU.mult), [r_qk, r_rop], [r_t4])
                        S.op(e1, lambda e1=e1, dst=dst: E[e1].tensor_tensor(out=dst[:, :, 64:128], in0=t3[:], in1=t4[:], op=ALU.add), [r_t3, r_t4], [r_qkr])
                    for k in range(8):
                        S.op("pe", lambda k=k: nc.tensor.transpose(out=tp[:, k, :], in_=qkr[:, k * 128:(k + 1) * 128], identity=ident_b[:]),
                             [r_qkr, r_identb], [r_tp], signal=(k == 7))
                    S.op("act", lambda: nc.scalar.copy(out=qkT[:], in_=tp[:]), [r_tp], [r_qkT])
                    S.op("act", lambda rgt=rgt: nc.scalar.activation(out=sil[:], in_=rgt[:], func=AF.Silu), [r_rg], [r_sil])
                    yield
                    for h in range(4):
                        sTt, r_sT = sT.next()
                        S.op("pe", lambda h=h, sTt=sTt: nc.tensor.matmul(sTt[:], lhsT=qkT[:, 4 + h, :], rhs=qkT[:, h, :], start=True, stop=True),
                             [r_qkT], [r_sT])
                        smt, r_sm = sm.next()
                        S.op("dve", lambda sTt=sTt, smt=smt: nc.vector.tensor_tensor(out=smt[:], in0=sTt[:], in1=mask01[:], op=ALU.mult),
                             [r_sT, r_mask01], [r_sm])
                        S.op("pe", lambda h=h, smt=smt, rvt=rvt: nc.tensor.matmul(po[:, h * 128:(h + 1) * 128], lhsT=smt[:], rhs=rvt[:, h * 128:(h + 1) * 128],
                                                                         start=True, stop=False), [r_sm, r_rv], [r_po], signal=False)
                        S.op("pe", lambda h=h: nc.tensor.matmul(po[:, h * 128:(h + 1) * 128], lhsT=qkT[:, h, :], rhs=Sb[h][0][:],
                                                                start=False, stop=True), [r_qkT, Sb[h][1]], [r_po])
                        kvt, r_kv = kv.next()
                        S.op("pe", lambda h=h, kvt=kvt, rvt=rvt: nc.tensor.matmul(kvt[:], lhsT=qkr[:, 512 + h * 128:512 + (h + 1) * 128], rhs=rvt[:, h * 128:(h + 1) * 128],
                                                                         start=True, stop=True), [r_qkr, r_rv], [r_kv])
                        S.op("dve", lambda h=h, kvt=kvt: nc.vector.tensor_tensor(out=Tt[:], in0=Sf[h][0][:], in1=kvt[:], op=ALU.add),
                             [Sf[h][1], r_kv], [r_Tt])
                        S.op("act", lambda h=h: nc.scalar.activation(out=Sf[h][0][:], in_=Tt[:], func=AF.Copy, scale=g128[h]), [r_Tt], [Sf[h][1]])
                        S.op("act", lambda h=h: nc.scalar.activation(out=Sb[h][0][:], in_=Tt[:], func=AF.Copy, scale=g128[h]), [r_Tt], [Sb[h][1]])
                    yield
                    head_norm(lambda h: po[:, h * 128:(h + 1) * 128], 4, st, r_st, mv, r_mv, nrm, r_nrm, r_po, 0)
                    S.op("pool", lambda: nc.gpsimd.tensor_tensor(out=nrm[:], in0=nrm[:], in1=gvec[:], op=ALU.mult), [r_nrm, r_gvec], [r_nrm])
                    mo, r_mo = mixo.next()
                    S.op("pool", lambda mo=mo: nc.gpsimd.tensor_tensor(out=mo[:], in0=nrm[:], in1=sil[:], op=ALU.mult), [r_nrm, r_sil], [r_mo])
                    dma("sp", MIX[rows, 0:512], mo[:], reads=[r_mo])
                run_skewed(tile2, NT)
                S.barrier()
            if stop_phase <= 2:
                break

            with ExitStack() as es:
                wg, r_wg = mk(es, "wgg", [16, 384], F32)
                bg, r_bg = mk(es, "bgg", [1, 384], F32)
                gvec, r_gvec = mk(es, "gvec3", [128, 768], F32)
                gat = mk(es, "gat", [16, 128], F32, 2)
                gqk = mk(es, "gqk", [128, 768], BF16, 2)
                gv = mk(es, "gv", [128, 768], BF16, 2)
                gr = mk(es, "gr", [128, 768], BF16, 2)
                ez_ring = mk(es, "ez", [128, 384], F32, 2)
                lz_ring = mk(es, "lz", [128, 384], F32, 2)
                eb_ring = mk(es, "eb", [128, 384], F32, 2)
                enb_ring = mk(es, "enb", [128, 384], F32, 2)
                qkh_ring = mk(es, "qkh", [128, 768], BF16, 2)
                qkT_ring = mk(es, "qkT3", [128, 6, 128], BF16, 2)
                dec_ring = mk(es, "dec", [128, 4], F32, 2)
                sm = mk(es, "sm3", [128, 128], BF16, 2)
                Sf = [mk(es, "Sg%d" % p_, [128, 256], F32) for p_ in range(3)]
                Sb = [mk(es, "Sgb%d" % p_, [128, 256], BF16) for p_ in range(3)]
                Tt, r_Tt = mk(es, "Tt3", [128, 256], F32)
                st_ring = mk(es, "st3", [128, 6, 6], F32, 2)
                mv_ring = mk(es, "mv3", [128, 6, 3], F32, 2)
                nrm_ring = mk(es, "nrm3", [128, 768], F32, 2)
                sil_ring = mk(es, "sil3", [128, 768], F32, 2)
                mixo = mk(es, "mixo3", [128, 768], BF16, 2)
                pz, r_pz = mkp(es, "pz", [128, 512], F32)
                pl, r_pl = mkp(es, "pl", [128, 512], F32)
                tp, r_tp = mkp(es, "tp3", [128, 8, 128], BF16)
                pm, r_pm = mkp(es, "pm3", [128, 512], F32)
                poA, r_poA = mkp(es, "poA", [128, 512], F32)
                poB, r_poB = mkp(es, "poB", [128, 512], F32)
                pkv, r_pkv = mkp(es, "pkv", [128, 512], F32)
                r_sT = [Res(), Res()]
                r_bl = Res()
                r_kvh = [Res(), Res()]
                dma("sp", wg[:], w_gg[l], writes=[r_wg])
                dma("sp", bg[:], b_gg[l:l + 1, :], writes=[r_bg])
                dma("sp", gvec[:], gla_g[l:l + 1, :].partition_broadcast(128), writes=[r_gvec])
                for p_ in range(3):
                    S.op("dve", lambda p_=p_: nc.vector.memset(Sf[p_][0][:], 0.0), [], [Sf[p_][1]])
                    S.op("dve", lambda p_=p_: nc.vector.memset(Sb[p_][0][:], 0.0), [], [Sb[p_][1]])

                def po_ap(h):
                    return poA[:, h * 128:(h + 1) * 128] if h < 4 else poB[:, (h - 4) * 128:(h - 3) * 128]

                def r_poh(h):
                    return r_poA if h < 4 else r_poB
                def tile3(j):
                    rows = slice(j * 128, (j + 1) * 128)
                    ez, r_ez = ez_ring.next()
                    lz, r_lz = lz_ring.next()
                    eb, r_eb = eb_ring.next()
                    enb, r_enb = enb_ring.next()
                    qkh, r_qkh = qkh_ring.next()
                    qkT, r_qkT = qkT_ring.next()
                    dec, r_dec = dec_ring.next()
                    st, r_st = st_ring.next()
                    mv, r_mv = mv_ring.next()
                    nrm, r_nrm = nrm_ring.next()
                    sil, r_sil = sil_ring.next()
                    gatt, r_gat = gat.next()
                    dma("sp", gatt[:], GAT[:, rows], writes=[r_gat])
                    gqkt, r_gqk = gqk.next()
                    dma("sp", gqkt[:], P[rows, 2816:3584], writes=[r_gqk])
                    gvt, r_gv = gv.next()
                    dma("sp", gvt[:], P[rows, 3584:4352], writes=[r_gv])
                    grt, r_gr = gr.next()
                    dma("sp", grt[:], P[rows, 4352:5120], writes=[r_gr])
                    S.op("pe", lambda gatt=gatt: nc.tensor.matmul(pz[:, 0:384], lhsT=gatt[:], rhs=wg[:], start=True, stop=False),
                         [r_gat, r_wg], [r_pz], signal=False)
                    S.op("pe", lambda: nc.tensor.matmul(pz[:, 0:384], lhsT=ones_f[0:1, :], rhs=bg[:], start=False, stop=True),
                         [r_onesf, r_bg], [r_pz])
                    S.op("act", lambda: nc.scalar.activation(out=ez[:], in_=pz[:, 0:384], func=AF.Exp, scale=-1.0), [r_pz], [r_ez])
                    S.op("act", lambda: nc.scalar.activation(out=lz[:], in_=ez[:], func=AF.Ln, bias=1.0, scale=1.0), [r_ez], [r_lz])
                    S.op("pe", lambda: nc.tensor.matmul(pl[:, 0:384], lhsT=mask01[:], rhs=lz[:], start=True, stop=True),
                         [r_mask01, r_lz], [r_pl])
                    for p_ in range(3):
                        S.op("pe", lambda p_=p_: nc.tensor.matmul(pm[:, 256 + p_:257 + p_], lhsT=lz[:, p_ * 128:(p_ + 1) * 128], rhs=ones_f[:, 0:1],
                                                                  start=True, stop=True), [r_lz, r_onesf], [r_bl], signal=(p_ == 2))
                    S.op("act", lambda: nc.scalar.activation(out=eb[:], in_=pl[:, 0:384], func=AF.Exp, scale=-1.0 / 16.0), [r_pl], [r_eb])
                    S.op("act", lambda: nc.scalar.activation(out=enb[:], in_=pl[:, 0:384], func=AF.Exp, scale=1.0 / 16.0), [r_pl], [r_enb])
                    S.op("act", lambda: nc.scalar.activation(out=dec[:, 0:3], in_=pm[:, 256:259], func=AF.Exp, scale=-1.0 / 16.0), [r_bl], [r_dec])
                    S.op("dve", lambda gqkt=gqkt: nc.vector.scalar_tensor_tensor(out=qkh[:, 0:384], in0=gqkt[:, 0:384], scalar=0.125, in1=eb[:],
                                                                                op0=ALU.mult, op1=ALU.mult), [r_gqk, r_eb], [r_qkh])
                    S.op("dve", lambda gqkt=gqkt: nc.vector.tensor_tensor(out=qkh[:, 384:768], in0=gqkt[:, 384:768], in1=enb[:], op=ALU.mult),
                         [r_gqk, r_enb], [r_qkh])
                    for k in range(6):
                        S.op("pe", lambda k=k: nc.tensor.transpose(out=tp[:, k, :], in_=qkh[:, k * 128:(k + 1) * 128], identity=ident_b[:]),
                             [r_qkh, r_identb], [r_tp], signal=(k == 5))
                    S.op("act", lambda: nc.scalar.copy(out=qkT[:], in_=tp[:, 0:6, :]), [r_tp], [r_qkT])
                    S.op("act", lambda grt=grt: nc.scalar.activation(out=sil[:], in_=grt[:], func=AF.Silu), [r_gr], [r_sil])
                    yield
                    for h in range(6):
                        p_, hh = h // 2, h % 2
                        R = slice(hh * 64, (hh + 1) * 64)
                        sTa = pm[:, (h % 2) * 128:(h % 2 + 1) * 128]
                        rs = r_sT[h % 2]
                        S.op("pe", lambda p_=p_, R=R, sTa=sTa: nc.tensor.matmul(sTa, lhsT=qkT[R, 3 + p_, :], rhs=qkT[R, p_, :], start=True, stop=True),
                             [r_qkT], [rs])
                        smt, r_sm = sm.next()
                        S.op("dve", lambda sTa=sTa, smt=smt: nc.vector.tensor_tensor(out=smt[:], in0=sTa, in1=mask01[:], op=ALU.mult),
                             [rs, r_mask01], [r_sm])
                        S.op("pe", lambda h=h, smt=smt, gvt=gvt: nc.tensor.matmul(po_ap(h), lhsT=smt[:], rhs=gvt[:, h * 128:(h + 1) * 128],
                                                                         start=True, stop=False), [r_sm, r_gv], [r_poh(h)], signal=False)
                        S.op("pe", lambda h=h, p_=p_, R=R, hh=hh: nc.tensor.matmul(po_ap(h), lhsT=qkT[R, p_, :], rhs=Sb[p_][0][R, hh * 128:(hh + 1) * 128],
                                                                          start=False, stop=True), [r_qkT, Sb[p_][1]], [r_poh(h)])
                    for p_ in range(3):
                        kva = pkv[:, (p_ % 2) * 256:(p_ % 2 + 1) * 256]
                        rk = r_kvh[p_ % 2]
                        S.op("pe", lambda p_=p_, kva=kva, gvt=gvt: nc.tensor.matmul(kva, lhsT=qkh[:, 384 + p_ * 128:384 + (p_ + 1) * 128], rhs=gvt[:, p_ * 256:(p_ + 1) * 256],
                                                                          start=True, stop=True), [r_qkh, r_gv], [rk])
                        S.op("dve", lambda p_=p_, kva=kva: nc.vector.tensor_tensor(out=Tt[:], in0=Sf[p_][0][:], in1=kva, op=ALU.add),
                             [Sf[p_][1], rk], [r_Tt])
                        S.op("act", lambda p_=p_: nc.scalar.activation(out=Sf[p_][0][:], in_=Tt[:], func=AF.Copy, scale=dec[:, p_:p_ + 1]), [r_Tt, r_dec], [Sf[p_][1]])
                        S.op("act", lambda p_=p_: nc.scalar.activation(out=Sb[p_][0][:], in_=Tt[:], func=AF.Copy, scale=dec[:, p_:p_ + 1]), [r_Tt, r_dec], [Sb[p_][1]])
                    yield
                    r_pob = Res()
                    for h in range(6):
                        S.op("dve", lambda h=h: nc.vector.bn_stats(out=st[:, h, :], in_=po_ap(h)), [r_poh(h)], [r_st])
                        S.op("dve", lambda h=h: nc.vector.bn_aggr(out=mv[:, h, 0:2], in_=st[:, h, :]), [r_st], [r_mv])
                    rstd(mv[:, :, 2:3], mv[:, :, 1:2], r_mv)
                    for h in range(6):
                        S.op("dve", lambda h=h: nc.vector.tensor_scalar(out=nrm[:, h * 128:(h + 1) * 128], in0=po_ap(h), scalar1=mv[:, h, 0:1],
                                                                        scalar2=mv[:, h, 2:3], op0=ALU.subtract, op1=ALU.mult),
                             [r_poh(h), r_mv], [r_nrm])
                    S.op("pool", lambda: nc.gpsimd.tensor_tensor(out=nrm[:], in0=nrm[:], in1=gvec[:], op=ALU.mult), [r_nrm, r_gvec], [r_nrm])
                    mo, r_mo = mixo.next()
                    S.op("pool", lambda mo=mo: nc.gpsimd.tensor_tensor(out=mo[:], in0=nrm[:], in1=sil[:], op=ALU.mult), [r_nrm, r_sil], [r_mo])
                    dma("sp", MIX[rows, 1280:2048], mo[:], reads=[r_mo])
                run_skewed(tile3, NT)
                S.barrier()
            if stop_phase <= 3:
                break

            with ExitStack() as es:
                QT, r_QT = mk(es, "QT", [128, 6, SEQ], BF16)
                KT, r_KT = mk(es, "KT", [128, 6, SEQ], BF16)
                dm0, r_dm0 = mk(es, "dm0", [128, 256], F32)
                dm1, r_dm1 = mk(es, "dm1", [128, 256], F32)
                mb0, r_mb0 = mk(es, "mb0", [128, 256], BF16)
                mb1, r_mb1 = mk(es, "mb1", [128, 256], BF16)
                V = mk(es, "V", [128, 768], BF16, 5)
                negm = mk(es, "negm", [128, 3], F32, 3)
                pexp = mk(es, "pexp", [128, 3, 256], BF16, 3)
                pTs = mk(es, "pTs", [128, 3, 256], BF16, 3)
                stage = Ring([(es.enter_context(nc.sbuf_tensor("dstage%d_%d" % (k, l), [128, 6, 130], F32)), [Res() for _ in range(2)]) for k in range(4)])
                ps_s = mkp(es, "ps_s", [128, 4, 256], F32, 2)
                ps_t = mkp(es, "ps_t", [128, 4, 256], BF16, 2)
                ps_o = mkp(es, "ps_o", [128, 4, 128], F32, 2)
                dma("sp", dm0[:], c_dmask0, writes=[r_dm0])
                dma("sp", dm1[:], c_dmask1, writes=[r_dm1])
                S.op("dve", lambda: nc.vector.tensor_copy(out=mb0[:], in_=dm0[:]), [r_dm0], [r_mb0])
                S.op("dve", lambda: nc.vector.tensor_copy(out=mb1[:], in_=dm1[:]), [r_dm1], [r_mb1])
                for h in range(6):
                    dma("sp", QT[:, h, :], PT[h * 128:(h + 1) * 128, :], writes=[r_QT])
                    dma("sp", KT[:, h, :], PT[768 + h * 128:768 + (h + 1) * 128, :], writes=[r_KT])
                batches = []
                for pi, dil in enumerate((1, 4, 16)):
                    nb = SEQ // (dil * 128)
                    for r in range(dil):
                        for n in range(nb):
                            for hb in range(2):
                                batches.append((pi, dil, r, n, hb))
                ctxs = {}
                ust = {}

                def sA(b):
                    pi, dil, r, n, hb = batches[b]
                    c = ctxs[b] = {}
                    row0 = n * 128 * dil + r
                    rsl = slice(row0, row0 + 127 * dil + 1, dil)
                    c["rsl"] = rsl
                    if hb == 0:
                        vt, r_v = V.next()
                        dma("sp", vt[:], P[rsl, 2048:2816], writes=[r_v])
                        vprev = ust.get("vprev") if n > 0 else (vt, r_v)
                        stg, r_stg = stage.next()
                        ust["cur"] = (vt, r_v, vprev, stg, r_stg)
                        ust["vprev"] = (vt, r_v)
                    c["u"] = ust["cur"]
                    h0 = hb * 3
                    pst, r_ps = ps_s.next()
                    c["pst"] = (pst, r_ps)
                    for hh in range(3):
                        h = h0 + hh
                        q_ap = QT[:, h, rsl]
                        if n == 0:
                            S.op("pe", lambda: nc.tensor.matmul(pst[:, hh, 128:256], lhsT=q_ap, rhs=KT[:, h, rsl], start=True, stop=False),
                                 [r_QT, r_KT], [r_ps], signal=False)
                            S.op("pe", lambda: nc.tensor.matmul(pst[:, hh, :], lhsT=ident_b[:], rhs=mb0[:], start=False, stop=True),
                                 [r_identb, r_mb0], [r_ps], signal=(hh == 2))
                        else:
                            ksl = slice(row0 - 128 * dil, row0 + 127 * dil + 1, dil)
                            S.op("pe", lambda: nc.tensor.matmul(pst[:, hh, :], lhsT=q_ap, rhs=KT[:, h, ksl], start=True, stop=False),
                                 [r_QT, r_KT], [r_ps], signal=False)
                            S.op("pe", lambda: nc.tensor.matmul(pst[:, hh, :], lhsT=ident_b[:], rhs=mb1[:], start=False, stop=True),
                                 [r_identb, r_mb1], [r_ps], signal=(hh == 2))

                def sB(b):
                    pi, dil, r, n, hb = batches[b]
                    c = ctxs[b]
                    h0 = hb * 3
                    pst, r_ps = c["pst"]
                    vt, r_v, vprev, stg, r_stg = c["u"]
                    S.op("dve", lambda: nc.vector.reduce_max(out=stg[:, h0:h0 + 3, 128], in_=pst[:, 0:3, :], axis=AX.X), [r_ps], [r_stg[hb]])
                    ngt, r_ng = negm.next()
                    S.op("dve", lambda: nc.vector.tensor_scalar(out=ngt[:], in0=stg[:, h0:h0 + 3, 128], scalar1=-1.0, scalar2=None, op0=ALU.mult),
                         [r_stg[hb]], [r_ng])
                    pet, r_pe = pexp.next()
                    c["pet"] = (pet, r_pe)
                    for hh in range(3):
                        S.op("act", lambda: nc.scalar.activation(out=pet[:, hh, :], in_=pst[:, hh, :], func=AF.Exp, bias=ngt[:, hh:hh + 1], scale=1.0,
                                                                 accum_out=stg[:, h0 + hh, 129:130]),
                             [r_ps, r_ng], [r_pe, r_stg[hb]])

                def sC(b):
                    c = ctxs[b]
                    pet, r_pe = c["pet"]
                    ptt, r_pt = ps_t.next()
                    for hh in range(3):
                        for kk in range(2):
                            S.op("pe", lambda: nc.tensor.transpose(out=ptt[:, hh, kk * 128:(kk + 1) * 128], in_=pet[:, hh, kk * 128:(kk + 1) * 128], identity=ident_b[:]),
                                 [r_pe, r_identb], [r_pt], signal=(hh == 2 and kk == 1))
                    pts, r_pts = pTs.next()
                    c["pts"] = (pts, r_pts)
                    S.op("dve", lambda: nc.vector.tensor_copy(out=pts[:], in_=ptt[:, 0:3, :]), [r_pt], [r_pts])

                def sD(b):
                    pi, dil, r, n, hb = batches[b]
                    c = ctxs.pop(b)
                    h0 = hb * 3
                    pts, r_pts = c["pts"]
                    vt, r_v, vprev, stg, r_stg = c["u"]
                    pot, r_po2 = ps_o.next()
                    for hh in range(3):
                        h = h0 + hh
                        S.op("pe", lambda: nc.tensor.matmul(pot[:, hh, :], lhsT=pts[:, hh, 0:128], rhs=vprev[0][:, h * 128:(h + 1) * 128], start=True, stop=False),
                             [r_pts, vprev[1]], [r_po2], signal=False)
                        S.op("pe", lambda: nc.tensor.matmul(pot[:, hh, :], lhsT=pts[:, hh, 128:256], rhs=vt[:, h * 128:(h + 1) * 128], start=False, stop=True),
                             [r_pts, r_v], [r_po2], signal=(hh == 2))
                    S.op("act", lambda: nc.scalar.copy(out=stg[:, h0:h0 + 3, 0:128], in_=pot[:, 0:3, :]), [r_po2], [r_stg[hb]])
                    if hb == 1:
                        dma("sp", DO[pi, c["rsl"], :], stg[:].rearrange("p a b -> p (a b)"), reads=r_stg)

                nbt = len(batches)
                for t in range(nbt + 3):
                    if 0 <= t - 3 < nbt:
                        sD(t - 3)
                    if 0 <= t - 2 < nbt:
                        sC(t - 2)
                    if 0 <= t - 1 < nbt:
                        sB(t - 1)
                    if t < nbt:
                        sA(t)
                S.barrier()
            with ExitStack() as es:
                D3 = mk(es, "D3", [128, 3, 780], F32, 2)
                mxx, r_mxx = mk(es, "mxx", [128, 6], F32)
                e3, r_e3 = mk(es, "e3", [128, 3, 6], F32)
                w3, r_w3 = mk(es, "w3", [128, 3, 6], F32)
                dn, r_dn = mk(es, "dn", [128, 6], F32)
                cf, r_cf = mk(es, "cf", [128, 3, 6], F32)
                acc = [mk(es, "dacc%d" % h, [128, 128], F32) for h in range(6)]
                outb = Ring([(es.enter_context(nc.sbuf_tensor("doutb%d_%d" % (k, l), [128, 768], BF16)), [Res() for _ in range(6)]) for k in range(2)])
                for j in range(NT):
                    rows = slice(j * 128, (j + 1) * 128)
                    d3, r_d3 = D3.next()
                    dma("sp", d3[:], DO[:, rows, :].rearrange("a p n -> p a n"), writes=[r_d3])
                    d4 = d3[:].rearrange("p a (h c) -> p a h c", h=6)
                    S.op("dve", lambda d4=d4: nc.vector.tensor_tensor(out=mxx[:], in0=d4[:, 0, :, 128], in1=d4[:, 1, :, 128], op=ALU.max), [r_d3], [r_mxx])
                    S.op("dve", lambda d4=d4: nc.vector.tensor_tensor(out=mxx[:], in0=mxx[:], in1=d4[:, 2, :, 128], op=ALU.max), [r_d3, r_mxx], [r_mxx])
                    for p_ in range(3):
                        S.op("dve", lambda d4=d4, p_=p_: nc.vector.tensor_tensor(out=e3[:, p_, :], in0=d4[:, p_, :, 128], in1=mxx[:], op=ALU.subtract), [r_d3, r_mxx], [r_e3])
                    S.op("act", lambda: nc.scalar.activation(out=e3[:], in_=e3[:], func=AF.Exp), [r_e3], [r_e3])
                    for p_ in range(3):
                        S.op("dve", lambda d4=d4, p_=p_: nc.vector.tensor_tensor(out=w3[:, p_, :], in0=e3[:, p_, :], in1=d4[:, p_, :, 129], op=ALU.mult), [r_d3, r_e3], [r_w3])
                    S.op("dve", lambda: nc.vector.tensor_tensor(out=dn[:], in0=w3[:, 0, :], in1=w3[:, 1, :], op=ALU.add), [r_w3], [r_dn])
                    S.op("dve", lambda: nc.vector.tensor_tensor(out=dn[:], in0=dn[:], in1=w3[:, 2, :], op=ALU.add), [r_w3, r_dn], [r_dn])
                    S.op("dve", lambda: nc.vector.reciprocal(out=dn[:], in_=dn[:]), [r_dn], [r_dn])
                    for p_ in range(3):
                        S.op("dve", lambda p_=p_: nc.vector.tensor_tensor(out=cf[:, p_, :], in0=e3[:, p_, :], in1=dn[:], op=ALU.mult), [r_e3, r_dn], [r_cf])
                    ob, r_ob = outb.next()
                    for h in range(6):
                        eng = "dve"
                        at, r_at = acc[h]
                        S.op(eng, lambda eng=eng, at=at, d4=d4, h=h: E[eng].tensor_scalar(out=at[:], in0=d4[:, 0, h, 0:128], scalar1=cf[:, 0, h:h + 1], scalar2=None, op0=ALU.mult),
                             [r_d3, r_cf], [r_at])
                        S.op(eng, lambda eng=eng, at=at, d4=d4, h=h: E[eng].scalar_tensor_tensor(out=at[:], in0=d4[:, 1, h, 0:128], scalar=cf[:, 1, h:h + 1], in1=at[:], op0=ALU.mult, op1=ALU.add),
                             [r_d3, r_cf, r_at], [r_at])
                        S.op(eng, lambda eng=eng, at=at, d4=d4, h=h, ob=ob: E[eng].scalar_tensor_tensor(out=ob[:, h * 128:(h + 1) * 128], in0=d4[:, 2, h, 0:128], scalar=cf[:, 2, h:h + 1], in1=at[:], op0=ALU.mult, op1=ALU.add),
                             [r_d3, r_cf, r_at], [r_ob[h]])
                    dma("sp", MIX[rows, 512:1280], ob[:], reads=r_ob)
                S.barrier()
            if stop_phase <= 4:
                break

            with ExitStack() as es:
                wo, r_wo = mk(es, "wo", [128, 16, DM], BF16)
                g1, r_g1 = mk(es, "g1", [128, DM], F32)
                b1, r_b1 = mk(es, "b1", [128, DM], F32)
                wr, r_wr = mk(es, "wr", [128, 16, 36], F32)
                br, r_br = mk(es, "br", [1, 36], F32)
                mixl = mk(es, "mixl", [128, DM], BF16, 2)
                mT_ring = mk(es, "mT", [128, 16, 128], BF16, 2)
                xr = mk(es, "xr", [128, DM], F32, 2)
                u_ring = mk(es, "u5", [128, DM], F32, 2)
                x1 = mk(es, "x1t", [128, DM], F32, 3)
                x1b = mk(es, "x1bt", [128, DM], BF16, 2)
                x1T_ring = mk(es, "x1T", [128, 16, 128], F32, 2)
                st_ring = mk(es, "st5", [128, 4, 6], F32, 2)
                mv_ring = mk(es, "mv5", [128, 3], F32, 2)
                L_ring = mk(es, "L", [128, 36], F32, 2)
                sc_ring = mk(es, "sc5", [128, 16], F32, 2)
                goh_ring = mk(es, "goh", [128, 4], F32, 2)
                gex_ring = mk(es, "gex", [128, 4], F32, 2)
                pen_ring = mk(es, "pen", [128, 4], F32, 2)
                em_ring = mk(es, "em", [128, 32], F32, 2)
                em2_ring = mk(es, "em2", [128, 32], F32, 2)
                tp = mkp(es, "tp5", [128, 512], BF16, 2)
                po = mkp(es, "po5", [128, 512], F32, 4)
                tpf, r_tpf = mkp(es, "tpf", [128, 512], F32)
                plog, r_plog = mkp(es, "plog", [128, 64], F32)
                dma("pool", wo[:], w_out[l].rearrange("(k p) n -> p k n", p=128), writes=[r_wo])
                dma("sp", g1[:], ln1_g[l:l + 1, :].partition_broadcast(128), writes=[r_g1])
                dma("sp", b1[:], ln1_b[l:l + 1, :].partition_broadcast(128), writes=[r_b1])
                dma("sp", wr[:], w_rt[l].rearrange("(k p) n -> p k n", p=128), writes=[r_wr])
                dma("sp", br[:], b_rt[l:l + 1, :], writes=[r_br])
                def tile5(j):
                    rows = slice(j * 128, (j + 1) * 128)
                    mT, r_mT = mT_ring.next()
                    u, r_u = u_ring.next()
                    x1T, r_x1T = x1T_ring.next()
                    st, r_st = st_ring.next()
                    mv, r_mv = mv_ring.next()
                    L, r_L = L_ring.next()
                    sc, r_sc = sc_ring.next()
                    goh, r_goh = goh_ring.next()
                    gex, r_gex = gex_ring.next()
                    pen, r_pen = pen_ring.next()
                    em, r_em = em_ring.next()
                    em2, r_em2 = em2_ring.next()
                    ml, r_ml = mixl.next()
                    dma("sp", ml[:], MIX[rows, :], writes=[r_ml])
                    xrt, r_xr = xr.next()
                    dma("sp", xrt[:], XCUR[rows, :], writes=[r_xr])
                    for g4 in range(4):
                        tpt, r_tp = tp.next()
                        for k in range(4):
                            kc = g4 * 4 + k
                            S.op("pe", lambda kc=kc, k=k, tpt=tpt, ml=ml: nc.tensor.transpose(out=tpt[:, k * 128:(k + 1) * 128], in_=ml[:, kc * 128:(kc + 1) * 128], identity=ident_b[:]),
                                 [r_ml, r_identb], [r_tp], signal=(k == 3))
                        eng = alt("act", "dve")
                        S.op(eng, lambda eng=eng, tpt=tpt, g4=g4: ecopy(eng, mT[:, g4 * 4:(g4 + 1) * 4, :], tpt[:].rearrange("p (k t) -> p k t", k=4)), [r_tp], [r_mT])
                    for n4 in range(4):
                        pot, r_po5 = po.next()
                        for kc in range(16):
                            S.op("pe", lambda kc=kc, pot=pot, n4=n4: nc.tensor.matmul(pot[:], lhsT=mT[:, kc, :], rhs=wo[:, kc, n4 * 512:(n4 + 1) * 512], start=(kc == 0), stop=(kc == 15)),
                                 [r_mT, r_wo], [r_po5], signal=(kc == 15))
                        S.op("dve", lambda pot=pot, n4=n4, xrt=xrt: nc.vector.scalar_tensor_tensor(out=u[:, n4 * 512:(n4 + 1) * 512], in0=xrt[:, n4 * 512:(n4 + 1) * 512], scalar=ALPHA,
                                                                                                  in1=pot[:], op0=ALU.mult, op1=ALU.add), [r_xr, r_po5], [r_u])
                    yield
                    x1t, r_x1 = x1.next()
                    layer_norm_tile(None, u, r_u, g1, r_g1, b1, r_b1, st, r_st, mv, r_mv, x1t, r_x1)
                    dma("sp", X1[rows, :], x1t[:], reads=[r_x1])
                    xbt, r_xb1 = x1b.next()
                    S.op("act", lambda xbt=xbt, x1t=x1t: nc.scalar.copy(out=xbt[:], in_=x1t[:]), [r_x1], [r_xb1])
                    dma("sp", X1B[rows, :], xbt[:], reads=[r_xb1])
                    yield
                    for g4 in range(4):
                        for k in range(4):
                            kc = g4 * 4 + k
                            S.op("pe", lambda kc=kc, k=k, x1t=x1t: nc.tensor.transpose(out=tpf[:, k * 128:(k + 1) * 128], in_=x1t[:, kc * 128:(kc + 1) * 128], identity=ident_f[:]),
                                 [r_x1, r_identf], [r_tpf], signal=(k == 3))
                        eng = alt("act", "dve")
                        S.op(eng, lambda eng=eng, g4=g4: ecopy(eng, x1T[:, g4 * 4:(g4 + 1) * 4, :], tpf[:].rearrange("p (k t) -> p k t", k=4)), [r_tpf], [r_x1T])
                    for kc in range(16):
                        S.op("pe", lambda kc=kc: nc.tensor.matmul(plog[:, 0:36], lhsT=x1T[:, kc, :], rhs=wr[:, kc, :], start=(kc == 0), stop=False), [r_x1T, r_wr], [r_plog], signal=False)
                    S.op("pe", lambda: nc.tensor.matmul(plog[:, 0:36], lhsT=ones_f[0:1, :], rhs=br[:], start=False, stop=True), [r_onesf, r_br], [r_plog])
                    S.op("act", lambda: nc.scalar.copy(out=L[:], in_=plog[:, 0:36]), [r_plog], [r_L])
                    yield
                    V_ = nc.vector
                    S.op("dve", lambda: V_.reduce_max(out=sc[:, 0:1], in_=L[:, 0:4], axis=AX.X), [r_L], [r_sc])
                    S.op("dve", lambda: V_.tensor_scalar(out=goh[:], in0=L[:, 0:4], scalar1=sc[:, 0:1], scalar2=None, op0=ALU.is_ge), [r_L, r_sc], [r_goh])
                    S.op("dve", lambda: V_.tensor_scalar(out=sc[:, 1:2], in0=sc[:, 0:1], scalar1=-1.0, scalar2=None, op0=ALU.mult), [r_sc], [r_sc])
                    S.op("act", lambda: nc.scalar.activation(out=gex[:], in_=L[:, 0:4], func=AF.Exp, bias=sc[:, 1:2], scale=1.0, accum_out=sc[:, 2:3]), [r_L, r_sc], [r_gex, r_sc])
                    S.op("dve", lambda: V_.reciprocal(out=sc[:, 3:4], in_=sc[:, 2:3]), [r_sc], [r_sc])
                    S.op("dve", lambda: V_.tensor_scalar(out=pen[:], in0=goh[:], scalar1=BIG, scalar2=-BIG, op0=ALU.mult, op1=ALU.add), [r_goh], [r_pen])
                    for g in range(4):
                        S.op("dve", lambda g=g: V_.tensor_scalar(out=em[:, g * 8:(g + 1) * 8], in0=L[:, 4 + g * 8:4 + (g + 1) * 8], scalar1=pen[:, g:g + 1], scalar2=None, op0=ALU.add),
                             [r_L, r_pen], [r_em])
                    S.op("dve", lambda: V_.reduce_max(out=sc[:, 4:5], in_=em[:], axis=AX.X), [r_em], [r_sc])
                    S.op("dve", lambda j=j: V_.tensor_scalar(out=OH[:, j, 0, :], in0=em[:], scalar1=sc[:, 4:5], scalar2=None, op0=ALU.is_ge), [r_em, r_sc], [r_OH])
                    S.op("dve", lambda j=j: V_.scalar_tensor_tensor(out=em2[:], in0=OH[:, j, 0, :], scalar=-BIG, in1=em[:], op0=ALU.mult, op1=ALU.add), [r_OH, r_em], [r_em2])
                    S.op("dve", lambda: V_.reduce_max(out=sc[:, 5:6], in_=em2[:], axis=AX.X), [r_em2], [r_sc])
                    S.op("dve", lambda j=j: V_.tensor_scalar(out=OH[:, j, 1, :], in0=em2[:], scalar1=sc[:, 5:6], scalar2=None, op0=ALU.is_ge), [r_em2, r_sc], [r_OH])
                    S.op("dve", lambda: V_.tensor_tensor(out=sc[:, 6:7], in0=sc[:, 5:6], in1=sc[:, 4:5], op=ALU.subtract), [r_sc], [r_sc])
                    S.op("act", lambda: nc.scalar.activation(out=sc[:, 7:8], in_=sc[:, 6:7], func=AF.Exp), [r_sc], [r_sc])
                    S.op("dve", lambda: V_.tensor_scalar(out=sc[:, 8:9], in0=sc[:, 7:8], scalar1=1.0, scalar2=None, op0=ALU.add), [r_sc], [r_sc])
                    S.op("dve", lambda: V_.reciprocal(out=sc[:, 9:10], in_=sc[:, 8:9]), [r_sc], [r_sc])
                    S.op("dve", lambda j=j: V_.tensor_tensor(out=GATE[:, j, 0:1], in0=sc[:, 3:4], in1=sc[:, 9:10], op=ALU.mult), [r_sc], [r_GATE])
                    S.op("dve", lambda: V_.tensor_tensor(out=sc[:, 10:11], in0=sc[:, 7:8], in1=sc[:, 9:10], op=ALU.mult), [r_sc], [r_sc])
                    S.op("dve", lambda j=j: V_.tensor_tensor(out=GATE[:, j, 1:2], in0=sc[:, 3:4], in1=sc[:, 10:11], op=ALU.mult), [r_sc], [r_GATE])
                    S.op("dve", lambda j=j: V_.tensor_tensor(out=A_b[:, j, :], in0=OH[:, j, 0, :], in1=OH[:, j, 1, :], op=ALU.add), [r_OH], [r_Ab])
                run_skewed(tile5, NT)
                S.barrier()
            if stop_phase <= 5:
                break

            with ExitStack() as es:
                V_ = nc.vector
                cnt, r_cnt = mk(es, "cnt", [128, 32], F32)
                pc, r_pc = mk(es, "pc", [128, 32], F32)
                pa, r_pa = mk(es, "pa", [128, 32], F32)
                pb, r_pb = mk(es, "pb", [128, 32], F32)
                pstart, r_pstart = mk(es, "pstart", [128, 32], F32)
                carry, r_carry = mk(es, "carry", [128, 32], F32)
                pos, r_pos = mk(es, "pos", [128, 32], F32)
                tmp, r_tmp = mk(es, "tmp6", [128, 32], F32)
                destf, r_destf = mk(es, "destf", [128, NT, 2], F32)
                EB, r_EB = mk(es, "EB", [128, NBLK], F32)
                bsg, r_bsg = mk(es, "bsg", [128, NBLK], F32)
                bsd, r_bsd = mk(es, "bsd", [128, NBLK], F32)
                ioi, r_ioi = mk(es, "ioi", [128, 1], I32)
                iof, r_iof = mk(es, "iof", [128, 1], F32)
                iof4, r_iof4 = mk(es, "iof4", [128, 1], F32)
                strib, r_strib = mk(es, "strib", [128, 128], BF16)
                strif, r_strif = mk(es, "strif", [128, 128], F32)
                xs = mk(es, "xs6", [128, DM], BF16, 3)
                ptot, r_ptot = mkp(es, "ptot", [128, 64], F32)
                prk = mkp(es, "prk", [128, 64], F32, 2)
                dma("sp", strif[:], c_stri, writes=[r_strif])
                S.op("dve", lambda: V_.tensor_copy(out=strib[:], in_=strif[:]), [r_strif], [r_strib])
                S.op("pool", lambda: nc.gpsimd.iota(ioi[:], pattern=[[0, 1]], base=0, channel_multiplier=1), [], [r_ioi])
                S.op("dve", lambda: V_.tensor_copy(out=iof[:], in_=ioi[:]), [r_ioi], [r_iof])
                for j in range(NT):
                    S.op("pe", lambda j=j: nc.tensor.matmul(ptot[:, 0:32], lhsT=ones_b[:], rhs=A_b[:, j, :], start=(j == 0), stop=(j == NT - 1)), [r_onesb, r_Ab], [r_ptot], signal=(j == NT - 1))
                S.op("dve", lambda: V_.tensor_copy(out=cnt[:], in_=ptot[:, 0:32]), [r_ptot], [r_cnt])
                S.op("dve", lambda: V_.memset(tmp[:], 0.0), [], [r_tmp])
                for m_ in range(-(-SEQ // BLK)):
                    S.op("dve", lambda m_=m_: V_.scalar_tensor_tensor(out=tmp[:], in0=cnt[:], scalar=float(m_ * BLK), in1=tmp[:], op0=ALU.is_gt, op1=ALU.add), [r_cnt, r_tmp], [r_tmp])
                S.op("dve", lambda: V_.tensor_scalar(out=pc[:], in0=tmp[:], scalar1=float(BLK), scalar2=None, op0=ALU.mult), [r_tmp], [r_pc])
                S.op("dve", lambda: V_.tensor_copy(out=pa[:], in_=pc[:]), [r_pc], [r_pa])
                cur, r_cur, oth, r_oth = pa, r_pa, pb, r_pb
                for sh in (1, 2, 4, 8, 16):
                    S.op("dve", lambda cur=cur, oth=oth, sh=sh: V_.tensor_copy(out=oth[:, 0:sh], in_=cur[:, 0:sh]), [r_cur], [r_oth])
                    S.op("dve", lambda cur=cur, oth=oth, sh=sh: V_.tensor_tensor(out=oth[:, sh:32], in0=cur[:, sh:32], in1=cur[:, 0:32 - sh], op=ALU.add), [r_cur], [r_oth])
                    cur, r_cur, oth, r_oth = oth, r_oth, cur, r_cur
                pend, r_pend = cur, r_cur
                S.op("dve", lambda: V_.tensor_tensor(out=pstart[:], in0=pend[:], in1=pc[:], op=ALU.subtract), [r_pend, r_pc], [r_pstart])
                S.op("dve", lambda: V_.memset(carry[:], 0.0), [], [r_carry])
                for j in range(NT):
                    prt, r_pr = prk.next()
                    S.op("pe", lambda prt=prt, j=j: nc.tensor.matmul(prt[:, 0:32], lhsT=strib[:], rhs=A_b[:, j, :], start=True, stop=True), [r_strib, r_Ab], [r_pr], signal=False)
                    S.op("pe", lambda prt=prt, j=j: nc.tensor.matmul(prt[:, 32:64], lhsT=ones_b[:], rhs=A_b[:, j, :], start=True, stop=True), [r_onesb, r_Ab], [r_pr])
                    S.op("dve", lambda prt=prt: V_.tensor_tensor(out=pos[:], in0=prt[:, 0:32], in1=carry[:], op=ALU.add), [r_pr, r_carry], [r_pos])
                    S.op("dve", lambda: V_.tensor_tensor(out=pos[:], in0=pos[:], in1=pstart[:], op=ALU.add), [r_pos, r_pstart], [r_pos])
                    for k in range(2):
                        S.op("dve", lambda j=j, k=k: V_.tensor_tensor(out=tmp[:], in0=OH[:, j, k, :], in1=pos[:], op=ALU.mult), [r_OH, r_pos], [r_tmp])
                        S.op("dve", lambda j=j, k=k: V_.reduce_sum(out=destf[:, j, k:k + 1], in_=tmp[:], axis=AX.X), [r_tmp], [r_destf])
                    S.op("dve", lambda prt=prt: V_.tensor_tensor(out=carry[:], in0=carry[:], in1=prt[:, 32:64], op=ALU.add), [r_pr, r_carry], [r_carry])
                S.op("dve", lambda: V_.tensor_copy(out=DESTI[:], in_=destf[:]), [r_destf], [r_DESTI])
                for j in range(NT):
                    rows = slice(j * 128, (j + 1) * 128)
                    xst, r_xs = xs.next()
                    dma("sp", xst[:], X1B[rows, :], writes=[r_xs])
                    for k in range(2):
                        S.dma("pool", lambda xst=xst, j=j, k=k: nc.gpsimd.indirect_dma_start(
                            out=XG, out_offset=bass.IndirectOffsetOnAxis(ap=DESTI[:, j, k:k + 1], axis=0), in_=xst[:], in_offset=None),
                            [r_xs, r_DESTI], [])
                for i in range(NBLK):
                    S.op("dve", lambda i=i: V_.tensor_scalar(out=tmp[:], in0=pend[:], scalar1=float(i * BLK), scalar2=None, op0=ALU.is_le), [r_pend], [r_tmp])
                    S.op("dve", lambda i=i: V_.reduce_sum(out=EB[:, i:i + 1], in_=tmp[:], axis=AX.X), [r_tmp], [r_EB])
                S.op("dve", lambda: V_.tensor_scalar(out=iof4[:], in0=iof[:], scalar1=4.0, scalar2=None, op0=ALU.mult), [r_iof], [r_iof4])
                S.op("dve", lambda: V_.tensor_scalar(out=bsg[:], in0=EB[:], scalar1=512.0, scalar2=iof4[:, 0:1], op0=ALU.mult, op1=ALU.add), [r_EB, r_iof4], [r_bsg])
                S.op("dve", lambda: V_.tensor_scalar(out=bsd[:], in0=EB[:], scalar1=512.0, scalar2=iof[:, 0:1], op0=ALU.mult, op1=ALU.add), [r_EB, r_iof], [r_bsd])
                for q4 in range(4):
                    S.op("dve", lambda q4=q4: V_.tensor_scalar(out=IDXG[:, :, q4], in0=bsg[:], scalar1=float(q4 + l * 32 * 512), scalar2=None, op0=ALU.add), [r_bsg], [r_IDXG])
                for fc in range(4):
                    S.op("dve", lambda fc=fc: V_.tensor_scalar(out=IDXD[:, :, fc], in0=bsd[:], scalar1=float(fc * 128 + l * 32 * 512), scalar2=None, op0=ALU.add), [r_bsd], [r_IDXD])
                S.barrier()
            if stop_phase <= 6:
                break

            with ExitStack() as es:
                def wring(name, shape, nres):
                    return Ring([(es.enter_context(nc.sbuf_tensor("%s%d_%d" % (name, k, l), shape, BF16)), [Res() for _ in range(nres)]) for k in range(2)])
                wgr = wring("wgt", [128, 16, 512], 16)
                wur = wring("wut", [128, 16, 512], 16)
                wdr = wring("wdt", [128, 4, DM], 4)
                xg = mk(es, "xg", [128, DM], BF16, 2)
                xgT = mk(es, "xgT", [128, 16, BLK], BF16, 2)
                sgm = mk(es, "sgm", [128, BLK], F32, 2)
                hT = mk(es, "hT", [128, 4, BLK], BF16, 2)
                ys = mk(es, "ys", [128, DM], F32, 2)
                tp = mkp(es, "tp7", [128, 512], BF16, 2)
                pg = mkp(es, "pg", [128, BLK], F32, 2)
                pu = mkp(es, "pu", [128, BLK], F32, 2)
                py = mkp(es, "py", [128, 512], F32, 2)
                bc_g = nc.gpsimd.to_reg((l + 1) * 32 * 512 - 1)
                bc_d = nc.gpsimd.to_reg((l + 1) * 32 * 512 - 1)
                for (ring_, nres_) in ((wgr, 16), (wur, 16), (wdr, 4)):
                    for (t_, rs_) in ring_.items:
                        S.op("pool", lambda t_=t_: nc.gpsimd.memset(t_[:], 0.0), [], rs_)
                def blk7(i):
                    wgt, r_wg = wgr.next()
                    wut, r_wu = wur.next()
                    wdt, r_wd = wdr.next()
                    for q4 in range(4):
                        S.dma("pool", lambda wgt=wgt, i=i, q4=q4: nc.gpsimd.indirect_dma_start(
                            out=wgt[:, 4 * q4:4 * q4 + 4, :].rearrange("p a b -> p (a b)"), out_offset=None, in_=w_eg, in_offset=bass.IndirectOffsetOnAxis(ap=IDXG[:, i, q4:q4 + 1], axis=0), bounds_check=bc_g, oob_is_err=False),
                            [r_IDXG], [r_wg[q4]])
                        S.dma("pool", lambda wut=wut, i=i, q4=q4: nc.gpsimd.indirect_dma_start(
                            out=wut[:, 4 * q4:4 * q4 + 4, :].rearrange("p a b -> p (a b)"), out_offset=None, in_=w_eu, in_offset=bass.IndirectOffsetOnAxis(ap=IDXG[:, i, q4:q4 + 1], axis=0), bounds_check=bc_g, oob_is_err=False),
                            [r_IDXG], [r_wu[q4]])
                    for fc in range(4):
                        S.dma("pool", lambda wdt=wdt, i=i, fc=fc: nc.gpsimd.indirect_dma_start(
                            out=wdt[:, fc, :], out_offset=None, in_=w_ed, in_offset=bass.IndirectOffsetOnAxis(ap=IDXD[:, i, fc:fc + 1], axis=0), bounds_check=bc_d, oob_is_err=False),
                            [r_IDXD], [r_wd[fc]])
                    xTt, r_xgT = xgT.next()
                    for sb in range(BLK // 128):
                        xgt, r_xg = xg.next()
                        r0 = i * BLK + sb * 128
                        dma("sp", xgt[:], XG[r0:r0 + 128, :], writes=[r_xg])
                        for g4 in range(4):
                            tpt, r_tp = tp.next()
                            for k in range(4):
                                kc = g4 * 4 + k
                                S.op("pe", lambda kc=kc, k=k, tpt=tpt, xgt=xgt: nc.tensor.transpose(out=tpt[:, k * 128:(k + 1) * 128], in_=xgt[:, kc:kc + 127 * 16 + 1:16], identity=ident_b[:]),
                                     [r_xg, r_identb], [r_tp], signal=(k == 3))
                            eng = alt("act", "dve")
                            S.op(eng, lambda eng=eng, tpt=tpt, g4=g4, sb=sb, xTt=xTt: ecopy(eng, xTt[:, g4 * 4:(g4 + 1) * 4, sb * 128:(sb + 1) * 128], tpt[:].rearrange("p (k t) -> p k t", k=4)),
                                 [r_tp], [r_xgT])
                    yield
                    hTt, r_hT = hT.next()
                    for fc in range(4):
                        pgt, r_pg = pg.next()
                        put, r_pu = pu.next()
                        for kc in range(16):
                            S.op("pe", lambda kc=kc, fc=fc, pgt=pgt, wgt=wgt, xTt=xTt: nc.tensor.matmul(pgt[:], lhsT=wgt[:, kc, fc * 128:(fc + 1) * 128], rhs=xTt[:, kc, :], start=(kc == 0), stop=(kc == 15)),
                                 [r_wg[kc // 4], r_xgT], [r_pg], signal=(kc == 15))
                        for kc in range(16):
                            S.op("pe", lambda kc=kc, fc=fc, put=put, wut=wut, xTt=xTt: nc.tensor.matmul(put[:], lhsT=wut[:, kc, fc * 128:(fc + 1) * 128], rhs=xTt[:, kc, :], start=(kc == 0), stop=(kc == 15)),
                                 [r_wu[kc // 4], r_xgT], [r_pu], signal=(kc == 15))
                        sgt, r_sg = sgm.next()
                        S.op("act", lambda sgt=sgt, pgt=pgt: nc.scalar.activation(out=sgt[:], in_=pgt[:], func=AF.Silu), [r_pg], [r_sg])
                        S.op("dve", lambda sgt=sgt, put=put, hTt=hTt, fc=fc: nc.vector.tensor_tensor(out=hTt[:, fc, :], in0=sgt[:], in1=put[:], op=ALU.mult), [r_sg, r_pu], [r_hT])
                    yield
                    for sb in range(BLK // 128):
                        yst, r_ys = ys.next()
                        for n4 in range(4):
                            pyt, r_py = py.next()
                            for fc in range(4):
                                S.op("pe", lambda fc=fc, pyt=pyt, hTt=hTt, wdt=wdt, sb=sb, n4=n4: nc.tensor.matmul(pyt[:], lhsT=hTt[:, fc, sb * 128:(sb + 1) * 128], rhs=wdt[:, fc, n4 * 512:(n4 + 1) * 512], start=(fc == 0), stop=(fc == 3)),
                                     [r_hT, r_wd[fc]], [r_py], signal=(fc == 3))
                            eng = alt("act", "dve")
                            S.op(eng, lambda eng=eng, yst=yst, pyt=pyt, n4=n4: ecopy(eng, yst[:, n4 * 512:(n4 + 1) * 512], pyt[:]), [r_py], [r_ys])
                        r0 = i * BLK + sb * 128
                        dma("sp", YB[r0:r0 + 128, :], yst[:], reads=[r_ys])
                run_skewed(blk7, NBLK)
                S.barrier()
                nc.gpsimd.free_register(bc_g)
                nc.gpsimd.free_register(bc_d)
            if stop_phase <= 7:
                break

            with ExitStack() as es:
                g2, r_g2 = mk(es, "g2", [128, DM], F32)
                b2, r_b2 = mk(es, "b2", [128, DM], F32)
                y1 = mk(es, "y1", [128, DM], F32, 2)
                y2 = mk(es, "y2", [128, DM], F32, 2)
                x1r = mk(es, "x1r", [128, DM], F32, 2)
                u_ring = mk(es, "u8", [128, DM], F32, 2)
                xo = mk(es, "xo", [128, DM], F32, 2)
                st_ring = mk(es, "st8", [128, 4, 6], F32, 2)
                mv_ring = mk(es, "mv8", [128, 3], F32, 2)
                dma("sp", g2[:], ln2_g[l:l + 1, :].partition_broadcast(128), writes=[r_g2])
                dma("sp", b2[:], ln2_b[l:l + 1, :].partition_broadcast(128), writes=[r_b2])
                def tile8(j):
                    rows = slice(j * 128, (j + 1) * 128)
                    u, r_u = u_ring.next()
                    st, r_st = st_ring.next()
                    mv, r_mv = mv_ring.next()
                    y1t, r_y1 = y1.next()
                    y2t, r_y2 = y2.next()
                    S.dma("pool", lambda y1t=y1t, j=j: nc.gpsimd.indirect_dma_start(out=y1t[:], out_offset=None, in_=YB, in_offset=bass.IndirectOffsetOnAxis(ap=DESTI[:, j, 0:1], axis=0)),
                          [r_DESTI], [r_y1])
                    S.dma("pool", lambda y2t=y2t, j=j: nc.gpsimd.indirect_dma_start(out=y2t[:], out_offset=None, in_=YB, in_offset=bass.IndirectOffsetOnAxis(ap=DESTI[:, j, 1:2], axis=0)),
                          [r_DESTI], [r_y2])
                    xt_, r_x1r = x1r.next()
                    dma("sp", xt_[:], X1[rows, :], writes=[r_x1r])
                    S.op("dve", lambda y1t=y1t, j=j: nc.vector.tensor_scalar(out=u[:], in0=y1t[:], scalar1=GATE[:, j, 0:1], scalar2=None, op0=ALU.mult), [r_y1, r_GATE], [r_u])
                    S.op("dve", lambda y2t=y2t, j=j: nc.vector.scalar_tensor_tensor(out=u[:], in0=y2t[:], scalar=GATE[:, j, 1:2], in1=u[:], op0=ALU.mult, op1=ALU.add), [r_y2, r_GATE, r_u], [r_u])
                    S.op("dve", lambda xt_=xt_: nc.vector.scalar_tensor_tensor(out=u[:], in0=xt_[:], scalar=ALPHA, in1=u[:], op0=ALU.mult, op1=ALU.add), [r_x1r, r_u], [r_u])
                    yield
                    xot, r_xo = xo.next()
                    layer_norm_tile(None, u, r_u, g2, r_g2, b2, r_b2, st, r_st, mv, r_mv, xot, r_xo)
                    dma("sp", XNEXT[rows, :], xot[:], reads=[r_xo])
                run_skewed(tile8, NT)
                S.barrier()
            XCUR = XNEXT
    return nc, S


def make_inputs(inputs, core, stop_phase=99):
    hc = host_consts()
    m = {}
    m["x"] = np.ascontiguousarray(inputs["x"][core])
    for k in ("w_in", "w_gla_gate", "b_gla_gate", "ret_norm_g", "gla_norm_g", "w_out", "ln1_g", "ln1_b", "ln2_g", "ln2_b"):
        m[k] = np.ascontiguousarray(inputs[k])
    m["w_router"] = np.ascontiguousarray(np.concatenate([inputs["w_router_group"], inputs["w_router_expert"]], axis=-1))
    m["b_router"] = np.ascontiguousarray(np.concatenate([inputs["b_router_group"], inputs["b_router_expert"]], axis=-1))
    if stop_phase >= 7:
        m["w_expert_gate"] = np.ascontiguousarray(inputs["w_expert_gate"]).reshape(DEPTH * 32 * 128 * 4, 2048)
        m["w_expert_up"] = np.ascontiguousarray(inputs["w_expert_up"]).reshape(DEPTH * 32 * 128 * 4, 2048)
        m["w_expert_down"] = np.ascontiguousarray(inputs["w_expert_down"]).reshape(DEPTH * 32 * 512, DM)
    m["c_ident"] = hc["ident"]
    m["c_mask01"] = hc["mask01"]
    m["c_stri"] = hc["stri"]
    m["c_dmask0"] = hc["dmask0"]
    m["c_dmask1"] = hc["dmask1"]
    m["c_rope"] = hc["rope"]
    return m


def kernel(**inputs):
    inputs = {k: np.asarray(v) for k, v in inputs.items()}
    nc, _ = build()
    in_maps = [make_inputs(inputs, c) for c in range(8)]
    res = run_bass_kernel_spmd(nc, in_maps, core_ids=list(range(8)))
    return np.stack([r["y"] for r in res.results], axis=0).astype(np.float32)
```

```python
import numpy as np
from contextlib import ExitStack
import concourse.bass as bass
import concourse.mybir as mybir
from concourse.bass_utils import run_bass_kernel_spmd

F32 = mybir.dt.float32
BF16 = mybir.dt.bfloat16
I32 = mybir.dt.int32
AF = mybir.ActivationFunctionType
ALU = mybir.AluOpType
AX = mybir.AxisListType

ND = 12
SEQ = 4096
DM = 2048
NT = SEQ // 128
DEPTH = 4
INW = 6672
ALPHA = float((2 * DEPTH) ** 0.25)
EPS = 1e-5
BLK = 384
NBLK = -(-(2 * SEQ + 32 * (BLK - 1)) // BLK)
NROWS = NBLK * BLK
BIG = 30000.0


class Res:
    __slots__ = ("name", "w", "r")

    def __init__(self, name=""):
        self.name = name
        self.w = {}
        self.r = {}


class Sched:
    def __init__(self, nc, es, same_sync=True):
        self.nc = nc
        self.same_sync = same_sync
        self.e = dict(pe=nc.tensor, act=nc.scalar, dve=nc.vector, pool=nc.gpsimd, sp=nc.sync)
        self.sem = {k: es.enter_context(nc.semaphore("s_" + k)) for k in ("pe", "act", "dve", "pool")}
        self.cnt = {k: 0 for k in self.sem}
        self.pending = {k: False for k in self.sem}
        self.dsem = {q: [es.enter_context(nc.semaphore("d_%s%d" % (q, i))) for i in range(ND)]
                     for q in ("sp", "pool", "act")}
        self.dcnt = {q: [0] * ND for q in self.dsem}
        self.dnext = {q: 0 for q in self.dsem}
        self.known = {k: {} for k in self.e}
        self.n_ins = 0
        self.n_wait = 0

    def _semof(self, key):
        if key[0] == "c":
            return self.sem[key[1]], 1
        return self.dsem[key[1]][key[2]], 16

    def _wait(self, eng, key, val):
        if self.known[eng].get(key, 0) >= val:
            return
        sem, mult = self._semof(key)
        self.e[eng].wait_ge(sem, val * mult)
        self.known[eng][key] = val
        self.n_wait += 1

    def _deps(self, eng, reads, writes):
        deps = {}
        own = ("c", eng)
        for r in reads:
            for k, v in r.w.items():
                if deps.get(k, 0) < v:
                    deps[k] = v
        for w in writes:
            for d in (w.w, w.r):
                for k, v in d.items():
                    if k == own:
                        continue
                    if deps.get(k, 0) < v:
                        deps[k] = v
        for k, v in deps.items():
            if k == own and (eng == "pe" or not self.same_sync):
                continue
            self._wait(eng, k, v)

    def _mark(self, key, val, reads, writes):
        for r in reads:
            if r.r.get(key, 0) < val:
                r.r[key] = val
        for w in writes:
            w.w = {key: val}
            w.r = {}

    def op(self, eng, fn, reads=(), writes=(), signal=True):
        self._deps(eng, reads, writes)
        ins = fn()
        self.n_ins += 1
        if signal:
            self.cnt[eng] += 1
            ins.then_inc(self.sem[eng], 1)
            self.pending[eng] = False
            val = self.cnt[eng]
        else:
            self.pending[eng] = True
            val = self.cnt[eng] + 1
        self._mark(("c", eng), val, reads, writes)
        return ins

    def dma(self, q, fn, reads=(), writes=()):
        slot = self.dnext[q]
        self.dnext[q] = (slot + 1) % ND
        key = ("d", q, slot)
        if self.dcnt[q][slot] > 0:
            self._wait(q, key, self.dcnt[q][slot])
        self._deps(q, reads, writes)
        ins = fn()
        self.n_ins += 1
        self.dcnt[q][slot] += 1
        ins.then_inc(self.dsem[q][slot], 16)
        self._mark(key, self.dcnt[q][slot], reads, writes)
        return ins

    def barrier(self):
        for k in self.pending:
            assert not self.pending[k], "pending unsignaled instruction on " + k
        for eng in self.e.keys():
            for k in self.cnt:
                if self.cnt[k] > 0:
                    self._wait(eng, ("c", k), self.cnt[k])
            for q in self.dcnt:
                for i in range(ND):
                    if self.dcnt[q][i] > 0:
                        self._wait(eng, ("d", q, i), self.dcnt[q][i])


class Ring:
    def __init__(self, items):
        self.items = items
        self.i = 0

    def next(self):
        it = self.items[self.i]
        self.i = (self.i + 1) % len(self.items)
        return it


def host_consts():
    c = {}
    i = np.arange(128)
    c["ident"] = np.eye(128, dtype=np.float32)
    c["mask01"] = (i[:, None] <= i[None, :]).astype(np.float32)
    c["stri"] = (i[:, None] < i[None, :]).astype(np.float32)
    j = np.arange(256)
    band = (j[None, :] >= i[:, None]) & (j[None, :] <= i[:, None] + 128)
    c["dmask1"] = np.where(band, 0.0, -BIG).astype(np.float32)
    c["dmask0"] = np.where(band & (j[None, :] >= 128), 0.0, -BIG).astype(np.float32)
    half = 64
    inv = (np.float32(10000.0) ** (-np.arange(half, dtype=np.float32) / np.float32(half))).astype(np.float32)
    pos = np.arange(SEQ, dtype=np.float32)
    ang = (pos[:, None] * inv[None, :]).astype(np.float32)
    cos = np.cos(ang).astype(np.float32)
    sin = np.sin(ang).astype(np.float32)
    h = np.arange(4, dtype=np.float32)
    log_g = np.log1p(-np.exp2(-5.0 - h)).astype(np.float64)
    cc = (np.arange(SEQ) % 128).astype(np.float64)
    qd = np.exp((cc[:, None] + 1.0) * log_g[None, :])
    kd = np.exp(-(cc[:, None] + 1.0) * log_g[None, :]) * (128.0 ** -0.5)
    rope = np.zeros((4, SEQ, 4, 64), np.float32)
    rope[0] = cos[:, None, :] * qd[:, :, None]
    rope[1] = sin[:, None, :] * qd[:, :, None]
    rope[2] = cos[:, None, :] * kd[:, :, None]
    rope[3] = sin[:, None, :] * kd[:, :, None]
    c["rope"] = rope.reshape(4, SEQ, 256)
    c["g128"] = [float(np.exp(128.0 * lg)) for lg in log_g]
    return c


def build(n_layers=DEPTH, debug=(), stop_phase=99):
    nc = bass.Bass("TRN2", target_bir_lowering=False)
    hc = host_consts()
    g128 = hc["g128"]

    def din(name, shape, dt=F32):
        return nc.dram_tensor(name, list(shape), dt, kind="ExternalInput").ap()

    def dscr(name, shape, dt):
        return nc.dram_tensor(name, list(shape), dt, kind=("ExternalOutput" if name in debug else "Internal")).ap()

    x_in = din("x", [SEQ, DM])
    w_in = din("w_in", [DEPTH, DM, INW])
    w_gg = din("w_gla_gate", [DEPTH, 16, 384])
    b_gg = din("b_gla_gate", [DEPTH, 384])
    ret_g = din("ret_norm_g", [DEPTH, 512])
    gla_g = din("gla_norm_g", [DEPTH, 768])
    w_out = din("w_out", [DEPTH, DM, DM])
    ln1_g = din("ln1_g", [DEPTH, DM])
    ln1_b = din("ln1_b", [DEPTH, DM])
    w_rt = din("w_router", [DEPTH, DM, 36])
    b_rt = din("b_router", [DEPTH, 36])
    if stop_phase >= 7:
        w_eg = din("w_expert_gate", [DEPTH * 32 * 128 * 4, 2048])
        w_eu = din("w_expert_up", [DEPTH * 32 * 128 * 4, 2048])
        w_ed = din("w_expert_down", [DEPTH * 32 * 512, DM])
    ln2_g = din("ln2_g", [DEPTH, DM])
    ln2_b = din("ln2_b", [DEPTH, DM])
    c_ident = din("c_ident", [128, 128])
    c_mask01 = din("c_mask01", [128, 128])
    c_stri = din("c_stri", [128, 128])
    c_dmask0 = din("c_dmask0", [128, 256])
    c_dmask1 = din("c_dmask1", [128, 256])
    c_rope = din("c_rope", [4, SEQ, 256])
    y_out = nc.dram_tensor("y", [SEQ, DM], F32, kind="ExternalOutput").ap()

    P = dscr("P", [SEQ, 5120], BF16)
    PT = dscr("PT", [1536, SEQ], BF16)
    GAT = dscr("GAT", [16, SEQ], F32)
    DO = dscr("DO", [3, SEQ, 6 * 130], F32)
    MIX = dscr("MIX", [SEQ, DM], BF16)
    X1 = dscr("X1", [SEQ, DM], F32)
    X1B = dscr("X1B", [SEQ, DM], BF16)
    XG = dscr("XG", [NROWS, DM], BF16)
    YB = dscr("YB", [NROWS, DM], F32)
    XA = dscr("XA", [SEQ, DM], F32)
    XB_ = dscr("XBb", [SEQ, DM], F32)

    with ExitStack() as es0:
        S = Sched(nc, es0)
        E = S.e
        rr = {"i": 0}

        def alt(*engs):
            rr["i"] += 1
            return engs[rr["i"] % len(engs)]

        def ecopy(eng, out, in_):
            if eng == "act":
                return nc.scalar.copy(out=out, in_=in_)
            return E[eng].tensor_copy(out=out, in_=in_)

        def dma(q, out, in_, reads=(), writes=()):
            return S.dma(q, lambda: E[q].dma_start(out=out, in_=in_), reads, writes)

        uid = [0]

        def mk(es, name, shape, dt, n=1):
            items = []
            for k in range(n):
                uid[0] += 1
                t = es.enter_context(nc.sbuf_tensor("%s%d_%d" % (name, k, uid[0]), list(shape), dt))
                items.append((t, Res(name)))
            return items[0] if n == 1 else Ring(items)

        def mkp(es, name, shape, dt, n=1):
            items = []
            for k in range(n):
                uid[0] += 1
                t = es.enter_context(nc.psum_tensor("%s%d_%d" % (name, k, uid[0]), list(shape), dt))
                items.append((t, Res(name)))
            return items[0] if n == 1 else Ring(items)

        ident_f, r_identf = mk(es0, "identf", [128, 128], F32)
        ident_b, r_identb = mk(es0, "identb", [128, 128], BF16)
        mask01, r_mask01 = mk(es0, "mask01", [128, 128], F32)
        ones_b, r_onesb = mk(es0, "onesb", [128, 128], BF16)
        ones_f, r_onesf = mk(es0, "onesf", [128, 128], F32)
        dma("sp", ident_f[:], c_ident, writes=[r_identf])
        dma("sp", mask01[:], c_mask01, writes=[r_mask01])
        S.op("dve", lambda: nc.vector.tensor_copy(out=ident_b[:], in_=ident_f[:]), [r_identf], [r_identb])
        S.op("dve", lambda: nc.vector.memset(ones_b[:], 1.0), [], [r_onesb])
        S.op("dve", lambda: nc.vector.memset(ones_f[:], 1.0), [], [r_onesf])

        OH, r_OH = mk(es0, "OH", [128, NT, 2, 32], F32)
        A_b, r_Ab = mk(es0, "A_b", [128, NT, 32], BF16)
        GATE, r_GATE = mk(es0, "GATE", [128, NT, 2], F32)
        DESTI, r_DESTI = mk(es0, "DESTI", [128, NT, 2], I32)
        IDXG, r_IDXG = mk(es0, "IDXG", [128, NBLK, 4], I32)
        IDXD, r_IDXD = mk(es0, "IDXD", [128, NBLK, 4], I32)
        if stop_phase >= 6:
            with ExitStack() as es:
                zt, r_zt = mk(es, "zt", [128, DM], BF16)
                S.op("dve", lambda: nc.vector.memset(zt[:], 0.0), [], [r_zt])
                for i in range(NROWS // 128):
                    dma("sp", XG[i * 128:(i + 1) * 128, :], zt[:], reads=[r_zt])
                S.barrier()

        def run_skewed(tile_fn, n):
            gens = []
            t = 0
            while t < n or gens:
                if t < n:
                    gens.append(tile_fn(t))
                for g in list(gens):
                    try:
                        next(g)
                    except StopIteration:
                        gens.remove(g)
                t += 1

        def rstd(out_ap, var_ap, r_mv):
            S.op("dve", lambda: nc.vector.tensor_scalar(out=out_ap, in0=var_ap, scalar1=EPS, scalar2=None, op0=ALU.add), [r_mv], [r_mv])
            S.op("act", lambda: nc.scalar.sqrt(out=out_ap, in_=out_ap), [r_mv], [r_mv])
            S.op("dve", lambda: nc.vector.reciprocal(out=out_ap, in_=out_ap), [r_mv], [r_mv])

        def layer_norm_tile(es_unused, u, r_u, gt, r_gt, bt, r_bt, st, r_st, mv, r_mv, outt, r_out):
            for c4 in range(4):
                S.op("dve", lambda c4=c4: nc.vector.bn_stats(out=st[:, c4, :], in_=u[:, c4 * 512:(c4 + 1) * 512]),
                     [r_u], [r_st])
            S.op("dve", lambda: nc.vector.bn_aggr(out=mv[:, 0:2], in_=st[:].rearrange("p a b -> p (a b)")), [r_st], [r_mv])
            rstd(mv[:, 2:3], mv[:, 1:2], r_mv)
            S.op("dve", lambda: nc.vector.tensor_scalar(out=outt[:], in0=u[:], scalar1=mv[:, 0:1], scalar2=mv[:, 2:3],
                                                        op0=ALU.subtract, op1=ALU.mult), [r_u, r_mv], [r_out])
            S.op("pool", lambda: nc.gpsimd.tensor_tensor(out=outt[:], in0=outt[:], in1=gt[:], op=ALU.mult),
                 [r_out, r_gt], [r_out])
            S.op("pool", lambda: nc.gpsimd.tensor_tensor(out=outt[:], in0=outt[:], in1=bt[:], op=ALU.add),
                 [r_out, r_bt], [r_out])

        def head_norm(o_ap, nh, st, r_st, mv, r_mv, outt, r_out, r_o, col0):
            for h in range(nh):
                S.op("dve", lambda h=h: nc.vector.bn_stats(out=st[:, h, :], in_=o_ap(h)), [r_o], [r_st])
                S.op("dve", lambda h=h: nc.vector.bn_aggr(out=mv[:, h, 0:2], in_=st[:, h, :]), [r_st], [r_mv])
            rstd(mv[:, 0:nh, 2:3], mv[:, 0:nh, 1:2], r_mv)
            for h in range(nh):
                S.op("dve", lambda h=h: nc.vector.tensor_scalar(
                    out=outt[:, col0 + h * 128: col0 + (h + 1) * 128], in0=o_ap(h), scalar1=mv[:, h, 0:1],
                    scalar2=mv[:, h, 2:3], op0=ALU.subtract, op1=ALU.mult), [r_o, r_mv], [r_out])

        XCUR = x_in
        for l in range(n_layers):
            XNEXT = y_out if l == n_layers - 1 else (XA if l % 2 == 0 else XB_)
            with ExitStack() as es:
                xT, r_xT = mk(es, "xT", [128, 16, SEQ], BF16)
                xb = mk(es, "xb", [128, DM], BF16, 2)
                wt = mk(es, "wt", [128, 16, 512], BF16, 2)
                stg = mk(es, "stg", [128, 4, 512], BF16, 2)
                stgf, r_stgf = mk(es, "stgf", [16, 512], F32)
                tp = mkp(es, "tp", [128, 512], BF16, 2)
                ps = mkp(es, "ps", [128, 512], F32, 4)
                for j in range(NT):
                    xbt, r_xb = xb.next()
                    dma("pool", xbt[:], XCUR[j * 128:(j + 1) * 128, :], writes=[r_xb])
                    for g4 in range(4):
                        tpt, r_tp = tp.next()
                        for k in range(4):
                            kc = g4 * 4 + k
                            S.op("pe", lambda kc=kc, k=k, tpt=tpt, xbt=xbt: nc.tensor.transpose(
                                out=tpt[:, k * 128:(k + 1) * 128], in_=xbt[:, kc * 128:(kc + 1) * 128], identity=ident_b[:]),
                                [r_xb, r_identb], [r_tp], signal=(k == 3))
                        eng = alt("act", "dve")
                        S.op(eng, lambda eng=eng, tpt=tpt, g4=g4, j=j: ecopy(
                            eng, xT[:, g4 * 4:(g4 + 1) * 4, j * 128:(j + 1) * 128],
                            tpt[:].rearrange("p (k t) -> p k t", k=4)), [r_tp], [r_xT])
                if stop_phase >= 1:
                    tm_tiles = [(c0, c0) for c0 in range(0, 2048, 512)] + [(c0, c0 - 1536) for c0 in range(3584, 6656, 512)]
                    for (wc, pc) in tm_tiles:
                        wtt, r_wt = wt.next()
                        dma("pool", wtt[:], w_in[l, :, wc:wc + 512].rearrange("(k p) n -> p k n", p=128), writes=[r_wt])
                        for j4 in range(NT // 4):
                            sg, r_sg = stg.next()
                            for jj in range(4):
                                j = j4 * 4 + jj
                                pst, r_ps = ps.next()
                                for kc in range(16):
                                    S.op("pe", lambda kc=kc, pst=pst, j=j, wtt=wtt: nc.tensor.matmul(
                                        pst[:], lhsT=xT[:, kc, j * 128:(j + 1) * 128], rhs=wtt[:, kc, :],
                                        start=(kc == 0), stop=(kc == 15)), [r_xT, r_wt], [r_ps], signal=(kc == 15))
                                eng = alt("act", "dve")
                                S.op(eng, lambda eng=eng, sg=sg, jj=jj, pst=pst: ecopy(eng, sg[:, jj, :], pst[:]), [r_ps], [r_sg])
                            dma("sp", P[j4 * 512:(j4 + 1) * 512, pc:pc + 512].rearrange("(j p) n -> p j n", p=128), sg[:],
                                reads=[r_sg])
                    for g in range(3):
                        wtt, r_wt = wt.next()
                        wc = 2048 + g * 512
                        dma("pool", wtt[:], w_in[l, :, wc:wc + 512].rearrange("(k p) n -> p k n", p=128), writes=[r_wt])
                        for cc in range(4):
                            row0 = g * 512 + cc * 128
                            for s4 in range(2):
                                sg, r_sg = stg.next()
                                for ss in range(4):
                                    s = s4 * 4 + ss
                                    pst, r_ps = ps.next()
                                    for kc in range(16):
                                        S.op("pe", lambda kc=kc, pst=pst, s=s, wtt=wtt, cc=cc: nc.tensor.matmul(
                                            pst[:], lhsT=wtt[:, kc, cc * 128:(cc + 1) * 128], rhs=xT[:, kc, s * 512:(s + 1) * 512],
                                            start=(kc == 0), stop=(kc == 15)), [r_xT, r_wt], [r_ps], signal=(kc == 15))
                                    eng = alt("act", "dve")
                                    if row0 < 768:
                                        if eng == "act":
                                            S.op(eng, lambda sg=sg, ss=ss, pst=pst: nc.scalar.activation(out=sg[:, ss, :], in_=pst[:], func=AF.Copy, scale=float(128 ** -0.5)), [r_ps], [r_sg])
                                        else:
                                            S.op(eng, lambda sg=sg, ss=ss, pst=pst: nc.vector.tensor_scalar(out=sg[:, ss, :], in0=pst[:], scalar1=float(128 ** -0.5), scalar2=None, op0=ALU.mult), [r_ps], [r_sg])
                                    else:
                                        S.op(eng, lambda eng=eng, sg=sg, ss=ss, pst=pst: ecopy(eng, sg[:, ss, :], pst[:]), [r_ps], [r_sg])
                                dma("sp", PT[row0:row0 + 128, s4 * 2048:(s4 + 1) * 2048], sg[:].rearrange("p a b -> p (a b)"),
                                    reads=[r_sg])
                    wtt, r_wt = wt.next()
                    dma("pool", wtt[:, :, 0:16], w_in[l, :, 6656:6672].rearrange("(k p) n -> p k n", p=128), writes=[r_wt])
                    for s in range(8):
                        pst, r_ps = ps.next()
                        for kc in range(16):
                            S.op("pe", lambda kc=kc, pst=pst, s=s, wtt=wtt: nc.tensor.matmul(
                                pst[0:16, :], lhsT=wtt[:, kc, 0:16], rhs=xT[:, kc, s * 512:(s + 1) * 512],
                                start=(kc == 0), stop=(kc == 15)), [r_xT, r_wt], [r_ps], signal=(kc == 15))
                        S.op("act", lambda pst=pst: nc.scalar.copy(out=stgf[:], in_=pst[0:16, :]), [r_ps], [r_stgf])
                        dma("sp", GAT[:, s * 512:(s + 1) * 512], stgf[:], reads=[r_stgf])
                S.barrier()
            if stop_phase <= 1:
                break

            with ExitStack() as es:
                rope = mk(es, "rope", [128, 4, 256], F32, 2)
                qk = mk(es, "qk", [128, 1024], BF16, 2)
                rv = mk(es, "rv", [128, 512], BF16, 2)
                rg = mk(es, "rg", [128, 512], BF16, 2)
                qkr_ring = mk(es, "qkr", [128, 1024], BF16, 2)
                t1_ring = mk(es, "t1", [128, 4, 64], F32, 2)
                t2_ring = mk(es, "t2", [128, 4, 64], F32, 2)
                t3_ring = mk(es, "t3", [128, 4, 64], F32, 2)
                t4_ring = mk(es, "t4", [128, 4, 64], F32, 2)
                qkT_ring = mk(es, "qkT", [128, 8, 128], BF16, 2)
                sm = mk(es, "sm", [128, 128], BF16, 2)
                Sf = [mk(es, "Sf%d" % h, [128, 128], F32) for h in range(4)]
                Sb = [mk(es, "Sb%d" % h, [128, 128], BF16) for h in range(4)]
                Tt, r_Tt = mk(es, "Tt", [128, 128], F32)
                st_ring = mk(es, "st", [128, 4, 6], F32, 2)
                mv_ring = mk(es, "mv", [128, 4, 3], F32, 2)
                nrm_ring = mk(es, "nrm", [128, 512], F32, 2)
                sil_ring = mk(es, "sil", [128, 512], F32, 2)
                mixo = mk(es, "mixo", [128, 512], BF16, 2)
                gvec, r_gvec = mk(es, "gvec", [128, 512], F32)
                tp_ring = mkp(es, "tp2", [128, 8, 128], BF16, 2)
                sT = mkp(es, "sT", [128, 128], F32, 2)
                po_ring = mkp(es, "po", [128, 512], F32, 2)
                kv = mkp(es, "kv", [128, 128], F32, 2)
                dma("sp", gvec[:], ret_g[l:l + 1, :].partition_broadcast(128), writes=[r_gvec])
                for h in range(4):
                    S.op("dve", lambda h=h: nc.vector.memset(Sf[h][0][:], 0.0), [], [Sf[h][1]])
                    S.op("dve", lambda h=h: nc.vector.memset(Sb[h][0][:], 0.0), [], [Sb[h][1]])
                def tile2(j):
                    rows = slice(j * 128, (j + 1) * 128)
                    qkr, r_qkr = qkr_ring.next()
                    t1, r_t1 = t1_ring.next()
                    t2, r_t2 = t2_ring.next()
                    t3, r_t3 = t3_ring.next()
                    t4, r_t4 = t4_ring.next()
                    qkT, r_qkT = qkT_ring.next()
                    st, r_st = st_ring.next()
                    mv, r_mv = mv_ring.next()
                    nrm, r_nrm = nrm_ring.next()
                    sil, r_sil = sil_ring.next()
                    tp, r_tp = tp_ring.next()
                    po, r_po = po_ring.next()
                    ropt, r_rop = rope.next()
                    dma("sp", ropt[:], c_rope[:, rows, :].rearrange("a p n -> p a n"), writes=[r_rop])
                    qkt, r_qk = qk.next()
                    dma("sp", qkt[:], P[rows, 0:1024], writes=[r_qk])
                    rvt, r_rv = rv.next()
                    dma("sp", rvt[:], P[rows, 1024:1536], writes=[r_rv])
                    rgt, r_rg = rg.next()
                    dma("sp", rgt[:], P[rows, 1536:2048], writes=[r_rg])
                    for qi in range(2):
                        src = qkt[:, qi * 512:(qi + 1) * 512].rearrange("p (h d) -> p h d", h=4)
                        dst = qkr[:, qi * 512:(qi + 1) * 512].rearrange("p (h d) -> p h d", h=4)
                        a1, a2 = src[:, :, 0:64], src[:, :, 64:128]
                        cosv = ropt[:, 2 * qi, :].rearrange("p (h d) -> p h d", h=4)
                        sinv = ropt[:, 2 * qi + 1, :].rearrange("p (h d) -> p h d", h=4)
                        e1 = "dve" if qi == 0 else "pool"
                        S.op(e1, lambda e1=e1, a1=a1, cosv=cosv: E[e1].tensor_tensor(out=t1[:], in0=a1, in1=cosv, op=ALU.mult), [r_qk, r_rop], [r_t1])
                        S.op(e1, lambda e1=e1, a2=a2, sinv=sinv: E[e1].tensor_tensor(out=t2[:], in0=a2, in1=sinv, op=ALU.mult), [r_qk, r_rop], [r_t2])
                        S.op(e1, lambda e1=e1, dst=dst: E[e1].tensor_tensor(out=dst[:, :, 0:64], in0=t1[:], in1=t2[:], op=ALU.subtract), [r_t1, r_t2], [r_qkr])
                        S.op(e1, lambda e1=e1, a1=a1, sinv=sinv: E[e1].tensor_tensor(out=t3[:], in0=a1, in1=sinv, op=ALU.mult), [r_qk, r_rop], [r_t3])
                        S.op(e1, lambda e1=e1, a2=a2, cosv=cosv: E[e1].tensor_tensor(out=t4[:], in0=a2, in1=cosv, op=ALU.mult), [r_qk, r_rop], [r_t4])
                        S.op(e1, lambda e1=e1, dst=dst: E[e1].tensor_tensor(out=dst[:, :, 64:128], in0=t3[:], in1=t4[:], op=ALU.add), [r_t3, r_t4], [r_qkr])
                    for k in range(8):
                        S.op("pe", lambda k=k: nc.tensor.transpose(out=tp[:, k, :], in_=qkr[:, k * 128:(k + 1) * 128], identity=ident_b[:]),
                             [r_qkr, r_identb], [r_tp], signal=(k == 7))
                    S.op("act", lambda: nc.scalar.copy(out=qkT[:], in_=tp[:]), [r_tp], [r_qkT])
                    S.op("act", lambda rgt=rgt: nc.scalar.activation(out=sil[:], in_=rgt[:], func=AF.Silu), [r_rg], [r_sil])
                    yield
                    for h in range(4):
                        sTt, r_sT = sT.next()
                        S.op("pe", lambda h=h, sTt=sTt: nc.tensor.matmul(sTt[:], lhsT=qkT[:, 4 + h, :], rhs=qkT[:, h, :], start=True, stop=True),
                             [r_qkT], [r_sT])
                        smt, r_sm = sm.next()
                        S.op("dve", lambda sTt=sTt, smt=smt: nc.vector.tensor_tensor(out=smt[:], in0=sTt[:], in1=mask01[:], op=ALU.mult),
                             [r_sT, r_mask01], [r_sm])
                        S.op("pe", lambda h=h, smt=smt, rvt=rvt: nc.tensor.matmul(po[:, h * 128:(h + 1) * 128], lhsT=smt[:], rhs=rvt[:, h * 128:(h + 1) * 128],
                                                                         start=True, stop=False), [r_sm, r_rv], [r_po], signal=False)
                        S.op("pe", lambda h=h: nc.tensor.matmul(po[:, h * 128:(h + 1) * 128], lhsT=qkT[:, h, :], rhs=Sb[h][0][:],
                                                                start=False, stop=True), [r_qkT, Sb[h][1]], [r_po])
                        kvt, r_kv = kv.next()
                        S.op("pe", lambda h=h, kvt=kvt, rvt=rvt: nc.tensor.matmul(kvt[:], lhsT=qkr[:, 512 + h * 128:512 + (h + 1) * 128], rhs=rvt[:, h * 128:(h + 1) * 128],
                                                                         start=True, stop=True), [r_qkr, r_rv], [r_kv])
                        S.op("dve", lambda h=h, kvt=kvt: nc.vector.tensor_tensor(out=Tt[:], in0=Sf[h][0][:], in1=kvt[:], op=ALU.add),
                             [Sf[h][1], r_kv], [r_Tt])
                        S.op("act", lambda h=h: nc.scalar.activation(out=Sf[h][0][:], in_=Tt[:], func=AF.Copy, scale=g128[h]), [r_Tt], [Sf[h][1]])
                        S.op("act", lambda h=h: nc.scalar.activation(out=Sb[h][0][:], in_=Tt[:], func=AF.Copy, scale=g128[h]), [r_Tt], [Sb[h][1]])
                    yield
                    head_norm(lambda h: po[:, h * 128:(h + 1) * 128], 4, st, r_st, mv, r_mv, nrm, r_nrm, r_po, 0)
                    S.op("pool", lambda: nc.gpsimd.tensor_tensor(out=nrm[:], in0=nrm[:], in1=gvec[:], op=ALU.mult), [r_nrm, r_gvec], [r_nrm])
                    mo, r_mo = mixo.next()
                    S.op("pool", lambda mo=mo: nc.gpsimd.tensor_tensor(out=mo[:], in0=nrm[:], in1=sil[:], op=ALU.mult), [r_nrm, r_sil], [r_mo])
                    dma("sp", MIX[rows, 0:512], mo[:], reads=[r_mo])
                run_skewed(tile2, NT)
                S.barrier()
            if stop_phase <= 2:
                break

            with ExitStack() as es:
                wg, r_wg = mk(es, "wgg", [16, 384], F32)
                bg, r_bg = mk(es, "bgg", [1, 384], F32)
                gvec, r_gvec = mk(es, "gvec3", [128, 768], F32)
                gat = mk(es, "gat", [16, 128], F32, 2)
                gqk = mk(es, "gqk", [128, 768], BF16, 2)
                gv = mk(es, "gv", [128, 768], BF16, 2)
                gr = mk(es, "gr", [128, 768], BF16, 2)
                ez_ring = mk(es, "ez", [128, 384], F32, 2)
                lz_ring = mk(es, "lz", [128, 384], F32, 2)
                eb_ring = mk(es, "eb", [128, 384], F32, 2)
                enb_ring = mk(es, "enb", [128, 384], F32, 2)
                qkh_ring = mk(es, "qkh", [128, 768], BF16, 2)
                qkT_ring = mk(es, "qkT3", [128, 6, 128], BF16, 2)
                dec_ring = mk(es, "dec", [128, 4], F32, 2)
                sm = mk(es, "sm3", [128, 128], BF16, 2)
                Sf = [mk(es, "Sg%d" % p_, [128, 256], F32) for p_ in range(3)]
                Sb = [mk(es, "Sgb%d" % p_, [128, 256], BF16) for p_ in range(3)]
                Tt, r_Tt = mk(es, "Tt3", [128, 256], F32)
                st_ring = mk(es, "st3", [128, 6, 6], F32, 2)
                mv_ring = mk(es, "mv3", [128, 6, 3], F32, 2)
                nrm_ring = mk(es, "nrm3", [128, 768], F32, 2)
                sil_ring = mk(es, "sil3", [128, 768], F32, 2)
                mixo = mk(es, "mixo3", [128, 768], BF16, 2)
                pz, r_pz = mkp(es, "pz", [128, 512], F32)
                pl, r_pl = mkp(es, "pl", [128, 512], F32)
                tp, r_tp = mkp(es, "tp3", [128, 8, 128], BF16)
                pm, r_pm = mkp(es, "pm3", [128, 512], F32)
                poA, r_poA = mkp(es, "poA", [128, 512], F32)
                poB, r_poB = mkp(es, "poB", [128, 512], F32)
                pkv, r_pkv = mkp(es, "pkv", [128, 512], F32)
                r_sT = [Res(), Res()]
                r_bl = Res()
                r_kvh = [Res(), Res()]
                dma("sp", wg[:], w_gg[l], writes=[r_wg])
                dma("sp", bg[:], b_gg[l:l + 1, :], writes=[r_bg])
                dma("sp", gvec[:], gla_g[l:l + 1, :].partition_broadcast(128), writes=[r_gvec])
                for p_ in range(3):
                    S.op("dve", lambda p_=p_: nc.vector.memset(Sf[p_][0][:], 0.0), [], [Sf[p_][1]])
                    S.op("dve", lambda p_=p_: nc.vector.memset(Sb[p_][0][:], 0.0), [], [Sb[p_][1]])

                def po_ap(h):
                    return poA[:, h * 128:(h + 1) * 128] if h < 4 else poB[:, (h - 4) * 128:(h - 3) * 128]

                def r_poh(h):
                    return r_poA if h < 4 else r_poB
                def tile3(j):
                    rows = slice(j * 128, (j + 1) * 128)
                    ez, r_ez = ez_ring.next()
                    lz, r_lz = lz_ring.next()
                    eb, r_eb = eb_ring.next()
                    enb, r_enb = enb_ring.next()
                    qkh, r_qkh = qkh_ring.next()
                    qkT, r_qkT = qkT_ring.next()
                    dec, r_dec = dec_ring.next()
                    st, r_st = st_ring.next()
                    mv, r_mv = mv_ring.next()
                    nrm, r_nrm = nrm_ring.next()
                    sil, r_sil = sil_ring.next()
                    gatt, r_gat = gat.next()
                    dma("sp", gatt[:], GAT[:, rows], writes=[r_gat])
                    gqkt, r_gqk = gqk.next()
                    dma("sp", gqkt[:], P[rows, 2816:3584], writes=[r_gqk])
                    gvt, r_gv = gv.next()
                    dma("sp", gvt[:], P[rows, 3584:4352], writes=[r_gv])
                    grt, r_gr = gr.next()
                    dma("sp", grt[:], P[rows, 4352:5120], writes=[r_gr])
                    S.op("pe", lambda gatt=gatt: nc.tensor.matmul(pz[:, 0:384], lhsT=gatt[:], rhs=wg[:], start=True, stop=False),
                         [r_gat, r_wg], [r_pz], signal=False)
                    S.op("pe", lambda: nc.tensor.matmul(pz[:, 0:384], lhsT=ones_f[0:1, :], rhs=bg[:], start=False, stop=True),
                         [r_onesf, r_bg], [r_pz])
                    S.op("act", lambda: nc.scalar.activation(out=ez[:], in_=pz[:, 0:384], func=AF.Exp, scale=-1.0), [r_pz], [r_ez])
                    S.op("act", lambda: nc.scalar.activation(out=lz[:], in_=ez[:], func=AF.Ln, bias=1.0, scale=1.0), [r_ez], [r_lz])
                    S.op("pe", lambda: nc.tensor.matmul(pl[:, 0:384], lhsT=mask01[:], rhs=lz[:], start=True, stop=True),
                         [r_mask01, r_lz], [r_pl])
                    for p_ in range(3):
                        S.op("pe", lambda p_=p_: nc.tensor.matmul(pm[:, 256 + p_:257 + p_], lhsT=lz[:, p_ * 128:(p_ + 1) * 128], rhs=ones_f[:, 0:1],
                                                                  start=True, stop=True), [r_lz, r_onesf], [r_bl], signal=(p_ == 2))
                    S.op("act", lambda: nc.scalar.activation(out=eb[:], in_=pl[:, 0:384], func=AF.Exp, scale=-1.0 / 16.0), [r_pl], [r_eb])
                    S.op("act", lambda: nc.scalar.activation(out=enb[:], in_=pl[:, 0:384], func=AF.Exp, scale=1.0 / 16.0), [r_pl], [r_enb])
                    S.op("act", lambda: nc.scalar.activation(out=dec[:, 0:3], in_=pm[:, 256:259], func=AF.Exp, scale=-1.0 / 16.0), [r_bl], [r_dec])
                    S.op("dve", lambda gqkt=gqkt: nc.vector.scalar_tensor_tensor(out=qkh[:, 0:384], in0=gqkt[:, 0:384], scalar=0.125, in1=eb[:],
                                                                                op0=ALU.mult, op1=ALU.mult), [r_gqk, r_eb], [r_qkh])
                    S.op("dve", lambda gqkt=gqkt: nc.vector.tensor_tensor(out=qkh[:, 384:768], in0=gqkt[:, 384:768], in1=enb[:], op=ALU.mult),
                         [r_gqk, r_enb], [r_qkh])
                    for k in range(6):
                        S.op("pe", lambda k=k: nc.tensor.transpose(out=tp[:, k, :], in_=qkh[:, k * 128:(k + 1) * 128], identity=ident_b[:]),
                             [r_qkh, r_identb], [r_tp], signal=(k == 5))
                    S.op("act", lambda: nc.scalar.copy(out=qkT[:], in_=tp[:, 0:6, :]), [r_tp], [r_qkT])
                    S.op("act", lambda grt=grt: nc.scalar.activation(out=sil[:], in_=grt[:], func=AF.Silu), [r_gr], [r_sil])
                    yield
                    for h in range(6):
                        p_, hh = h // 2, h % 2
                        R = slice(hh * 64, (hh + 1) * 64)
                        sTa = pm[:, (h % 2) * 128:(h % 2 + 1) * 128]
                        rs = r_sT[h % 2]
                        S.op("pe", lambda p_=p_, R=R, sTa=sTa: nc.tensor.matmul(sTa, lhsT=qkT[R, 3 + p_, :], rhs=qkT[R, p_, :], start=True, stop=True),
                             [r_qkT], [rs])
                        smt, r_sm = sm.next()
                        S.op("dve", lambda sTa=sTa, smt=smt: nc.vector.tensor_tensor(out=smt[:], in0=sTa, in1=mask01[:], op=ALU.mult),
                             [rs, r_mask01], [r_sm])
                        S.op("pe", lambda h=h, smt=smt, gvt=gvt: nc.tensor.matmul(po_ap(h), lhsT=smt[:], rhs=gvt[:, h * 128:(h + 1) * 128],
                                                                         start=True, stop=False), [r_sm, r_gv], [r_poh(h)], signal=False)
                        S.op("pe", lambda h=h, p_=p_, R=R, hh=hh: nc.tensor.matmul(po_ap(h), lhsT=qkT[R, p_, :], rhs=Sb[p_][0][R, hh * 128:(hh + 1) * 128],
                                                                          start=False, stop=True), [r_qkT, Sb[p_][1]], [r_poh(h)])
                    for p_ in range(3):
                        kva = pkv[:, (p_ % 2) * 256:(p_ % 2 + 1) * 256]
                        rk = r_kvh[p_ % 2]
                        S.op("pe", lambda p_=p_, kva=kva, gvt=gvt: nc.tensor.matmul(kva, lhsT=qkh[:, 384 + p_ * 128:384 + (p_ + 1) * 128], rhs=gvt[:, p_ * 256:(p_ + 1) * 256],
                                                                          start=True, stop=True), [r_qkh, r_gv], [rk])
                        S.op("dve", lambda p_=p_, kva=kva: nc.vector.tensor_tensor(out=Tt[:], in0=Sf[p_][0][:], in1=kva, op=ALU.add),
                             [Sf[p_][1], rk], [r_Tt])
                        S.op("act", lambda p_=p_: nc.scalar.activation(out=Sf[p_][0][:], in_=Tt[:], func=AF.Copy, scale=dec[:, p_:p_ + 1]), [r_Tt, r_dec], [Sf[p_][1]])
                        S.op("act", lambda p_=p_: nc.scalar.activation(out=Sb[p_][0][:], in_=Tt[:], func=AF.Copy, scale=dec[:, p_:p_ + 1]), [r_Tt, r_dec], [Sb[p_][1]])
                    yield
                    r_pob = Res()
                    for h in range(6):
                        S.op("dve", lambda h=h: nc.vector.bn_stats(out=st[:, h, :], in_=po_ap(h)), [r_poh(h)], [r_st])
                        S.op("dve", lambda h=h: nc.vector.bn_aggr(out=mv[:, h, 0:2], in_=st[:, h, :]), [r_st], [r_mv])
                    rstd(mv[:, :, 2:3], mv[:, :, 1:2], r_mv)
                    for h in range(6):
                        S.op("dve", lambda h=h: nc.vector.tensor_scalar(out=nrm[:, h * 128:(h + 1) * 128], in0=po_ap(h), scalar1=mv[:, h, 0:1],
                                                                        scalar2=mv[:, h, 2:3], op0=ALU.subtract, op1=ALU.mult),
                             [r_poh(h), r_mv], [r_nrm])
                    S.op("pool", lambda: nc.gpsimd.tensor_tensor(out=nrm[:], in0=nrm[:], in1=gvec[:], op=ALU.mult), [r_nrm, r_gvec], [r_nrm])
                    mo, r_mo = mixo.next()
                    S.op("pool", lambda mo=mo: nc.gpsimd.tensor_tensor(out=mo[:], in0=nrm[:], in1=sil[:], op=ALU.mult), [r_nrm, r_sil], [r_mo])
                    dma("sp", MIX[rows, 1280:2048], mo[:], reads=[r_mo])
                run_skewed(tile3, NT)
                S.barrier()
            if stop_phase <= 3:
                break

            with ExitStack() as es:
                QT, r_QT = mk(es, "QT", [128, 6, SEQ], BF16)
                KT, r_KT = mk(es, "KT", [128, 6, SEQ], BF16)
                dm0, r_dm0 = mk(es, "dm0", [128, 256], F32)
                dm1, r_dm1 = mk(es, "dm1", [128, 256], F32)
                mb0, r_mb0 = mk(es, "mb0", [128, 256], BF16)
                mb1, r_mb1 = mk(es, "mb1", [128, 256], BF16)
                V = mk(es, "V", [128, 768], BF16, 5)
                negm = mk(es, "negm", [128, 3], F32, 3)
                pexp = mk(es, "pexp", [128, 3, 256], BF16, 3)
                pTs = mk(es, "pTs", [128, 3, 256], BF16, 3)
                stage = Ring([(es.enter_context(nc.sbuf_tensor("dstage%d_%d" % (k, l), [128, 6, 130], F32)), [Res() for _ in range(2)]) for k in range(4)])
                ps_s = mkp(es, "ps_s", [128, 4, 256], F32, 2)
                ps_t = mkp(es, "ps_t", [128, 4, 256], BF16, 2)
                ps_o = mkp(es, "ps_o", [128, 4, 128], F32, 2)
                dma("sp", dm0[:], c_dmask0, writes=[r_dm0])
                dma("sp", dm1[:], c_dmask1, writes=[r_dm1])
                S.op("dve", lambda: nc.vector.tensor_copy(out=mb0[:], in_=dm0[:]), [r_dm0], [r_mb0])
                S.op("dve", lambda: nc.vector.tensor_copy(out=mb1[:], in_=dm1[:]), [r_dm1], [r_mb1])
                for h in range(6):
                    dma("sp", QT[:, h, :], PT[h * 128:(h + 1) * 128, :], writes=[r_QT])
                    dma("sp", KT[:, h, :], PT[768 + h * 128:768 + (h + 1) * 128, :], writes=[r_KT])
                batches = []
                for pi, dil in enumerate((1, 4, 16)):
                    nb = SEQ // (dil * 128)
                    for r in range(dil):
                        for n in range(nb):
                            for hb in range(2):
                                batches.append((pi, dil, r, n, hb))
                ctxs = {}
                ust = {}

                def sA(b):
                    pi, dil, r, n, hb = batches[b]
                    c = ctxs[b] = {}
                    row0 = n * 128 * dil + r
                    rsl = slice(row0, row0 + 127 * dil + 1, dil)
                    c["rsl"] = rsl
                    if hb == 0:
                        vt, r_v = V.next()
                        dma("sp", vt[:], P[rsl, 2048:2816], writes=[r_v])
                        vprev = ust.get("vprev") if n > 0 else (vt, r_v)
                        stg, r_stg = stage.next()
                        ust["cur"] = (vt, r_v, vprev, stg, r_stg)
                        ust["vprev"] = (vt, r_v)
                    c["u"] = ust["cur"]
                    h0 = hb * 3
                    pst, r_ps = ps_s.next()
                    c["pst"] = (pst, r_ps)
                    for hh in range(3):
                        h = h0 + hh
                        q_ap = QT[:, h, rsl]
                        if n == 0:
                            S.op("pe", lambda: nc.tensor.matmul(pst[:, hh, 128:256], lhsT=q_ap, rhs=KT[:, h, rsl], start=True, stop=False),
                                 [r_QT, r_KT], [r_ps], signal=False)
                            S.op("pe", lambda: nc.tensor.matmul(pst[:, hh, :], lhsT=ident_b[:], rhs=mb0[:], start=False, stop=True),
                                 [r_identb, r_mb0], [r_ps], signal=(hh == 2))
                        else:
                            ksl = slice(row0 - 128 * dil, row0 + 127 * dil + 1, dil)
                            S.op("pe", lambda: nc.tensor.matmul(pst[:, hh, :], lhsT=q_ap, rhs=KT[:, h, ksl], start=True, stop=False),
                                 [r_QT, r_KT], [r_ps], signal=False)
                            S.op("pe", lambda: nc.tensor.matmul(pst[:, hh, :], lhsT=ident_b[:], rhs=mb1[:], start=False, stop=True),
                                 [r_identb, r_mb1], [r_ps], signal=(hh == 2))

                def sB(b):
                    pi, dil, r, n, hb = batches[b]
                    c = ctxs[b]
                    h0 = hb * 3
                    pst, r_ps = c["pst"]
                    vt, r_v, vprev, stg, r_stg = c["u"]
                    S.op("dve", lambda: nc.vector.reduce_max(out=stg[:, h0:h0 + 3, 128], in_=pst[:, 0:3, :], axis=AX.X), [r_ps], [r_stg[hb]])
                    ngt, r_ng = negm.next()
                    S.op("dve", lambda: nc.vector.tensor_scalar(out=ngt[:], in0=stg[:, h0:h0 + 3, 128], scalar1=-1.0, scalar2=None, op0=ALU.mult),
                         [r_stg[hb]], [r_ng])
                    pet, r_pe = pexp.next()
                    c["pet"] = (pet, r_pe)
                    for hh in range(3):
                        S.op("act", lambda: nc.scalar.activation(out=pet[:, hh, :], in_=pst[:, hh, :], func=AF.Exp, bias=ngt[:, hh:hh + 1], scale=1.0,
                                                                 accum_out=stg[:, h0 + hh, 129:130]),
                             [r_ps, r_ng], [r_pe, r_stg[hb]])

                def sC(b):
                    c = ctxs[b]
                    pet, r_pe = c["pet"]
                    ptt, r_pt = ps_t.next()
                    for hh in range(3):
                        for kk in range(2):
                            S.op("pe", lambda: nc.tensor.transpose(out=ptt[:, hh, kk * 128:(kk + 1) * 128], in_=pet[:, hh, kk * 128:(kk + 1) * 128], identity=ident_b[:]),
                                 [r_pe, r_identb], [r_pt], signal=(hh == 2 and kk == 1))
                    pts, r_pts = pTs.next()
                    c["pts"] = (pts, r_pts)
                    S.op("dve", lambda: nc.vector.tensor_copy(out=pts[:], in_=ptt[:, 0:3, :]), [r_pt], [r_pts])

                def sD(b):
                    pi, dil, r, n, hb = batches[b]
                    c = ctxs.pop(b)
                    h0 = hb * 3
                    pts, r_pts = c["pts"]
                    vt, r_v, vprev, stg, r_stg = c["u"]
                    pot, r_po2 = ps_o.next()
                    for hh in range(3):
                        h = h0 + hh
                        S.op("pe", lambda: nc.tensor.matmul(pot[:, hh, :], lhsT=pts[:, hh, 0:128], rhs=vprev[0][:, h * 128:(h + 1) * 128], start=True, stop=False),
                             [r_pts, vprev[1]], [r_po2], signal=False)
                        S.op("pe", lambda: nc.tensor.matmul(pot[:, hh, :], lhsT=pts[:, hh, 128:256], rhs=vt[:, h * 128:(h + 1) * 128], start=False, stop=True),
                             [r_pts, r_v], [r_po2], signal=(hh == 2))
                    S.op("act", lambda: nc.scalar.copy(out=stg[:, h0:h0 + 3, 0:128], in_=pot[:, 0:3, :]), [r_po2], [r_stg[hb]])
                    if hb == 1:
                        dma("sp", DO[pi, c["rsl"], :], stg[:].rearrange("p a b -> p (a b)"), reads=r_stg)

                nbt = len(batches)
                for t in range(nbt + 3):
                    if 0 <= t - 3 < nbt:
                        sD(t - 3)
                    if 0 <= t - 2 < nbt:
                        sC(t - 2)
                    if 0 <= t - 1 < nbt:
                        sB(t - 1)
                    if t < nbt:
                        sA(t)
                S.barrier()
            with ExitStack() as es:
                D3 = mk(es, "D3", [128, 3, 780], F32, 2)
                mxx, r_mxx = mk(es, "mxx", [128, 6], F32)
                e3, r_e3 = mk(es, "e3", [128, 3, 6], F32)
                w3, r_w3 = mk(es, "w3", [128, 3, 6], F32)
                dn, r_dn = mk(es, "dn", [128, 6], F32)
                cf, r_cf = mk(es, "cf", [128, 3, 6], F32)
                acc = [mk(es, "dacc%d" % h, [128, 128], F32) for h in range(6)]
                outb = Ring([(es.enter_context(nc.sbuf_tensor("doutb%d_%d" % (k, l), [128, 768], BF16)), [Res() for _ in range(6)]) for k in range(2)])
                for j in range(NT):
                    rows = slice(j * 128, (j + 1) * 128)
                    d3, r_d3 = D3.next()
                    dma("sp", d3[:], DO[:, rows, :].rearrange("a p n -> p a n"), writes=[r_d3])
                    d4 = d3[:].rearrange("p a (h c) -> p a h c", h=6)
                    S.op("dve", lambda d4=d4: nc.vector.tensor_tensor(out=mxx[:], in0=d4[:, 0, :, 128], in1=d4[:, 1, :, 128], op=ALU.max), [r_d3], [r_mxx])
                    S.op("dve", lambda d4=d4: nc.vector.tensor_tensor(out=mxx[:], in0=mxx[:], in1=d4[:, 2, :, 128], op=ALU.max), [r_d3, r_mxx], [r_mxx])
                    for p_ in range(3):
                        S.op("dve", lambda d4=d4, p_=p_: nc.vector.tensor_tensor(out=e3[:, p_, :], in0=d4[:, p_, :, 128], in1=mxx[:], op=ALU.subtract), [r_d3, r_mxx], [r_e3])
                    S.op("act", lambda: nc.scalar.activation(out=e3[:], in_=e3[:], func=AF.Exp), [r_e3], [r_e3])
                    for p_ in range(3):
                        S.op("dve", lambda d4=d4, p_=p_: nc.vector.tensor_tensor(out=w3[:, p_, :], in0=e3[:, p_, :], in1=d4[:, p_, :, 129], op=ALU.mult), [r_d3, r_e3], [r_w3])
                    S.op("dve", lambda: nc.vector.tensor_tensor(out=dn[:], in0=w3[:, 0, :], in1=w3[:, 1, :], op=ALU.add), [r_w3], [r_dn])
                    S.op("dve", lambda: nc.vector.tensor_tensor(out=dn[:], in0=dn[:], in1=w3[:, 2, :], op=ALU.add), [r_w3, r_dn], [r_dn])
                    S.op("dve", lambda: nc.vector.reciprocal(out=dn[:], in_=dn[:]), [r_dn], [r_dn])
                    for p_ in range(3):
                        S.op("dve", lambda p_=p_: nc.vector.tensor_tensor(out=cf[:, p_, :], in0=e3[:, p_, :], in1=dn[:], op=ALU.mult), [r_e3, r_dn], [r_cf])
                    ob, r_ob = outb.next()
                    for h in range(6):
                        eng = "dve"
                        at, r_at = acc[h]
                        S.op(eng, lambda eng=eng, at=at, d4=d4, h=h: E[eng].tensor_scalar(out=at[:], in0=d4[:, 0, h, 0:128], scalar1=cf[:, 0, h:h + 1], scalar2=None, op0=ALU.mult),
                             [r_d3, r_cf], [r_at])
                        S.op(eng, lambda eng=eng, at=at, d4=d4, h=h: E[eng].scalar_tensor_tensor(out=at[:], in0=d4[:, 1, h, 0:128], scalar=cf[:, 1, h:h + 1], in1=at[:], op0=ALU.mult, op1=ALU.add),
                             [r_d3, r_cf, r_at], [r_at])
                        S.op(eng, lambda eng=eng, at=at, d4=d4, h=h, ob=ob: E[eng].scalar_tensor_tensor(out=ob[:, h * 128:(h + 1) * 128], in0=d4[:, 2, h, 0:128], scalar=cf[:, 2, h:h + 1], in1=at[:], op0=ALU.mult, op1=ALU.add),
                             [r_d3, r_cf, r_at], [r_ob[h]])
                    dma("sp", MIX[rows, 512:1280], ob[:], reads=r_ob)
                S.barrier()
            if stop_phase <= 4:
                break

            with ExitStack() as es:
                wo, r_wo = mk(es, "wo", [128, 16, DM], BF16)
                g1, r_g1 = mk(es, "g1", [128, DM], F32)
                b1, r_b1 = mk(es, "b1", [128, DM], F32)
                wr, r_wr = mk(es, "wr", [128, 16, 36], F32)
                br, r_br = mk(es, "br", [1, 36], F32)
                mixl = mk(es, "mixl", [128, DM], BF16, 2)
                mT_ring = mk(es, "mT", [128, 16, 128], BF16, 2)
                xr = mk(es, "xr", [128, DM], F32, 2)
                u_ring = mk(es, "u5", [128, DM], F32, 2)
                x1 = mk(es, "x1t", [128, DM], F32, 3)
                x1b = mk(es, "x1bt", [128, DM], BF16, 2)
                x1T_ring = mk(es, "x1T", [128, 16, 128], F32, 2)
                st_ring = mk(es, "st5", [128, 4, 6], F32, 2)
                mv_ring = mk(es, "mv5", [128, 3], F32, 2)
                L_ring = mk(es, "L", [128, 36], F32, 2)
                sc_ring = mk(es, "sc5", [128, 16], F32, 2)
                goh_ring = mk(es, "goh", [128, 4], F32, 2)
                gex_ring = mk(es, "gex", [128, 4], F32, 2)
                pen_ring = mk(es, "pen", [128, 4], F32, 2)
                em_ring = mk(es, "em", [128, 32], F32, 2)
                em2_ring = mk(es, "em2", [128, 32], F32, 2)
                tp = mkp(es, "tp5", [128, 512], BF16, 2)
                po = mkp(es, "po5", [128, 512], F32, 4)
                tpf, r_tpf = mkp(es, "tpf", [128, 512], F32)
                plog, r_plog = mkp(es, "plog", [128, 64], F32)
                dma("pool", wo[:], w_out[l].rearrange("(k p) n -> p k n", p=128), writes=[r_wo])
                dma("sp", g1[:], ln1_g[l:l + 1, :].partition_broadcast(128), writes=[r_g1])
                dma("sp", b1[:], ln1_b[l:l + 1, :].partition_broadcast(128), writes=[r_b1])
                dma("sp", wr[:], w_rt[l].rearrange("(k p) n -> p k n", p=128), writes=[r_wr])
                dma("sp", br[:], b_rt[l:l + 1, :], writes=[r_br])
                def tile5(j):
                    rows = slice(j * 128, (j + 1) * 128)
                    mT, r_mT = mT_ring.next()
                    u, r_u = u_ring.next()
                    x1T, r_x1T = x1T_ring.next()
                    st, r_st = st_ring.next()
                    mv, r_mv = mv_ring.next()
                    L, r_L = L_ring.next()
                    sc, r_sc = sc_ring.next()
                    goh, r_goh = goh_ring.next()
                    gex, r_gex = gex_ring.next()
                    pen, r_pen = pen_ring.next()
                    em, r_em = em_ring.next()
                    em2, r_em2 = em2_ring.next()
                    ml, r_ml = mixl.next()
                    dma("sp", ml[:], MIX[rows, :], writes=[r_ml])
                    xrt, r_xr = xr.next()
                    dma("sp", xrt[:], XCUR[rows, :], writes=[r_xr])
                    for g4 in range(4):
                        tpt, r_tp = tp.next()
                        for k in range(4):
                            kc = g4 * 4 + k
                            S.op("pe", lambda kc=kc, k=k, tpt=tpt, ml=ml: nc.tensor.transpose(out=tpt[:, k * 128:(k + 1) * 128], in_=ml[:, kc * 128:(kc + 1) * 128], identity=ident_b[:]),
                                 [r_ml, r_identb], [r_tp], signal=(k == 3))
                        eng = alt("act", "dve")
                        S.op(eng, lambda eng=eng, tpt=tpt, g4=g4: ecopy(eng, mT[:, g4 * 4:(g4 + 1) * 4, :], tpt[:].rearrange("p (k t) -> p k t", k=4)), [r_tp], [r_mT])
                    for n4 in range(4):
                        pot, r_po5 = po.next()
                        for kc in range(16):
                            S.op("pe", lambda kc=kc, pot=pot, n4=n4: nc.tensor.matmul(pot[:], lhsT=mT[:, kc, :], rhs=wo[:, kc, n4 * 512:(n4 + 1) * 512], start=(kc == 0), stop=(kc == 15)),
                                 [r_mT, r_wo], [r_po5], signal=(kc == 15))
                        S.op("dve", lambda pot=pot, n4=n4, xrt=xrt: nc.vector.scalar_tensor_tensor(out=u[:, n4 * 512:(n4 + 1) * 512], in0=xrt[:, n4 * 512:(n4 + 1) * 512], scalar=ALPHA,
                                                                                                  in1=pot[:], op0=ALU.mult, op1=ALU.add), [r_xr, r_po5], [r_u])
                    yield
                    x1t, r_x1 = x1.next()
                    layer_norm_tile(None, u, r_u, g1, r_g1, b1, r_b1, st, r_st, mv, r_mv, x1t, r_x1)
                    dma("sp", X1[rows, :], x1t[:], reads=[r_x1])
                    xbt, r_xb1 = x1b.next()
                    S.op("act", lambda xbt=xbt, x1t=x1t: nc.scalar.copy(out=xbt[:], in_=x1t[:]), [r_x1], [r_xb1])
                    dma("sp", X1B[rows, :], xbt[:], reads=[r_xb1])
                    yield
                    for g4 in range(4):
                        for k in range(4):
                            kc = g4 * 4 + k
                            S.op("pe", lambda kc=kc, k=k, x1t=x1t: nc.tensor.transpose(out=tpf[:, k * 128:(k + 1) * 128], in_=x1t[:, kc * 128:(kc + 1) * 128], identity=ident_f[:]),
                                 [r_x1, r_identf], [r_tpf], signal=(k == 3))
                        eng = alt("act", "dve")
                        S.op(eng, lambda eng=eng, g4=g4: ecopy(eng, x1T[:, g4 * 4:(g4 + 1) * 4, :], tpf[:].rearrange("p (k t) -> p k t", k=4)), [r_tpf], [r_x1T])
                    for kc in range(16):
                        S.op("pe", lambda kc=kc: nc.tensor.matmul(plog[:, 0:36], lhsT=x1T[:, kc, :], rhs=wr[:, kc, :], start=(kc == 0), stop=False), [r_x1T, r_wr], [r_plog], signal=False)
                    S.op("pe", lambda: nc.tensor.matmul(plog[:, 0:36], lhsT=ones_f[0:1, :], rhs=br[:], start=False, stop=True), [r_onesf, r_br], [r_plog])
                    S.op("act", lambda: nc.scalar.copy(out=L[:], in_=plog[:, 0:36]), [r_plog], [r_L])
                    yield
                    V_ = nc.vector
                    S.op("dve", lambda: V_.reduce_max(out=sc[:, 0:1], in_=L[:, 0:4], axis=AX.X), [r_L], [r_sc])
                    S.op("dve", lambda: V_.tensor_scalar(out=goh[:], in0=L[:, 0:4], scalar1=sc[:, 0:1], scalar2=None, op0=ALU.is_ge), [r_L, r_sc], [r_goh])
                    S.op("dve", lambda: V_.tensor_scalar(out=sc[:, 1:2], in0=sc[:, 0:1], scalar1=-1.0, scalar2=None, op0=ALU.mult), [r_sc], [r_sc])
                    S.op("act", lambda: nc.scalar.activation(out=gex[:], in_=L[:, 0:4], func=AF.Exp, bias=sc[:, 1:2], scale=1.0, accum_out=sc[:, 2:3]), [r_L, r_sc], [r_gex, r_sc])
                    S.op("dve", lambda: V_.reciprocal(out=sc[:, 3:4], in_=sc[:, 2:3]), [r_sc], [r_sc])
                    S.op("dve", lambda: V_.tensor_scalar(out=pen[:], in0=goh[:], scalar1=BIG, scalar2=-BIG, op0=ALU.mult, op1=ALU.add), [r_goh], [r_pen])
                    for g in range(4):
                        S.op("dve", lambda g=g: V_.tensor_scalar(out=em[:, g * 8:(g + 1) * 8], in0=L[:, 4 + g * 8:4 + (g + 1) * 8], scalar1=pen[:, g:g + 1], scalar2=None, op0=ALU.add),
                             [r_L, r_pen], [r_em])
                    S.op("dve", lambda: V_.reduce_max(out=sc[:, 4:5], in_=em[:], axis=AX.X), [r_em], [r_sc])
                    S.op("dve", lambda j=j: V_.tensor_scalar(out=OH[:, j, 0, :], in0=em[:], scalar1=sc[:, 4:5], scalar2=None, op0=ALU.is_ge), [r_em, r_sc], [r_OH])
                    S.op("dve", lambda j=j: V_.scalar_tensor_tensor(out=em2[:], in0=OH[:, j, 0, :], scalar=-BIG, in1=em[:], op0=ALU.mult, op1=ALU.add), [r_OH, r_em], [r_em2])
                    S.op("dve", lambda: V_.reduce_max(out=sc[:, 5:6], in_=em2[:], axis=AX.X), [r_em2], [r_sc])
                    S.op("dve", lambda j=j: V_.tensor_scalar(out=OH[:, j, 1, :], in0=em2[:], scalar1=sc[:, 5:6], scalar2=None, op0=ALU.is_ge), [r_em2, r_sc], [r_OH])
                    S.op("dve", lambda: V_.tensor_tensor(out=sc[:, 6:7], in0=sc[:, 5:6], in1=sc[:, 4:5], op=ALU.subtract), [r_sc], [r_sc])
                    S.op("act", lambda: nc.scalar.activation(out=sc[:, 7:8], in_=sc[:, 6:7], func=AF.Exp), [r_sc], [r_sc])
                    S.op("dve", lambda: V_.tensor_scalar(out=sc[:, 8:9], in0=sc[:, 7:8], scalar1=1.0, scalar2=None, op0=ALU.add), [r_sc], [r_sc])
                    S.op("dve", lambda: V_.reciprocal(out=sc[:, 9:10], in_=sc[:, 8:9]), [r_sc], [r_sc])
                    S.op("dve", lambda j=j: V_.tensor_tensor(out=GATE[:, j, 0:1], in0=sc[:, 3:4], in1=sc[:, 9:10], op=ALU.mult), [r_sc], [r_GATE])
                    S.op("dve", lambda: V_.tensor_tensor(out=sc[:, 10:11], in0=sc[:, 7:8], in1=sc[:, 9:10], op=ALU.mult), [r_sc], [r_sc])
                    S.op("dve", lambda j=j: V_.tensor_tensor(out=GATE[:, j, 1:2], in0=sc[:, 3:4], in1=sc[:, 10:11], op=ALU.mult), [r_sc], [r_GATE])
                    S.op("dve", lambda j=j: V_.tensor_tensor(out=A_b[:, j, :], in0=OH[:, j, 0, :], in1=OH[:, j, 1, :], op=ALU.add), [r_OH], [r_Ab])
                run_skewed(tile5, NT)
                S.barrier()
            if stop_phase <= 5:
                break

            with ExitStack() as es:
                V_ = nc.vector
                cnt, r_cnt = mk(es, "cnt", [128, 32], F32)
                pc, r_pc = mk(es, "pc", [128, 32], F32)
                pa, r_pa = mk(es, "pa", [128, 32], F32)
                pb, r_pb = mk(es, "pb", [128, 32], F32)
                pstart, r_pstart = mk(es, "pstart", [128, 32], F32)
                carry, r_carry = mk(es, "carry", [128, 32], F32)
                pos, r_pos = mk(es, "pos", [128, 32], F32)
                tmp, r_tmp = mk(es, "tmp6", [128, 32], F32)
                destf, r_destf = mk(es, "destf", [128, NT, 2], F32)
                EB, r_EB = mk(es, "EB", [128, NBLK], F32)
                bsg, r_bsg = mk(es, "bsg", [128, NBLK], F32)
                bsd, r_bsd = mk(es, "bsd", [128, NBLK], F32)
                ioi, r_ioi = mk(es, "ioi", [128, 1], I32)
                iof, r_iof = mk(es, "iof", [128, 1], F32)
                iof4, r_iof4 = mk(es, "iof4", [128, 1], F32)
                strib, r_strib = mk(es, "strib", [128, 128], BF16)
                strif, r_strif = mk(es, "strif", [128, 128], F32)
                xs = mk(es, "xs6", [128, DM], BF16, 3)
                ptot, r_ptot = mkp(es, "ptot", [128, 64], F32)
                prk = mkp(es, "prk", [128, 64], F32, 2)
                dma("sp", strif[:], c_stri, writes=[r_strif])
                S.op("dve", lambda: V_.tensor_copy(out=strib[:], in_=strif[:]), [r_strif], [r_strib])
                S.op("pool", lambda: nc.gpsimd.iota(ioi[:], pattern=[[0, 1]], base=0, channel_multiplier=1), [], [r_ioi])
                S.op("dve", lambda: V_.tensor_copy(out=iof[:], in_=ioi[:]), [r_ioi], [r_iof])
                for j in range(NT):
                    S.op("pe", lambda j=j: nc.tensor.matmul(ptot[:, 0:32], lhsT=ones_b[:], rhs=A_b[:, j, :], start=(j == 0), stop=(j == NT - 1)), [r_onesb, r_Ab], [r_ptot], signal=(j == NT - 1))
                S.op("dve", lambda: V_.tensor_copy(out=cnt[:], in_=ptot[:, 0:32]), [r_ptot], [r_cnt])
                S.op("dve", lambda: V_.memset(tmp[:], 0.0), [], [r_tmp])
                for m_ in range(-(-SEQ // BLK)):
                    S.op("dve", lambda m_=m_: V_.scalar_tensor_tensor(out=tmp[:], in0=cnt[:], scalar=float(m_ * BLK), in1=tmp[:], op0=ALU.is_gt, op1=ALU.add), [r_cnt, r_tmp], [r_tmp])
                S.op("dve", lambda: V_.tensor_scalar(out=pc[:], in0=tmp[:], scalar1=float(BLK), scalar2=None, op0=ALU.mult), [r_tmp], [r_pc])
                S.op("dve", lambda: V_.tensor_copy(out=pa[:], in_=pc[:]), [r_pc], [r_pa])
                cur, r_cur, oth, r_oth = pa, r_pa, pb, r_pb
                for sh in (1, 2, 4, 8, 16):
                    S.op("dve", lambda cur=cur, oth=oth, sh=sh: V_.tensor_copy(out=oth[:, 0:sh], in_=cur[:, 0:sh]), [r_cur], [r_oth])
                    S.op("dve", lambda cur=cur, oth=oth, sh=sh: V_.tensor_tensor(out=oth[:, sh:32], in0=cur[:, sh:32], in1=cur[:, 0:32 - sh], op=ALU.add), [r_cur], [r_oth])
                    cur, r_cur, oth, r_oth = oth, r_oth, cur, r_cur
                pend, r_pend = cur, r_cur
                S.op("dve", lambda: V_.tensor_tensor(out=pstart[:], in0=pend[:], in1=pc[:], op=ALU.subtract), [r_pend, r_pc], [r_pstart])
                S.op("dve", lambda: V_.memset(carry[:], 0.0), [], [r_carry])
                for j in range(NT):
                    prt, r_pr = prk.next()
                    S.op("pe", lambda prt=prt, j=j: nc.tensor.matmul(prt[:, 0:32], lhsT=strib[:], rhs=A_b[:, j, :], start=True, stop=True), [r_strib, r_Ab], [r_pr], signal=False)
                    S.op("pe", lambda prt=prt, j=j: nc.tensor.matmul(prt[:, 32:64], lhsT=ones_b[:], rhs=A_b[:, j, :], start=True, stop=True), [r_onesb, r_Ab], [r_pr])
                    S.op("dve", lambda prt=prt: V_.tensor_tensor(out=pos[:], in0=prt[:, 0:32], in1=carry[:], op=ALU.add), [r_pr, r_carry], [r_pos])
                    S.op("dve", lambda: V_.tensor_tensor(out=pos[:], in0=pos[:], in1=pstart[:], op=ALU.add), [r_pos, r_pstart], [r_pos])
                    for k in range(2):
                        S.op("dve", lambda j=j, k=k: V_.tensor_tensor(out=tmp[:], in0=OH[:, j, k, :], in1=pos[:], op=ALU.mult), [r_OH, r_pos], [r_tmp])
                        S.op("dve", lambda j=j, k=k: V_.reduce_sum(out=destf[:, j, k:k + 1], in_=tmp[:], axis=AX.X), [r_tmp], [r_destf])
                    S.op("dve", lambda prt=prt: V_.tensor_tensor(out=carry[:], in0=carry[:], in1=prt[:, 32:64], op=ALU.add), [r_pr, r_carry], [r_carry])
                S.op("dve", lambda: V_.tensor_copy(out=DESTI[:], in_=destf[:]), [r_destf], [r_DESTI])
                for j in range(NT):
                    rows = slice(j * 128, (j + 1) * 128)
                    xst, r_xs = xs.next()
                    dma("sp", xst[:], X1B[rows, :], writes=[r_xs])
                    for k in range(2):
                        S.dma("pool", lambda xst=xst, j=j, k=k: nc.gpsimd.indirect_dma_start(
                            out=XG, out_offset=bass.IndirectOffsetOnAxis(ap=DESTI[:, j, k:k + 1], axis=0), in_=xst[:], in_offset=None),
                            [r_xs, r_DESTI], [])
                for i in range(NBLK):
                    S.op("dve", lambda i=i: V_.tensor_scalar(out=tmp[:], in0=pend[:], scalar1=float(i * BLK), scalar2=None, op0=ALU.is_le), [r_pend], [r_tmp])
                    S.op("dve", lambda i=i: V_.reduce_sum(out=EB[:, i:i + 1], in_=tmp[:], axis=AX.X), [r_tmp], [r_EB])
                S.op("dve", lambda: V_.tensor_scalar(out=iof4[:], in0=iof[:], scalar1=4.0, scalar2=None, op0=ALU.mult), [r_iof], [r_iof4])
                S.op("dve", lambda: V_.tensor_scalar(out=bsg[:], in0=EB[:], scalar1=512.0, scalar2=iof4[:, 0:1], op0=ALU.mult, op1=ALU.add), [r_EB, r_iof4], [r_bsg])
                S.op("dve", lambda: V_.tensor_scalar(out=bsd[:], in0=EB[:], scalar1=512.0, scalar2=iof[:, 0:1], op0=ALU.mult, op1=ALU.add), [r_EB, r_iof], [r_bsd])
                for q4 in range(4):
                    S.op("dve", lambda q4=q4: V_.tensor_scalar(out=IDXG[:, :, q4], in0=bsg[:], scalar1=float(q4 + l * 32 * 512), scalar2=None, op0=ALU.add), [r_bsg], [r_IDXG])
                for fc in range(4):
                    S.op("dve", lambda fc=fc: V_.tensor_scalar(out=IDXD[:, :, fc], in0=bsd[:], scalar1=float(fc * 128 + l * 32 * 512), scalar2=None, op0=ALU.add), [r_bsd], [r_IDXD])
                S.barrier()
            if stop_phase <= 6:
                break

            with ExitStack() as es:
                def wring(name, shape, nres):
                    return Ring([(es.enter_context(nc.sbuf_tensor("%s%d_%d" % (name, k, l), shape, BF16)), [Res() for _ in range(nres)]) for k in range(2)])
                wgr = wring("wgt", [128, 16, 512], 16)
                wur = wring("wut", [128, 16, 512], 16)
                wdr = wring("wdt", [128, 4, DM], 4)
                xg = mk(es, "xg", [128, DM], BF16, 2)
                xgT = mk(es, "xgT", [128, 16, BLK], BF16, 2)
                sgm = mk(es, "sgm", [128, BLK], F32, 2)
                hT = mk(es, "hT", [128, 4, BLK], BF16, 2)
                ys = mk(es, "ys", [128, DM], F32, 2)
                tp = mkp(es, "tp7", [128, 512], BF16, 2)
                pg = mkp(es, "pg", [128, BLK], F32, 2)
                pu = mkp(es, "pu", [128, BLK], F32, 2)
                py = mkp(es, "py", [128, 512], F32, 2)
                bc_g = nc.gpsimd.to_reg((l + 1) * 32 * 512 - 1)
                bc_d = nc.gpsimd.to_reg((l + 1) * 32 * 512 - 1)
                for (ring_, nres_) in ((wgr, 16), (wur, 16), (wdr, 4)):
                    for (t_, rs_) in ring_.items:
                        S.op("pool", lambda t_=t_: nc.gpsimd.memset(t_[:], 0.0), [], rs_)
                def blk7(i):
                    wgt, r_wg = wgr.next()
                    wut, r_wu = wur.next()
                    wdt, r_wd = wdr.next()
                    for q4 in range(4):
                        S.dma("pool", lambda wgt=wgt, i=i, q4=q4: nc.gpsimd.indirect_dma_start(
                            out=wgt[:, 4 * q4:4 * q4 + 4, :].rearrange("p a b -> p (a b)"), out_offset=None, in_=w_eg, in_offset=bass.IndirectOffsetOnAxis(ap=IDXG[:, i, q4:q4 + 1], axis=0), bounds_check=bc_g, oob_is_err=False),
                            [r_IDXG], [r_wg[q4]])
                        S.dma("pool", lambda wut=wut, i=i, q4=q4: nc.gpsimd.indirect_dma_start(
                            out=wut[:, 4 * q4:4 * q4 + 4, :].rearrange("p a b -> p (a b)"), out_offset=None, in_=w_eu, in_offset=bass.IndirectOffsetOnAxis(ap=IDXG[:, i, q4:q4 + 1], axis=0), bounds_check=bc_g, oob_is_err=False),
                            [r_IDXG], [r_wu[q4]])
                    for fc in range(4):
                        S.dma("pool", lambda wdt=wdt, i=i, fc=fc: nc.gpsimd.indirect_dma_start(
                            out=wdt[:, fc, :], out_offset=None, in_=w_ed, in_offset=bass.IndirectOffsetOnAxis(ap=IDXD[:, i, fc:fc + 1], axis=0), bounds_check=bc_d, oob_is_err=False),
                            [r_IDXD], [r_wd[fc]])
                    xTt, r_xgT = xgT.next()
                    for sb in range(BLK // 128):
                        xgt, r_xg = xg.next()
                        r0 = i * BLK + sb * 128
                        dma("sp", xgt[:], XG[r0:r0 + 128, :], writes=[r_xg])
                        for g4 in range(4):
                            tpt, r_tp = tp.next()
                            for k in range(4):
                                kc = g4 * 4 + k
                                S.op("pe", lambda kc=kc, k=k, tpt=tpt, xgt=xgt: nc.tensor.transpose(out=tpt[:, k * 128:(k + 1) * 128], in_=xgt[:, kc:kc + 127 * 16 + 1:16], identity=ident_b[:]),
                                     [r_xg, r_identb], [r_tp], signal=(k == 3))
                            eng = alt("act", "dve")
                            S.op(eng, lambda eng=eng, tpt=tpt, g4=g4, sb=sb, xTt=xTt: ecopy(eng, xTt[:, g4 * 4:(g4 + 1) * 4, sb * 128:(sb + 1) * 128], tpt[:].rearrange("p (k t) -> p k t", k=4)),
                                 [r_tp], [r_xgT])
                    yield
                    hTt, r_hT = hT.next()
                    for fc in range(4):
                        pgt, r_pg = pg.next()
                        put, r_pu = pu.next()
                        for kc in range(16):
                            S.op("pe", lambda kc=kc, fc=fc, pgt=pgt, wgt=wgt, xTt=xTt: nc.tensor.matmul(pgt[:], lhsT=wgt[:, kc, fc * 128:(fc + 1) * 128], rhs=xTt[:, kc, :], start=(kc == 0), stop=(kc == 15)),
                                 [r_wg[kc // 4], r_xgT], [r_pg], signal=(kc == 15))
                        for kc in range(16):
                            S.op("pe", lambda kc=kc, fc=fc, put=put, wut=wut, xTt=xTt: nc.tensor.matmul(put[:], lhsT=wut[:, kc, fc * 128:(fc + 1) * 128], rhs=xTt[:, kc, :], start=(kc == 0), stop=(kc == 15)),
                                 [r_wu[kc // 4], r_xgT], [r_pu], signal=(kc == 15))
                        sgt, r_sg = sgm.next()
                        S.op("act", lambda sgt=sgt, pgt=pgt: nc.scalar.activation(out=sgt[:], in_=pgt[:], func=AF.Silu), [r_pg], [r_sg])
                        S.op("dve", lambda sgt=sgt, put=put, hTt=hTt, fc=fc: nc.vector.tensor_tensor(out=hTt[:, fc, :], in0=sgt[:], in1=put[:], op=ALU.mult), [r_sg, r_pu], [r_hT])
                    yield
                    for sb in range(BLK // 128):
                        yst, r_ys = ys.next()
                        for n4 in range(4):
                            pyt, r_py = py.next()
                            for fc in range(4):
                                S.op("pe", lambda fc=fc, pyt=pyt, hTt=hTt, wdt=wdt, sb=sb, n4=n4: nc.tensor.matmul(pyt[:], lhsT=hTt[:, fc, sb * 128:(sb + 1) * 128], rhs=wdt[:, fc, n4 * 512:(n4 + 1) * 512], start=(fc == 0), stop=(fc == 3)),
                                     [r_hT, r_wd[fc]], [r_py], signal=(fc == 3))
                            eng = alt("act", "dve")
                            S.op(eng, lambda eng=eng, yst=yst, pyt=pyt, n4=n4: ecopy(eng, yst[:, n4 * 512:(n4 + 1) * 512], pyt[:]), [r_py], [r_ys])
                        r0 = i * BLK + sb * 128
                        dma("sp", YB[r0:r0 + 128, :], yst[:], reads=[r_ys])
                run_skewed(blk7, NBLK)
                S.barrier()
                nc.gpsimd.free_register(bc_g)
                nc.gpsimd.free_register(bc_d)
            if stop_phase <= 7:
                break

            with ExitStack() as es:
                g2, r_g2 = mk(es, "g2", [128, DM], F32)
                b2, r_b2 = mk(es, "b2", [128, DM], F32)
                y1 = mk(es, "y1", [128, DM], F32, 4)
                y2 = mk(es, "y2", [128, DM], F32, 4)
                x1r = mk(es, "x1r", [128, DM], F32, 4)
                u_ring = mk(es, "u8", [128, DM], F32, 2)
                xo = mk(es, "xo", [128, DM], F32, 2)
                st_ring = mk(es, "st8", [128, 4, 6], F32, 2)
                mv_ring = mk(es, "mv8", [128, 3], F32, 2)
                dma("sp", g2[:], ln2_g[l:l + 1, :].partition_broadcast(128), writes=[r_g2])
                dma("sp", b2[:], ln2_b[l:l + 1, :].partition_broadcast(128), writes=[r_b2])
                def tile8(j):
                    rows = slice(j * 128, (j + 1) * 128)
                    u, r_u = u_ring.next()
                    st, r_st = st_ring.next()
                    mv, r_mv = mv_ring.next()
                    y1t, r_y1 = y1.next()
                    y2t, r_y2 = y2.next()
                    S.dma("pool", lambda y1t=y1t, j=j: nc.gpsimd.indirect_dma_start(out=y1t[:], out_offset=None, in_=YB, in_offset=bass.IndirectOffsetOnAxis(ap=DESTI[:, j, 0:1], axis=0)),
                          [r_DESTI], [r_y1])
                    S.dma("pool", lambda y2t=y2t, j=j: nc.gpsimd.indirect_dma_start(out=y2t[:], out_offset=None, in_=YB, in_offset=bass.IndirectOffsetOnAxis(ap=DESTI[:, j, 1:2], axis=0)),
                          [r_DESTI], [r_y2])
                    xt_, r_x1r = x1r.next()
                    dma("sp", xt_[:], X1[rows, :], writes=[r_x1r])
                    yield
                    yield
                    S.op("dve", lambda y1t=y1t, j=j: nc.vector.tensor_scalar(out=u[:], in0=y1t[:], scalar1=GATE[:, j, 0:1], scalar2=None, op0=ALU.mult), [r_y1, r_GATE], [r_u])
                    S.op("dve", lambda y2t=y2t, j=j: nc.vector.scalar_tensor_tensor(out=u[:], in0=y2t[:], scalar=GATE[:, j, 1:2], in1=u[:], op0=ALU.mult, op1=ALU.add), [r_y2, r_GATE, r_u], [r_u])
                    S.op("dve", lambda xt_=xt_: nc.vector.scalar_tensor_tensor(out=u[:], in0=xt_[:], scalar=ALPHA, in1=u[:], op0=ALU.mult, op1=ALU.add), [r_x1r, r_u], [r_u])
                    yield
                    xot, r_xo = xo.next()
                    layer_norm_tile(None, u, r_u, g2, r_g2, b2, r_b2, st, r_st, mv, r_mv, xot, r_xo)
                    dma("sp", XNEXT[rows, :], xot[:], reads=[r_xo])
                run_skewed(tile8, NT)
                S.barrier()
            XCUR = XNEXT
    return nc, S


def make_inputs(inputs, core, stop_phase=99):
    hc = host_consts()
    m = {}
    m["x"] = np.ascontiguousarray(inputs["x"][core])
    for k in ("w_in", "w_gla_gate", "b_gla_gate", "ret_norm_g", "gla_norm_g", "w_out", "ln1_g", "ln1_b", "ln2_g", "ln2_b"):
        m[k] = np.ascontiguousarray(inputs[k])
    m["w_router"] = np.ascontiguousarray(np.concatenate([inputs["w_router_group"], inputs["w_router_expert"]], axis=-1))
    m["b_router"] = np.ascontiguousarray(np.concatenate([inputs["b_router_group"], inputs["b_router_expert"]], axis=-1))
    if stop_phase >= 7:
        m["w_expert_gate"] = np.ascontiguousarray(inputs["w_expert_gate"]).reshape(DEPTH * 32 * 128 * 4, 2048)
        m["w_expert_up"] = np.ascontiguousarray(inputs["w_expert_up"]).reshape(DEPTH * 32 * 128 * 4, 2048)
        m["w_expert_down"] = np.ascontiguousarray(inputs["w_expert_down"]).reshape(DEPTH * 32 * 512, DM)
    m["c_ident"] = hc["ident"]
    m["c_mask01"] = hc["mask01"]
    m["c_stri"] = hc["stri"]
    m["c_dmask0"] = hc["dmask0"]
    m["c_dmask1"] = hc["dmask1"]
    m["c_rope"] = hc["rope"]
    return m


def kernel(**inputs):
    inputs = {k: np.asarray(v) for k, v in inputs.items()}
    nc, _ = build()
    in_maps = [make_inputs(inputs, c) for c in range(8)]
    res = run_bass_kernel_spmd(nc, in_maps, core_ids=list(range(8)))
    return np.stack([r["y"] for r in res.results], axis=0).astype(np.float32)
```
